# Optimizing a Trainium2 kernel written in Bass

```python
import math
import jax, jax.numpy as jnp
from jax import lax
import numpy as np

D_MODEL = 1024
BATCH = 2
SEQ = 8192
DEPTH = 2

N_MIXERS = 2
N_DSA_LAYERS = (DEPTH + 1) // 2
N_GLA_LAYERS = DEPTH // 2

DN_ALPHA = (2.0 * DEPTH) ** 0.25
DN_BETA = (8.0 * DEPTH) ** -0.25
LN_EPS = 1e-5
RMS_EPS = 1e-6

A_HEADS = 16
A_QK_DIM = 64
A_V_DIM = 64
A_LATENT = 256
IDX_HEADS = 8
IDX_DIM = 64
IDX_TOPK_MAX = 256
Q_BLOCK = 128
D_IN_A = A_HEADS * A_QK_DIM + A_LATENT + IDX_HEADS * IDX_DIM + IDX_DIM + IDX_HEADS

B_HEADS = 4
B_KEY_DIM = D_MODEL // 2
B_VAL_DIM = D_MODEL
B_KH = B_KEY_DIM // B_HEADS
B_VH = B_VAL_DIM // B_HEADS
B_GATE_RANK = 16
B_GATE_TAU = 16.0
B_CHUNK = 64
D_IN_B = 2 * B_KEY_DIM + B_VAL_DIM + B_GATE_RANK + B_VAL_DIM

N_EXPERTS = 32
TOP_K = 4
D_FF = D_MODEL
SWIGLU_LIMIT = 7.0
SWIGLU_ALPHA = 1.702
MOE_BLOCK = 256

kernel_name = "hybrid_dsa_gla_moe_deepnorm"


def layer_norm(x, g, b):
    xf = x.astype(jnp.float32)
    mu = jnp.mean(xf, axis=-1, keepdims=True)
    xc = xf - mu
    var = jnp.mean(xc * xc, axis=-1, keepdims=True)
    y = xc * lax.rsqrt(var + LN_EPS) * g.astype(jnp.float32) + b.astype(jnp.float32)
    return y.astype(x.dtype)


def rms_norm(x, g):
    xf = x.astype(jnp.float32)
    y = xf * lax.rsqrt(jnp.mean(xf * xf, axis=-1, keepdims=True) + RMS_EPS)
    return (y * g.astype(jnp.float32)).astype(x.dtype)


def dsa_mixer(x, w_in, kv_norm, w_uk, w_uv, w_out):
    Bsz, L, _ = x.shape
    proj = x @ w_in
    o1 = A_HEADS * A_QK_DIM
    o2 = o1 + A_LATENT
    o3 = o2 + IDX_HEADS * IDX_DIM
    o4 = o3 + IDX_DIM
    q, c, qi, ki, wi = jnp.split(proj, [o1, o2, o3, o4], axis=-1)
    q = q.reshape(Bsz, L, A_HEADS, A_QK_DIM)
    c = rms_norm(c, kv_norm)
    qi = qi.reshape(Bsz, L, IDX_HEADS, IDX_DIM)
    wi = wi * (IDX_HEADS ** -0.5)
    topk = min(IDX_TOPK_MAX, L // 4)
    nblk = L // Q_BLOCK
    key_pos = jnp.arange(L)

    def to_blocks(t):
        return jnp.moveaxis(t.reshape((Bsz, nblk, Q_BLOCK) + t.shape[2:]), 1, 0)

    def block_fn(args):
        q_b, qi_b, wi_b, blk = args
        qpos = blk * Q_BLOCK + jnp.arange(Q_BLOCK)
        s = jnp.einsum('bqhd,bsd->bqhs', qi_b, ki).astype(jnp.float32) * (IDX_DIM ** -0.5)
        score = jnp.einsum('bqh,bqhs->bqs', wi_b.astype(jnp.float32), jax.nn.relu(s))
        causal = key_pos[None, :] <= qpos[:, None]
        score = jnp.where(causal[None], score, -jnp.inf)
        _, idx = lax.top_k(score, topk)
        valid = idx <= qpos[None, :, None]
        c_sel = jax.vmap(lambda cb, ib: cb[ib])(c, idx)
        q_lat = jnp.einsum('bqhd,hdc->bqhc', q_b, w_uk)
        logits = jnp.einsum('bqhc,bqkc->bqhk', q_lat, c_sel).astype(jnp.float32) * (A_QK_DIM ** -0.5)
        logits = jnp.where(valid[:, :, None, :], logits, -jnp.inf)
        p = jax.nn.softmax(logits, axis=-1).astype(c_sel.dtype)
        return jnp.einsum('bqhk,bqkc->bqhc', p, c_sel)

    o = lax.map(block_fn, (to_blocks(q), to_blocks(qi), to_blocks(wi), jnp.arange(nblk)))
    o = jnp.moveaxis(o, 0, 1).reshape(Bsz, L, A_HEADS, A_LATENT)
    o = jnp.einsum('blhc,hcv->blhv', o, w_uv).reshape(Bsz, L, A_HEADS * A_V_DIM)
    return (o @ w_out).astype(x.dtype)


def gla_mixer(x, w_in, w_g2, g_bias, norm_g, w_out):
    Bsz, L, _ = x.shape
    proj = x @ w_in
    q, k, v, g_lr, r = jnp.split(
        proj, [B_KEY_DIM, 2 * B_KEY_DIM, 2 * B_KEY_DIM + B_VAL_DIM,
               2 * B_KEY_DIM + B_VAL_DIM + B_GATE_RANK], axis=-1)
    gate_logit = (g_lr @ w_g2 + g_bias).astype(jnp.float32)
    log_a = jax.nn.log_sigmoid(gate_logit) / B_GATE_TAU
    nC = L // B_CHUNK

    def to_chunks(t, hd):
        t = t.astype(jnp.float32).reshape(Bsz, nC, B_CHUNK, B_HEADS, hd)
        return jnp.transpose(t, (1, 0, 3, 2, 4))

    qc = to_chunks(q, B_KH) * (B_KH ** -0.5)
    kc = to_chunks(k, B_KH)
    vc = to_chunks(v, B_VH)
    ac = to_chunks(log_a, B_KH)
    tril = jnp.tril(jnp.ones((B_CHUNK, B_CHUNK), dtype=bool))

    def step(S, inp):
        qb, kb, vb, ab = inp
        bcum = jnp.cumsum(ab, axis=2)
        o_inter = jnp.einsum('bhid,bhde->bhie', qb * jnp.exp(bcum), S)
        diff = bcum[:, :, :, None, :] - bcum[:, :, None, :, :]
        decay = jnp.exp(jnp.where(tril[None, None, :, :, None], diff, -jnp.inf))
        A = jnp.einsum('bhijd,bhjd->bhij', qb[:, :, :, None, :] * decay, kb)
        o_intra = jnp.einsum('bhij,bhje->bhie', A, vb)
        b_last = bcum[:, :, -1:, :]
        S_new = jnp.exp(b_last[:, :, 0, :])[..., None] * S + jnp.einsum(
            'bhjd,bhje->bhde', kb * jnp.exp(b_last - bcum), vb)
        return S_new, o_inter + o_intra

    S0 = jnp.zeros((Bsz, B_HEADS, B_KH, B_VH), jnp.float32)
    _, o = lax.scan(step, S0, (qc, kc, vc, ac))
    o = jnp.transpose(o, (1, 0, 3, 2, 4)).reshape(Bsz, L, B_HEADS, B_VH)
    o = rms_norm(o, norm_g.reshape(B_HEADS, B_VH))
    o = o.reshape(Bsz, L, B_VAL_DIM) * jax.nn.silu(r.astype(jnp.float32))
    return (o.astype(x.dtype) @ w_out).astype(x.dtype)


def moe_ffn(x, w_router, b_router, w1, b1, w2, b2):
    Bsz, L, D = x.shape
    N = Bsz * L
    xt = x.reshape(N, D)
    logits = (xt @ w_router + b_router).astype(jnp.float32)
    top_vals, top_idx = lax.top_k(logits, TOP_K)
    gates = jax.nn.softmax(top_vals, axis=-1)
    NK = N * TOP_K
    flat_e = top_idx.reshape(-1)
    flat_tok = jnp.repeat(jnp.arange(N, dtype=jnp.int32), TOP_K)
    flat_gate = gates.reshape(-1)
    order = jnp.argsort(flat_e)
    se, stok, sgate = flat_e[order], flat_tok[order], flat_gate[order]
    counts = jnp.bincount(flat_e, length=N_EXPERTS)
    padded = ((counts + MOE_BLOCK - 1) // MOE_BLOCK) * MOE_BLOCK
    start = jnp.cumsum(counts) - counts
    pend = jnp.cumsum(padded)
    pstart = pend - padded
    dest = pstart[se] + (jnp.arange(NK) - start[se])
    n_blocks = -(-NK // MOE_BLOCK) + N_EXPERTS
    P = n_blocks * MOE_BLOCK
    row_tok = jnp.zeros((P,), jnp.int32).at[dest].set(stok)
    row_gate = jnp.zeros((P,), jnp.float32).at[dest].set(sgate)
    blk_e = jnp.minimum(jnp.searchsorted(pend, jnp.arange(n_blocks) * MOE_BLOCK, side='right'),
                        N_EXPERTS - 1)
    xs = xt[row_tok].reshape(n_blocks, MOE_BLOCK, D)

    def expert_block(args):
        xb, e = args
        h = xb @ w1[e] + b1[e]
        g, u = h[:, :D_FF], h[:, D_FF:]
        g = jnp.minimum(g, SWIGLU_LIMIT)
        u = jnp.clip(u, -SWIGLU_LIMIT, SWIGLU_LIMIT)
        glu = g * jax.nn.sigmoid(g * SWIGLU_ALPHA)
        return ((u + 1.0) * glu) @ w2[e] + b2[e]

    ys = lax.map(expert_block, (xs, blk_e)).reshape(P, D)
    y = jnp.zeros((N, D), x.dtype).at[row_tok].add((ys * row_gate[:, None].astype(ys.dtype)).astype(x.dtype))
    return y.reshape(Bsz, L, D)


def setup_inputs(seed: int = 0) -> dict:
    key = jax.random.key(seed)
    ks = jax.random.split(key, 24)
    f32 = jnp.float32
    nrm = lambda k, shape, s: jax.random.normal(k, shape, f32) * s
    D = D_MODEL
    LA, LB = N_DSA_LAYERS, N_GLA_LAYERS
    return {
        "x": nrm(ks[0], (BATCH, SEQ, D), 1.0),
        "a_w_in": nrm(ks[1], (LA, D, D_IN_A), D ** -0.5),
        "a_kv_norm": 1.0 + nrm(ks[2], (LA, A_LATENT), 0.01),
        "a_w_uk": nrm(ks[3], (LA, A_HEADS, A_QK_DIM, A_LATENT), A_LATENT ** -0.5),
        "a_w_uv": nrm(ks[4], (LA, A_HEADS, A_LATENT, A_V_DIM), A_LATENT ** -0.5 * DN_BETA),
        "a_w_out": nrm(ks[5], (LA, A_HEADS * A_V_DIM, D), (A_HEADS * A_V_DIM) ** -0.5 * DN_BETA),
        "b_w_in": nrm(ks[6], (LB, D, D_IN_B), D ** -0.5),
        "b_w_g2": nrm(ks[7], (LB, B_GATE_RANK, B_KEY_DIM), B_GATE_RANK ** -0.5),
        "b_g_bias": nrm(ks[8], (LB, B_KEY_DIM), 0.1),
        "b_norm": 1.0 + nrm(ks[9], (LB, B_VAL_DIM), 0.01),
        "b_w_out": nrm(ks[10], (LB, B_VAL_DIM, D), B_VAL_DIM ** -0.5 * DN_BETA),
        "m_w_router": nrm(ks[11], (DEPTH, D, N_EXPERTS), D ** -0.5),
        "m_b_router": nrm(ks[12], (DEPTH, N_EXPERTS), 0.01),
        "m_w1": nrm(ks[13], (DEPTH, N_EXPERTS, D, 2 * D_FF), D ** -0.5),
        "m_b1": nrm(ks[14], (DEPTH, N_EXPERTS, 2 * D_FF), 0.01),
        "m_w2": nrm(ks[15], (DEPTH, N_EXPERTS, D_FF, D), D_FF ** -0.5 * DN_BETA),
        "m_b2": nrm(ks[16], (DEPTH, N_EXPERTS, D), 0.01),
        "ln1_g": 1.0 + nrm(ks[17], (DEPTH, D), 0.01),
        "ln1_b": nrm(ks[18], (DEPTH, D), 0.01),
        "ln2_g": 1.0 + nrm(ks[19], (DEPTH, D), 0.01),
        "ln2_b": nrm(ks[20], (DEPTH, D), 0.01),
    }


def reference(x, a_w_in, a_kv_norm, a_w_uk, a_w_uv, a_w_out,
              b_w_in, b_w_g2, b_g_bias, b_norm, b_w_out,
              m_w_router, m_b_router, m_w1, m_b1, m_w2, m_b2,
              ln1_g, ln1_b, ln2_g, ln2_b):
    for i in range(DEPTH):
        j = i // N_MIXERS
        if i % N_MIXERS == 0:
            h = dsa_mixer(x, a_w_in[j], a_kv_norm[j], a_w_uk[j], a_w_uv[j], a_w_out[j])
        else:
            h = gla_mixer(x, b_w_in[j], b_w_g2[j], b_g_bias[j], b_norm[j], b_w_out[j])
        x = layer_norm(DN_ALPHA * x + h, ln1_g[i], ln1_b[i])
        f = moe_ffn(x, m_w_router[i], m_b_router[i], m_w1[i], m_b1[i], m_w2[i], m_b2[i])
        x = layer_norm(DN_ALPHA * x + f, ln2_g[i], ln2_b[i])
    return x
```

```python
import contextlib
import numpy as np
import concourse.bass as bass
import concourse.mybir as mybir
from concourse.bass_utils import run_bass_kernel_spmd

F32 = mybir.dt.float32
BF16 = mybir.dt.bfloat16
I32 = mybir.dt.int32
ALU = mybir.AluOpType
AF = mybir.ActivationFunctionType
AX = mybir.AxisListType

COMPUTE = ("pe", "act", "dve", "pool")


class Op:
    __slots__ = ("eng", "fn", "reads", "writes", "dma", "stream", "seq",
                 "waits", "signal", "sigval", "clock")


class Prog:
    def __init__(self, nc):
        self.nc = nc
        self.ops = []
        self.stack = contextlib.ExitStack()
        self.nsb = 0

    def sb(self, shape, dtype, name=None):
        self.nsb += 1
        return self.stack.enter_context(
            self.nc.sbuf_tensor(name or f"sb{self.nsb}", list(shape), dtype))

    def ps(self, shape, dtype, name=None):
        self.nsb += 1
        return self.stack.enter_context(
            self.nc.psum_tensor(name or f"ps{self.nsb}", list(shape), dtype))

    def add(self, eng, fn, reads=(), writes=(), dma=False, semkey=None):
        o = Op()
        o.eng = eng
        o.fn = fn
        o.reads = tuple(reads)
        o.writes = tuple(writes)
        o.dma = dma
        if dma:
            if semkey is None:
                semkey = o.writes[0]
            o.stream = ("dma", semkey)
        else:
            o.stream = eng
        o.waits = []
        o.signal = False
        self.ops.append(o)
        return o

    def pe(self, fn, reads, writes):
        return self.add("pe", fn, reads, writes)

    def act(self, fn, reads, writes):
        return self.add("act", fn, reads, writes)

    def dve(self, fn, reads, writes):
        return self.add("dve", fn, reads, writes)

    def pool(self, fn, reads, writes):
        return self.add("pool", fn, reads, writes)

    def dma(self, q, out, in_, reads, writes, semkey=None, **kw):
        return self.add(q, lambda e: e.dma_start(out=out, in_=in_, **kw),
                        reads, writes, dma=True, semkey=semkey)

    def finalize(self, final_wait_eng="sp"):
        nc = self.nc
        ops = self.ops
        seqc = {}
        last_writer = {}
        readers = {}
        known = {}
        dma_outstanding = []
        for o in ops:
            seqc[o.stream] = seqc.get(o.stream, 0) + 1
            o.seq = seqc[o.stream]
            deps = []
            for k in o.reads:
                p = last_writer.get(k)
                if p is not None:
                    deps.append((p, "raw"))
            for k in o.writes:
                p = last_writer.get(k)
                if p is not None:
                    deps.append((p, "waw"))
                for p in readers.get(k, {}).values():
                    deps.append((p, "war"))
            kn = known.setdefault(o.eng, {})
            for p, kind in deps:
                if p is o:
                    continue
                if (not p.dma) and (not o.dma) and p.eng == o.eng and kind != "raw":
                    continue
                if (not p.dma) and (not o.dma) and p.eng == o.eng == "pe":
                    continue
                if kn.get(p.stream, 0) >= p.seq:
                    continue
                o.waits.append(p)
                p.signal = True
                for s, v in p.clock.items():
                    if kn.get(s, 0) < v:
                        kn[s] = v
                kn[p.stream] = p.seq
            o.clock = dict(kn)
            for k in o.writes:
                last_writer[k] = o
                readers[k] = {}
            for k in o.reads:
                readers.setdefault(k, {})[o.stream] = o
            if o.dma:
                o.signal = True
        sigc = {}
        for o in ops:
            if o.signal:
                inc = 16 if o.dma else 1
                sigc[o.stream] = sigc.get(o.stream, 0) + inc
                o.sigval = sigc[o.stream]
        sems = {}
        for s in sigc:
            sems[s] = self.stack.enter_context(nc.semaphore(f"s{len(sems)}"))
        self.n_sems = len(sems)
        finals = [(sems[s], v) for s, v in sigc.items()
                  if isinstance(s, tuple)]
        by_eng = {}
        for o in ops:
            by_eng.setdefault(o.eng, []).append(o)

        def emit(name, e):
            for o in by_eng.get(name, []):
                for p in o.waits:
                    e.wait_ge(sems[p.stream], p.sigval)
                ins = o.fn(e)
                if o.signal:
                    ins.then_inc(sems[o.stream], 16 if o.dma else 1)
            if name == final_wait_eng:
                for s, v in finals:
                    e.wait_ge(s, v)

        with nc.Block() as block:
            @block.tensor
            def _(e):
                emit("pe", e)

            @block.scalar
            def _(e):
                emit("act", e)

            @block.vector
            def _(e):
                emit("dve", e)

            @block.gpsimd
            def _(e):
                emit("pool", e)

            @block.sync
            def _(e):
                emit("sp", e)
        self.stack.close()


DN_ALPHA = 4.0 ** 0.25
LN_EPS = 1e-5
NE = 32


def build_post(NT=2048, n_exp=NE, with_outproj=True):
    TT = NT // 512
    nc = bass.Bass("TRN2", target_bir_lowering=False)
    dt = lambda n, s: nc.dram_tensor(n, s, F32, kind="ExternalInput").ap()
    xT = dt("xT", [1024, NT]); mT = dt("mT", [1024, NT]); wout = dt("wout", [1024, 1024])
    lnp = dt("lnp", [128, 32])
    wr = dt("wr", [1024, 32]); brt = dt("brt", [128, 32])
    w1 = dt("w1", [NE, 1024, 2048]); b1T = dt("b1T", [128, NE * 16])
    w2 = dt("w2", [NE, 1024, 1024]); b2 = dt("b2", [NE, 1024])
    ident = dt("ident", [128, 128]); sel = dt("sel", [32, NE * 128])
    outT = nc.dram_tensor("outT", [1024, NT], F32, kind="ExternalOutput").ap()

    P = Prog(nc)
    z = P.sb([128, 8, NT], F32, "z")
    zb = P.sb([128, 8, NT], BF16, "zb")
    w1s = P.sb([128, 2, 8, 1024], BF16, "w1s")
    w2s = P.sb([128, 2, 4, 1024], BF16, "w2s")
    aT = P.sb([128, 2, 4, 512], BF16, "aT")
    tmp = P.sb([128, 8, 512], F32, "tmp")
    lnt = P.sb([128, 6, 512], F32, "lnt")
    gT = P.sb([32, NT], BF16, "gT")
    b1s = P.sb([128, NE * 16], F32, "b1s")
    b2b = P.sb([32, 1024], BF16, "b2b")
    lns = P.sb([128, 32], F32, "lns")
    wrs = P.sb([128, 8, 32], F32, "wrs")
    brs = P.sb([128, 32], F32, "brs")
    ids = P.sb([128, 128], F32, "ids")
    sels = P.sb([32, NE * 128], BF16, "sels")
    ones = P.sb([128, 128], F32, "ones")
    rt = P.sb([128, 8, 32], F32, "rt")
    ps = P.ps([128, 8, 512], F32, "ps")

    P.dma("sp", z[:], xT.rearrange("(c p) t -> p c t", p=128), [], ["z_all"])
    P.dma("pool", zb[:], mT.rearrange("(c p) t -> p c t", p=128), [], ["zb_all"])
    P.dma("pool", w1s[:, 0], wout.rearrange("(c p) n -> p c n", p=128), [], [("w1s", 0)])
    P.dma("sp", lns[:], lnp, [], ["lns"])
    P.dma("sp", wrs[:], wr.rearrange("(c p) n -> p c n", p=128), [], ["wrs"])
    P.dma("sp", brs[:], brt, [], ["brs"])
    P.dma("sp", b1s[:], b1T, [], ["b1s"])
    P.dma("sp", ids[:], ident, [], ["ids"])
    P.dma("pool", b2b[:], b2, [], ["b2b"])
    P.dma("pool", sels[:], sel, [], ["sels"])
    P.dve(lambda e: e.memset(ones[:], 1.0), [], ["ones"])
    b1v = b1s[:].rearrange("p (e c) -> p e c", c=16)
    P.dve(lambda e: e.tensor_scalar(out=b1v[:, :, 8:16], in0=b1v[:, :, 8:16], scalar1=1.0,
                                    scalar2=None, op0=ALU.add), ["b1s"], ["b1s"])

    def zk(c, t):
        return ("z", c, t)

    first_z = [True]

    def zreads(c, t):
        return [zk(c, t), "z_all"]

    if with_outproj:
        for t in range(TT):
            ts = slice(t * 512, (t + 1) * 512)
            for j in range(8):
                bank = (t * 8 + j) % 4
                for kc in range(8):
                    P.pe(lambda e, bank=bank, kc=kc, j=j, ts=ts: e.matmul(
                        ps[:, bank, :], lhsT=w1s[:, 0, kc, j * 128:(j + 1) * 128],
                        rhs=zb[:, kc, ts], start=(kc == 0), stop=(kc == 7)),
                        [("w1s", 0), "zb_all"], [("ps", bank)])
                P.dve(lambda e, bank=bank, j=j, ts=ts: e.scalar_tensor_tensor(
                    out=z[:, j, ts], in0=z[:, j, ts], scalar=DN_ALPHA, op0=ALU.mult,
                    in1=ps[:, bank, :], op1=ALU.add),
                    zreads(j, t) + [("ps", bank)], [zk(j, t)])

    def layer_norm(goff, boff, make_bf16):
        for t in range(TT):
            ts = slice(t * 512, (t + 1) * 512)
            for c in range(8):
                P.act(lambda e, c=c, ts=ts: e.activation(out=tmp[:, c, :], in_=z[:, c, ts], func=AF.Square),
                      zreads(c, t), [("tmp", c)])
            for c in range(8):
                P.pe(lambda e, c=c, ts=ts: e.matmul(ps[:, 6, :], lhsT=ones[:], rhs=z[:, c, ts],
                                                    start=(c == 0), stop=(c == 7)),
                     zreads(c, t) + ["ones"], [("ps", 6)])
            for c in range(8):
                P.pe(lambda e, c=c: e.matmul(ps[:, 7, :], lhsT=ones[:], rhs=tmp[:, c, :],
                                             start=(c == 0), stop=(c == 7)),
                     [("tmp", c), "ones"], [("ps", 7)])
            mean, m2, var, rstd, mr = (lnt[:, i, :] for i in range(5))
            P.dve(lambda e: e.tensor_scalar(out=mean, in0=ps[:, 6, :], scalar1=1.0 / 1024, scalar2=None,
                                            op0=ALU.mult), [("ps", 6)], [("lnt", 0)])
            P.dve(lambda e: e.tensor_tensor(out=m2, in0=mean, in1=mean, op=ALU.mult),
                  [("lnt", 0)], [("lnt", 1)])
            P.dve(lambda e: e.scalar_tensor_tensor(out=var, in0=ps[:, 7, :], scalar=1.0 / 1024, op0=ALU.mult,
                                                   in1=m2, op1=ALU.subtract),
                  [("ps", 7), ("lnt", 1)], [("lnt", 2)])
            P.dve(lambda e: e.tensor_scalar(out=var, in0=var, scalar1=LN_EPS, scalar2=None, op0=ALU.add),
                  [("lnt", 2)], [("lnt", 2)])
            P.act(lambda e: e.activation(out=rstd, in_=var, func=AF.Sqrt),
                  [("lnt", 2)], [("lnt", 3)])
            P.dve(lambda e: e.reciprocal(out=rstd, in_=rstd), [("lnt", 3)], [("lnt", 3)])
            P.dve(lambda e: e.tensor_tensor(out=mr, in0=mean, in1=rstd, op=ALU.mult),
                  [("lnt", 0), ("lnt", 3)], [("lnt", 4)])
            for c in range(8):
                P.dve(lambda e, c=c, ts=ts: e.tensor_tensor(out=tmp[:, c, :], in0=z[:, c, ts], in1=rstd, op=ALU.mult),
                      zreads(c, t) + [("lnt", 3)], [("tmp", c)])
                P.dve(lambda e, c=c: e.tensor_tensor(out=tmp[:, c, :], in0=tmp[:, c, :], in1=mr, op=ALU.subtract),
                      [("tmp", c), ("lnt", 4)], [("tmp", c)])
                P.act(lambda e, c=c, ts=ts: e.activation(out=z[:, c, ts], in_=tmp[:, c, :], func=AF.Identity,
                                                         scale=lns[:, goff + c:goff + c + 1],
                                                         bias=lns[:, boff + c:boff + c + 1]),
                      [("tmp", c), "lns", "z_all"], [zk(c, t)])
                if make_bf16:
                    P.pool(lambda e, c=c, ts=ts: e.tensor_copy(out=zb[:, c, ts], in_=z[:, c, ts]),
                           [zk(c, t), "zb_all"], [("zb", t)])

    layer_norm(0, 8, True)

    for tt in range(NT // 128):
        tsl = slice(tt * 128, (tt + 1) * 128)
        t = tt // 4
        for c in range(8):
            P.pe(lambda e, c=c, tsl=tsl: e.matmul(ps[:, 7, 0:32], lhsT=z[:, c, tsl], rhs=wrs[:, c, :],
                                                  start=(c == 0), stop=(c == 7)),
                 [zk(c, t), "wrs"], [("ps", 7)])
        lg, m8, negm, ex, em, ssum, gt = (rt[:, i, :] for i in range(7))
        P.dve(lambda e: e.tensor_tensor(out=lg, in0=ps[:, 7, 0:32], in1=brs[:], op=ALU.add),
              [("ps", 7), "brs"], ["rt0"])
        P.dve(lambda e: e.max(out=m8[:, 0:8], in_=lg), ["rt0"], ["rt1"])
        P.dve(lambda e: e.tensor_scalar(out=negm[:, 0:1], in0=m8[:, 0:1], scalar1=-1.0, scalar2=None, op0=ALU.mult),
              ["rt1"], ["rt2"])
        P.act(lambda e: e.activation(out=ex, in_=lg, func=AF.Exp, bias=negm[:, 0:1], scale=1.0),
              ["rt0", "rt2"], ["rt3"])
        P.dve(lambda e: e.scalar_tensor_tensor(out=em, in0=lg, scalar=m8[:, 3:4], op0=ALU.is_ge,
                                               in1=ex, op1=ALU.mult, accum_out=ssum[:, 0:1]),
              ["rt0", "rt1", "rt3"], ["rt4", "rt5"])
        P.dve(lambda e: e.reciprocal(out=ssum[:, 1:2], in_=ssum[:, 0:1]), ["rt5"], ["rt5b"])
        P.dve(lambda e: e.tensor_scalar(out=gt, in0=em, scalar1=ssum[:, 1:2], scalar2=None, op0=ALU.mult),
              ["rt4", "rt5b"], ["rt6"])
        P.pe(lambda e: e.transpose(ps[0:32, 6, 0:128], gt, ids[:]), ["rt6", "ids"], [("ps", 6)])
        P.act(lambda e, tsl=tsl: e.copy(out=gT[:, tsl], in_=ps[0:32, 6, 0:128]), [("ps", 6)], ["gT"])

    for t in range(TT):
        ts = slice(t * 512, (t + 1) * 512)
        for j in range(8):
            bank = 4 + (j % 2)
            P.pe(lambda e, bank=bank, j=j, ts=ts: e.matmul(ps[:, bank, :], lhsT=b2b[:, j * 128:(j + 1) * 128],
                                                          rhs=gT[:, ts], start=True, stop=True),
                 ["b2b", "gT"], [("ps", bank)])
            P.dve(lambda e, bank=bank, j=j, ts=ts: e.scalar_tensor_tensor(
                out=z[:, j, ts], in0=z[:, j, ts], scalar=DN_ALPHA, op0=ALU.mult,
                in1=ps[:, bank, :], op1=ALU.add),
                [zk(j, t), ("ps", bank)], [zk(j, t)])

    w1v = w1.rearrange("e (kc p) n -> e p kc n", p=128)
    w2v = w2.rearrange("e (fc p) d -> e p fc d", p=128)
    units = [(ex_, h) for ex_ in range(n_exp) for h in range(2)]

    def load_unit(u):
        ex_, h = units[u]
        s = u % 2
        P.dma("pool", w1s[:, s, :, 0:512], w1v[ex_, :, :, h * 512:(h + 1) * 512], [], [("w1s", s)])
        P.dma("pool", w1s[:, s, :, 512:1024], w1v[ex_, :, :, 1024 + h * 512:1024 + (h + 1) * 512], [], [("w1s", s)])
        P.dma("pool", w2s[:, s], w2v[ex_, :, h * 4:(h + 1) * 4, :], [], [("w2s", s)])

    cnt = [0]

    def emit_gu(u, t):
        ex_, h = units[u]
        s = u % 2
        it = cnt[0]
        cnt[0] += 1
        ab = it % 2
        ts = slice(t * 512, (t + 1) * 512)
        P.pe(lambda e: e.matmul(ps[:, 6, :], lhsT=sels[:, ex_ * 128:(ex_ + 1) * 128], rhs=gT[:, ts],
                                start=True, stop=True), ["sels", "gT"], [("ps", 6)])
        for fl in range(4):
            pb = 2 * ((it * 4 + fl) % 2)
            tb = 4 * ((it * 4 + fl) % 2)
            for kc in range(8):
                P.pe(lambda e, kc=kc, fl=fl, pb=pb: e.matmul(
                    ps[:, pb, :], lhsT=w1s[:, s, kc, fl * 128:(fl + 1) * 128], rhs=zb[:, kc, ts],
                    start=(kc == 0), stop=(kc == 7)), [("w1s", s), ("zb", t)], [("ps", pb)])
            for kc in range(8):
                P.pe(lambda e, kc=kc, fl=fl, pb=pb: e.matmul(
                    ps[:, pb + 1, :], lhsT=w1s[:, s, kc, 512 + fl * 128:512 + (fl + 1) * 128], rhs=zb[:, kc, ts],
                    start=(kc == 0), stop=(kc == 7)), [("w1s", s), ("zb", t)], [("ps", pb + 1)])
            fch = h * 4 + fl
            bg = b1s[:, ex_ * 16 + fch:ex_ * 16 + fch + 1]
            bu = b1s[:, ex_ * 16 + 8 + fch:ex_ * 16 + 8 + fch + 1]
            g, sg, glu, u1 = (tmp[:, tb + i, :] for i in range(4))
            P.dve(lambda e, g=g, pb=pb, bg=bg: e.tensor_scalar(out=g, in0=ps[:, pb, :], scalar1=bg, scalar2=7.0,
                                                               op0=ALU.add, op1=ALU.min),
                  [("ps", pb), "b1s"], [("tmp", tb)])
            P.act(lambda e, g=g, sg=sg: e.activation(out=sg, in_=g, func=AF.Sigmoid, scale=1.702),
                  [("tmp", tb)], [("tmp", tb + 1)])
            P.dve(lambda e, u1=u1, pb=pb, bu=bu: e.tensor_scalar(out=u1, in0=ps[:, pb + 1, :], scalar1=bu, scalar2=-6.0,
                                                                 op0=ALU.add, op1=ALU.max),
                  [("ps", pb + 1), "b1s"], [("tmp", tb + 3)])
            P.dve(lambda e, g=g, sg=sg, glu=glu: e.tensor_tensor(out=glu, in0=g, in1=sg, op=ALU.mult),
                  [("tmp", tb), ("tmp", tb + 1)], [("tmp", tb + 2)])
            P.dve(lambda e, u1=u1, glu=glu: e.scalar_tensor_tensor(out=u1, in0=u1, scalar=8.0, op0=ALU.min,
                                                                  in1=glu, op1=ALU.mult),
                  [("tmp", tb + 3), ("tmp", tb + 2)], [("tmp", tb + 3)])
            P.dve(lambda e, u1=u1, fl=fl: e.tensor_tensor(out=aT[:, ab, fl, :], in0=u1, in1=ps[:, 6, :], op=ALU.mult),
                  [("tmp", tb + 3), ("ps", 6)], [("aT", ab)])
        return (u, t, ab)

    def emit_y(info):
        u, t, ab = info
        s = u % 2
        ts = slice(t * 512, (t + 1) * 512)
        for j in range(8):
            bank = 4 + (j % 2)
            for fl in range(4):
                P.pe(lambda e, fl=fl, j=j, bank=bank: e.matmul(
                    ps[:, bank, :], lhsT=w2s[:, s, fl, j * 128:(j + 1) * 128], rhs=aT[:, ab, fl, :],
                    start=(fl == 0), stop=(fl == 3)), [("w2s", s), ("aT", ab)], [("ps", bank)])
            P.dve(lambda e, j=j, bank=bank: e.tensor_tensor(out=z[:, j, ts], in0=z[:, j, ts], in1=ps[:, bank, :],
                                                            op=ALU.add),
                  [zk(j, t), ("ps", bank)], [zk(j, t)])

    load_unit(0)
    prev = None
    for u in range(len(units)):
        for t in range(TT):
            info = emit_gu(u, t)
            if prev is not None:
                emit_y(prev)
            prev = info
            if t == 0 and u + 1 < len(units):
                load_unit(u + 1)
    emit_y(prev)

    layer_norm(16, 24, False)
    ov = outT.rearrange("(c p) t -> p c t", p=128)
    for t in range(TT):
        ts = slice(t * 512, (t + 1) * 512)
        P.dma("sp", ov[:, :, ts], z[:, :, ts], [zk(c, t) for c in range(8)], [("out", t)], semkey="store")
    P.finalize()
    return nc, len(P.ops)


def post_inputs(xT, mT, wout, g1, b1_, g2, b2_, wr, br, w1, b1, w2, b2):
    pc = lambda v: np.ascontiguousarray(v.reshape(8, 128).T)
    lnp = np.concatenate([pc(g1), pc(b1_), pc(g2), pc(b2_)], axis=1).astype(np.float32)
    b1T = np.ascontiguousarray(b1.reshape(NE, 16, 128).transpose(2, 0, 1).reshape(128, NE * 16))
    sel = np.zeros((32, NE, 128), np.float32)
    for e in range(NE):
        sel[e, e, :] = 1.0
    return {
        "xT": np.ascontiguousarray(xT), "mT": np.ascontiguousarray(mT), "wout": np.ascontiguousarray(wout),
        "lnp": lnp, "wr": np.ascontiguousarray(wr), "brt": np.ascontiguousarray(np.broadcast_to(br[None, :], (128, 32))),
        "w1": w1, "b1T": b1T, "w2": w2, "b2": np.ascontiguousarray(b2),
        "ident": np.eye(128, dtype=np.float32), "sel": sel.reshape(32, NE * 128),
    }


RMS_EPS = 1e-6
NEG = -1.0e30
NBIS = 24


def build_dsa(n_slots=16, SEQ=8192):
    NKT = SEQ // 512
    NQ = n_slots * 128
    nc = bass.Bass("TRN2", target_bir_lowering=False, dynamic_dma_scratch_size=8192)
    dt = lambda n, s: nc.dram_tensor(n, s, F32, kind="ExternalInput").ap()
    xbT = dt("xbT", [1024, SEQ]); xqT = dt("xqT", [1024, 2048])
    wq = dt("wq", [1024, 1024]); wc = dt("wc", [1024, 256]); wqi = dt("wqi", [1024, 512])
    wki = dt("wki", [1024, 64]); wwi = dt("wwi", [1024, 8])
    kvp = dt("kvp", [128, 2]); kvb = dt("kvb", [128, 256])
    wuk = dt("wuk", [16, 64, 256]); wuv = dt("wuv", [16, 256, 64])
    cmask = dt("cmask", [128, 512]); ident = dt("ident", [128, 128])
    ovT = nc.dram_tensor("ovT", [1024, 2048], F32, kind="ExternalOutput").ap()

    P = Prog(nc)
    kiT = P.sb([64, SEQ], BF16, "kiT")
    cT = P.sb([128, 2, SEQ], BF16, "cT")
    C = P.sb([128, SEQ // 128, 256], BF16, "C")
    sc = P.sb([128, SEQ], F32, "sc")
    junk = P.sb([128, 2048], BF16, "junk")
    maskT = P.sb([128, SEQ // 128, 128], BF16, "maskT")
    wqb = P.sb([128, 8, 1024], BF16, "wqb")
    wqib = P.sb([128, 8, 512], BF16, "wqib")
    wcb = P.sb([128, 8, 256], BF16, "wcb")
    wkib = P.sb([128, 8, 64], BF16, "wkib")
    wwib = P.sb([128, 8, 8], BF16, "wwib")
    wukb = P.sb([64, 16, 256], BF16, "wukb")
    wuvb = P.sb([128, 16, 2, 64], BF16, "wuvb")
    kvps = P.sb([128, 2], F32, "kvps"); kvbs = P.sb([128, 256], F32, "kvbs")
    cms = P.sb([128, 512], F32, "cms")
    idb = P.sb([128, 128], BF16, "idb")
    onesf = P.sb([128, 128], F32, "onesf"); onesb = P.sb([128, 128], BF16, "onesb")
    half = P.sb([128, 1], F32, "half")
    R = P.sb([128, 14336], BF16, "R")
    xkb = R[:, 0:8192].rearrange("p (s c t) -> p s c t", s=2, c=8)
    cpre = R[:, 8192:10240].bitcast(F32).rearrange("p (c t) -> p c t", c=2)
    sq = R[:, 10240:12288].bitcast(F32).rearrange("p (c t) -> p c t", c=2)
    rinvk = R[:, 12288:13312].bitcast(F32)
    ktmp = R[:, 13312:13824].bitcast(F32)
    ksm = P.sb([128, 4], F32, "ksm")
    qT = R[0:64, 0:2048].rearrange("p (h q) -> p h q", h=16)
    qiT = R[0:64, 2048:3072].rearrange("p (h q) -> p h q", h=8)
    qlT = R[:, 3072:7168].rearrange("p (c h q) -> p c h q", c=2, h=16)
    rbuf = R[:, 7168:8704].rearrange("p (r t) -> p r t", r=3)
    pe_ = R[:, 8704:9728].rearrange("p (r t) -> p r t", r=2)
    pm = R[:, 9728:10752].rearrange("p (r t) -> p r t", r=2)
    oT = R[:, 10752:11776].rearrange("p (h c q) -> p h c q", h=4, c=2)
    rinv = R[:, 11776:12800].bitcast(F32)
    xqb = P.sb([128, 8, 128], BF16, "xqb")
    wis = P.sb([128, 8], F32, "wis")
    ovs = P.sb([64, 16, 128], F32, "ovs")
    fdum = P.sb([128, 1], F32, "fdum")
    bs = P.sb([128, 8], F32, "bs")
    cnt4 = P.sb([128, 4], F32, "cnt4")
    ps = P.ps([128, 8, 512], F32, "ps")
    psb = ps[:].bitcast(BF16)

    r3 = lambda a: a.rearrange("(c p) n -> p c n", p=128)
    P.dma("pool", wcb[:], r3(wc), [], ["wcb"])
    P.dma("pool", wkib[:], r3(wki), [], ["wkib"])
    P.dma("pool", wqb[:], r3(wq), [], ["wqb"])
    P.dma("pool", wqib[:], r3(wqi), [], ["wqib"])
    P.dma("pool", wwib[:], r3(wwi), [], ["wwib"])
    P.dma("pool", wukb[:], wuk.rearrange("h d c -> d h c"), [], ["wukb"])
    P.dma("pool", wuvb[:], wuv.rearrange("h (cc p) v -> p h cc v", p=128), [], ["wuvb"])
    P.dma("pool", idb[:], ident, [], ["idb"])
    P.dma("sp", kvps[:], kvp, [], ["kvps"])
    P.dma("sp", kvbs[:], kvb, [], ["kvbs"])
    P.dma("sp", cms[:], cmask, [], ["cms"])
    P.dve(lambda e: e.memset(onesf[:], 1.0), [], ["onesf"])
    P.dve(lambda e: e.memset(onesb[:], 1.0), [], ["onesb"])
    P.dve(lambda e: e.memset(half[:], 0.5), [], ["half"])
    zerosb = P.sb([128, 128], BF16, "zerosb")
    P.dve(lambda e: e.memset(zerosb[:], 0.0), [], ["zerosb"])

    xbv = xbT.rearrange("(c p) t -> p c t", p=128)
    for kt in range(NKT):
        sl = kt % 2
        ks = slice(kt * 512, (kt + 1) * 512)
        P.dma("pool", xkb[:, sl], xbv[:, :, ks], [], [("xkb", sl)])
        for kc in range(8):
            P.pe(lambda e, kc=kc, sl=sl: e.matmul(ps[0:64, 0, :], lhsT=wkib[:, kc, :], rhs=xkb[:, sl, kc, :],
                                                  start=(kc == 0), stop=(kc == 7)),
                 ["wkib", ("xkb", sl)], [("ps", 0)])
        P.act(lambda e, ks=ks: e.copy(out=kiT[:, ks], in_=ps[0:64, 0, :]), [("ps", 0)], ["kiT"])
        for cc in range(2):
            for kc in range(8):
                P.pe(lambda e, kc=kc, sl=sl, cc=cc: e.matmul(ps[:, 1 + cc, :], lhsT=wcb[:, kc, cc * 128:(cc + 1) * 128],
                                                            rhs=xkb[:, sl, kc, :], start=(kc == 0), stop=(kc == 7)),
                     ["wcb", ("xkb", sl)], [("ps", 1 + cc)])
            P.act(lambda e, cc=cc: e.copy(out=cpre[:, cc, :], in_=ps[:, 1 + cc, :]), [("ps", 1 + cc)], [("cpre", cc)])
            P.act(lambda e, cc=cc: e.activation(out=sq[:, cc, :], in_=ps[:, 1 + cc, :], func=AF.Square),
                  [("ps", 1 + cc)], [("sq", cc)])
        for cc in range(2):
            P.pe(lambda e, cc=cc: e.matmul(ps[:, 3, :], lhsT=onesf[:], rhs=sq[:, cc, :], start=(cc == 0), stop=(cc == 1)),
                 ["onesf", ("sq", cc)], [("ps", 3)])
        P.dve(lambda e: e.tensor_scalar(out=rinvk, in0=ps[:, 3, :], scalar1=1.0 / 256, scalar2=RMS_EPS,
                                        op0=ALU.mult, op1=ALU.add), [("ps", 3)], ["rinvk"])
        P.act(lambda e: e.activation(out=rinvk, in_=rinvk, func=AF.Sqrt), ["rinvk"], ["rinvk"])
        P.dve(lambda e: e.reciprocal(out=rinvk, in_=rinvk), ["rinvk"], ["rinvk"])
        for cc in range(2):
            P.dve(lambda e, cc=cc, ks=ks: e.scalar_tensor_tensor(out=cT[:, cc, ks], in0=cpre[:, cc, :],
                                                                 scalar=kvps[:, cc:cc + 1], op0=ALU.mult,
                                                                 in1=rinvk, op1=ALU.mult),
                  [("cpre", cc), "kvps", "rinvk"], ["cT"])
        for k4 in range(4):
            kb = kt * 4 + k4
            bank = 4 + (kb % 2)
            for kc in range(8):
                P.pe(lambda e, kc=kc, sl=sl, k4=k4, bank=bank: e.matmul(
                    ps[:, bank, 0:256], lhsT=xkb[:, sl, kc, k4 * 128:(k4 + 1) * 128], rhs=wcb[:, kc, :],
                    start=(kc == 0), stop=(kc == 7)), ["wcb", ("xkb", sl)], [("ps", bank)])
            P.act(lambda e, bank=bank: e.activation(out=ktmp, in_=ps[:, bank, 0:256], func=AF.Square,
                                                    accum_out=ksm[:, 0:1]), [("ps", bank)], ["ktmp", "ksm0"])
            P.dve(lambda e: e.tensor_scalar(out=ksm[:, 1:2], in0=ksm[:, 0:1], scalar1=1.0 / 256, scalar2=RMS_EPS,
                                            op0=ALU.mult, op1=ALU.add), ["ksm0"], ["ksm1"])
            P.act(lambda e: e.activation(out=ksm[:, 2:3], in_=ksm[:, 1:2], func=AF.Sqrt), ["ksm1"], ["ksm2"])
            P.dve(lambda e: e.reciprocal(out=ksm[:, 3:4], in_=ksm[:, 2:3]), ["ksm2"], ["ksm3"])
            P.dve(lambda e, kb=kb, bank=bank: e.scalar_tensor_tensor(out=C[:, kb, :], in0=ps[:, bank, 0:256],
                                                                    scalar=ksm[:, 3:4], op0=ALU.mult,
                                                                    in1=kvbs[:], op1=ALU.mult),
                  [("ps", bank), "ksm3", "kvbs"], ["C"])

    kkeys = [("xkb", 0), ("xkb", 1), ("cpre", 0), ("cpre", 1), ("sq", 0), ("sq", 1), "rinvk", "ktmp"]
    qkeys = ["qT", "qiT", "qlT", ("rbuf", 0), ("rbuf", 1), ("rbuf", 2), ("pe", 0), ("pe", 1),
             ("pm", 0), ("pm", 1), "oT", "rinv"]
    P.dve(lambda e: e.memset(fdum[:], 0.0), [], kkeys + qkeys + ["fdum"])
    xqv = xqT.rearrange("(c p) t -> p c t", p=128)
    ovv = ovT.rearrange("(h p) t -> p h t", p=64)
    lo, hi, mid, cnt, ge, d1 = (bs[:, i:i + 1] for i in range(6))
    for s in range(n_slots):
        qs = slice(s * 128, (s + 1) * 128)
        nk = 512 * (s + 1)
        nkb = nk // 128
        P.dma("pool", xqb[:], xqv[:, :, qs], [], ["xqb"])
        for h in range(16):
            bank = h % 2
            for kc in range(8):
                P.pe(lambda e, h=h, kc=kc, bank=bank: e.matmul(ps[0:64, bank, 0:128], lhsT=wqb[:, kc, h * 64:(h + 1) * 64],
                                                                rhs=xqb[:, kc, :], start=(kc == 0), stop=(kc == 7)),
                     ["wqb", "xqb"], [("ps", bank)])
            P.act(lambda e, h=h, bank=bank: e.copy(out=qT[:, h, :], in_=ps[0:64, bank, 0:128]), [("ps", bank)], ["qT"])
        for h in range(8):
            bank = h % 2
            for kc in range(8):
                P.pe(lambda e, h=h, kc=kc, bank=bank: e.matmul(ps[0:64, bank, 0:128], lhsT=wqib[:, kc, h * 64:(h + 1) * 64],
                                                                rhs=xqb[:, kc, :], start=(kc == 0), stop=(kc == 7)),
                     ["wqib", "xqb"], [("ps", bank)])
            P.act(lambda e, h=h, bank=bank: e.copy(out=qiT[:, h, :], in_=ps[0:64, bank, 0:128]), [("ps", bank)], ["qiT"])
        for kc in range(8):
            P.pe(lambda e, kc=kc: e.matmul(ps[:, 2, 0:8], lhsT=xqb[:, kc, :], rhs=wwib[:, kc, :],
                                           start=(kc == 0), stop=(kc == 7)), ["wwib", "xqb"], [("ps", 2)])
        P.act(lambda e: e.copy(out=wis[:], in_=ps[:, 2, 0:8]), [("ps", 2)], ["wis"])
        for cc in range(2):
            for hg in range(4):
                bank = 4 + ((cc * 4 + hg) % 2)
                for hl in range(4):
                    h = hg * 4 + hl
                    P.pe(lambda e, h=h, hl=hl, cc=cc, bank=bank: e.matmul(
                        ps[:, bank, hl * 128:(hl + 1) * 128], lhsT=wukb[:, h, cc * 128:(cc + 1) * 128], rhs=qT[:, h, :],
                        start=True, stop=True), ["wukb", "qT"], [("ps", bank)])
                P.act(lambda e, cc=cc, hg=hg, bank=bank: e.activation(
                    out=qlT[:, cc, hg * 4:(hg + 1) * 4, :], in_=ps[:, bank, :].rearrange("p (h q) -> p h q", h=4),
                    func=AF.Copy, scale=0.125), [("ps", bank)], ["qlT"])
        it = 0
        for kt in range(s + 1):
            ks = slice(kt * 512, (kt + 1) * 512)
            for h in range(8):
                bank = it % 2
                rb = it % 3
                it += 1
                P.pe(lambda e, h=h, ks=ks, bank=bank: e.matmul(ps[:, bank, :], lhsT=qiT[:, h, :], rhs=kiT[:, ks],
                                                               start=True, stop=True), ["qiT", "kiT"], [("ps", bank)])
                P.act(lambda e, bank=bank, rb=rb: e.activation(out=rbuf[:, rb, :], in_=ps[:, bank, :], func=AF.Relu),
                      [("ps", bank)], [("rbuf", rb)])
                if h == 0:
                    P.dve(lambda e, rb=rb, ks=ks: e.tensor_scalar(out=sc[:, ks], in0=rbuf[:, rb, :], scalar1=wis[:, 0:1],
                                                                  scalar2=None, op0=ALU.mult),
                          [("rbuf", rb), "wis"], [("sc", kt)])
                else:
                    P.dve(lambda e, rb=rb, ks=ks, h=h: e.scalar_tensor_tensor(
                        out=sc[:, ks], in0=rbuf[:, rb, :], scalar=wis[:, h:h + 1], op0=ALU.mult,
                        in1=sc[:, ks], op1=ALU.add), [("rbuf", rb), "wis", ("sc", kt)], [("sc", kt)])
        sck = [("sc", kt) for kt in range(s + 1)]
        P.dve(lambda e, nk=nk: e.tensor_reduce(out=lo, in_=sc[:, 0:nk], op=ALU.min, axis=AX.X), sck, ["lo"])
        P.dve(lambda e: e.tensor_scalar(out=lo, in0=lo, scalar1=-1.0, scalar2=None, op0=ALU.add), ["lo"], ["lo"])
        P.dve(lambda e, nk=nk: e.tensor_tensor(out=sc[:, nk - 512:nk], in0=sc[:, nk - 512:nk], in1=cms[:], op=ALU.add),
              [("sc", s), "cms"], [("sc", s)])
        P.dve(lambda e, nk=nk: e.tensor_reduce(out=hi, in_=sc[:, 0:nk], op=ALU.max, axis=AX.X), sck, ["hi"])
        nch = (nk + 2047) // 2048
        for itb in range(NBIS):
            P.dve(lambda e: e.scalar_tensor_tensor(out=mid, in0=lo, scalar=hi, op0=ALU.add, in1=half[:], op1=ALU.mult),
                  ["lo", "hi", "half"], ["mid"])
            for ch in range(nch):
                c0 = ch * 2048
                c1 = min(nk, c0 + 2048)
                P.dve(lambda e, c0=c0, c1=c1, ch=ch: e.tensor_scalar(out=junk[:, 0:c1 - c0], in0=sc[:, c0:c1], scalar1=mid,
                                                                     scalar2=None, op0=ALU.is_ge, op1=ALU.add,
                                                                     accum_out=cnt4[:, ch:ch + 1]),
                      sck + ["mid"], ["junk", ("cnt4", ch)])
            if nch == 1:
                P.dve(lambda e: e.tensor_copy(out=cnt, in_=cnt4[:, 0:1]), [("cnt4", 0)], ["cnt"])
            else:
                P.dve(lambda e, nch=nch: e.tensor_reduce(out=cnt, in_=cnt4[:, 0:nch], op=ALU.add, axis=AX.X),
                      [("cnt4", i) for i in range(nch)], ["cnt"])
            P.dve(lambda e: e.tensor_scalar(out=ge, in0=cnt, scalar1=256.0, scalar2=None, op0=ALU.is_ge),
                  ["cnt"], ["ge"])
            P.dve(lambda e: e.tensor_tensor(out=d1, in0=mid, in1=lo, op=ALU.subtract), ["mid", "lo"], ["d1"])
            P.dve(lambda e: e.scalar_tensor_tensor(out=lo, in0=d1, scalar=ge, op0=ALU.mult, in1=lo, op1=ALU.add),
                  ["d1", "ge", "lo"], ["lo"])
            P.dve(lambda e: e.tensor_tensor(out=d1, in0=hi, in1=mid, op=ALU.subtract), ["mid", "hi"], ["d1"])
            P.dve(lambda e: e.scalar_tensor_tensor(out=hi, in0=d1, scalar=ge, op0=ALU.mult, in1=mid, op1=ALU.add),
                  ["d1", "ge", "mid"], ["hi"])
        for ch in range(nch):
            c0 = ch * 2048
            c1 = min(nk, c0 + 2048)
            P.dve(lambda e, c0=c0, c1=c1: e.tensor_scalar(out=junk[:, 0:c1 - c0], in0=sc[:, c0:c1], scalar1=lo,
                                                          scalar2=None, op0=ALU.is_ge), sck + ["lo"], ["junk"])
            for g4 in range((c1 - c0) // 512):
                bank = 2 + (g4 % 2)
                for k4 in range(4):
                    col = g4 * 512 + k4 * 128
                    P.pe(lambda e, col=col, k4=k4, bank=bank: e.transpose(
                        psb[:, bank, k4 * 128:(k4 + 1) * 128], junk[:, col:col + 128], idb[:]),
                        ["junk", "idb"], [("ps", bank)])
                kb0 = (c0 + g4 * 512) // 128
                P.act(lambda e, kb0=kb0, bank=bank: e.copy(
                    out=maskT[:, kb0:kb0 + 4, :], in_=psb[:, bank, 0:512].rearrange("p (k q) -> p k q", k=4)),
                    [("ps", bank)], ["maskT"])
        step = 0
        for hg in range(4):
            for kb in range(nkb):
                sb_ = step % 2
                step += 1
                for cc in range(2):
                    P.pe(lambda e, cc=cc, kb=kb, sb_=sb_, hg=hg: e.matmul(
                        ps[:, sb_, :], lhsT=cT[:, cc, kb * 128:(kb + 1) * 128],
                        rhs=qlT[:, cc, hg * 4:(hg + 1) * 4, :], start=(cc == 0), stop=(cc == 1)),
                        ["cT", "qlT"], [("ps", sb_)])
                P.act(lambda e, sb_=sb_: e.activation(out=pe_[:, sb_, :], in_=ps[:, sb_, :], func=AF.Exp),
                      [("ps", sb_)], [("pe", sb_)])
                P.dve(lambda e, sb_=sb_, kb=kb: e.tensor_tensor(
                    out=pm[:, sb_, :].rearrange("p (h q) -> p h q", h=4),
                    in0=pe_[:, sb_, :].rearrange("p (h q) -> p h q", h=4),
                    in1=maskT[:, kb, :].unsqueeze(1).broadcast_to([128, 4, 128]), op=ALU.mult),
                    [("pe", sb_), "maskT"], [("pm", sb_)])
                if kb == 0:
                    for b2 in range(2):
                        P.pe(lambda e, b2=b2, sb_=sb_: e.matmul(ps[:, 4 + b2, :], lhsT=zerosb[:], rhs=pm[:, sb_, :],
                                                              start=True, stop=False),
                             ["zerosb", ("pm", sb_)], [("ps", 4 + b2)])
                for hl in range(4):
                    for cc in range(2):
                        P.pe(lambda e, hl=hl, cc=cc, kb=kb, sb_=sb_, nkb_=nkb: e.matmul(
                            ps[:, 4 + hl // 2, ((hl % 2) * 2 + cc) * 128:((hl % 2) * 2 + cc + 1) * 128],
                            lhsT=C[:, kb, cc * 128:(cc + 1) * 128], rhs=pm[:, sb_, hl * 128:(hl + 1) * 128],
                            start=False, stop=(kb == nkb_ - 1)), ["C", ("pm", sb_)], [("ps", 4 + hl // 2)])
                P.pe(lambda e, kb=kb, sb_=sb_, nkb_=nkb: e.matmul(ps[:, 6, :], lhsT=onesb[:], rhs=pm[:, sb_, :],
                                                        start=(kb == 0), stop=(kb == nkb_ - 1)),
                     ["onesb", ("pm", sb_)], [("ps", 6)])
            P.dve(lambda e: e.reciprocal(out=rinv, in_=ps[:, 6, :]), [("ps", 6)], ["rinv"])
            for hl in range(4):
                for cc in range(2):
                    P.dve(lambda e, hl=hl, cc=cc: e.tensor_tensor(
                        out=oT[:, hl, cc, :],
                        in0=ps[:, 4 + hl // 2, ((hl % 2) * 2 + cc) * 128:((hl % 2) * 2 + cc + 1) * 128],
                        in1=rinv[:, hl * 128:(hl + 1) * 128], op=ALU.mult),
                        [("ps", 4 + hl // 2), "rinv"], ["oT"])
            for hl in range(4):
                h = hg * 4 + hl
                for cc in range(2):
                    P.pe(lambda e, h=h, hl=hl, cc=cc: e.matmul(ps[0:64, 7, hl * 128:(hl + 1) * 128],
                                                               lhsT=wuvb[:, h, cc, :], rhs=oT[:, hl, cc, :],
                                                               start=(cc == 0), stop=(cc == 1)),
                         ["wuvb", "oT"], [("ps", 7)])
            P.act(lambda e, hg=hg: e.copy(out=ovs[:, hg * 4:(hg + 1) * 4, :],
                                          in_=ps[0:64, 7, :].rearrange("p (h q) -> p h q", h=4)),
                  [("ps", 7)], ["ovs"])
        P.dma("sp", ovv[:, :, qs], ovs[:], ["ovs"], [("ov", s)], semkey="store")
    P.finalize()
    return nc, len(P.ops)


def dsa_inputs(xb, j, w_in, kv_norm, w_uk, w_uv):
    SEQ = xb.shape[0]
    blocks = [4 * s + j for s in range(16)]
    xq = np.concatenate([xb[g * 128:(g + 1) * 128] for g in blocks], axis=0)
    o1, o2, o3, o4 = 1024, 1280, 1792, 1856
    cm = np.zeros((128, 512), np.float32)
    kk = np.arange(512)[None, :]
    qi = np.arange(128)[:, None]
    cm[kk > 128 * j + qi] = NEG
    return {
        "xbT": np.ascontiguousarray(xb.T), "xqT": np.ascontiguousarray(xq.T),
        "wq": np.ascontiguousarray(w_in[:, :o1]), "wc": np.ascontiguousarray(w_in[:, o1:o2]),
        "wqi": np.ascontiguousarray(w_in[:, o2:o3]), "wki": np.ascontiguousarray(w_in[:, o3:o4]),
        "wwi": np.ascontiguousarray(w_in[:, o4:]),
        "kvp": np.ascontiguousarray(kv_norm.reshape(2, 128).T),
        "kvb": np.ascontiguousarray(np.broadcast_to(kv_norm[None, :], (128, 256))),
        "wuk": np.ascontiguousarray(w_uk), "wuv": np.ascontiguousarray(w_uv),
        "cmask": cm, "ident": np.eye(128, dtype=np.float32),
    }


RMS_EPS = 1e-6


def build_gla(n_groups=16, SEQ=8192, debug=False):
    nc = bass.Bass("TRN2", target_bir_lowering=False, dynamic_dma_scratch_size=8192)
    dt = lambda n, s: nc.dram_tensor(n, s, F32, kind="ExternalInput").ap()
    xT = dt("xT", [1024, SEQ])
    wq = dt("wq", [1024, 128]); wk = dt("wk", [1024, 128]); wv = dt("wv", [1024, 256])
    wg = dt("wg", [1024, 16]); wr = dt("wr", [1024, 256])
    wg2 = dt("wg2", [16, 128]); gb = dt("gb", [1, 128]); ngb = dt("ngb", [64, 256])
    triA = dt("triA", [64, 64]); triUA = dt("triUA", [64, 64]); triM = dt("triM", [64, 64])
    og = nc.dram_tensor("og", [SEQ, 256], F32, kind="ExternalOutput").ap()

    P = Prog(nc)
    xg = P.sb([128, 2, 8, 512], BF16, "xg")
    wqb = P.sb([128, 8, 128], BF16, "wqb"); wkb = P.sb([128, 8, 128], BF16, "wkb")
    wvb = P.sb([128, 8, 256], BF16, "wvb"); wrb = P.sb([128, 8, 256], BF16, "wrb")
    wgb = P.sb([128, 8, 16], BF16, "wgb")
    wg2s = P.sb([16, 128], F32, "wg2s"); gbs = P.sb([1, 128], F32, "gbs"); ngs = P.sb([64, 256], F32, "ngs")
    triAs = P.sb([64, 64], F32, "triAs"); triUAs = P.sb([64, 64], F32, "triUAs"); triMb = P.sb([64, 64], BF16, "triMb")
    ones1 = P.sb([1, 64], F32, "ones1")
    glr = P.sb([16, 512], F32, "glr")
    e1 = P.sb([64, 8, 128], F32, "e1"); l1 = P.sb([64, 8, 128], F32, "l1")
    ebc = P.sb([128, 512], F32, "ebc"); enb = P.sb([128, 512], F32, "enb")
    eb = P.sb([128, 8], F32, "eb")
    qt = P.sb([128, 512], BF16, "qt"); kt = P.sb([128, 512], BF16, "kt")
    ed2 = P.sb([64, 8, 128], F32, "ed2"); kh = P.sb([64, 8, 128], BF16, "kh")
    vb = P.sb([64, 8, 256], BF16, "vb")
    sig = P.sb([64, 8, 256], F32, "sig"); sr = P.sb([64, 8, 256], F32, "sr")
    Am = P.sb([64, 8, 64], BF16, "Am")
    obuf = P.sb([64, 8, 256], F32, "obuf"); otmp = P.sb([64, 8, 256], F32, "otmp")
    ss = P.sb([64, 8], F32, "ss")
    S = P.sb([128, 256], F32, "S"); Sb = P.sb([128, 256], BF16, "Sb")
    ps = P.ps([128, 8, 512], F32, "ps")

    r3 = lambda a: a.rearrange("(c p) n -> p c n", p=128)
    for dst, src, k in ((wqb, wq, "wqb"), (wkb, wk, "wkb"), (wvb, wv, "wvb"), (wrb, wr, "wrb"), (wgb, wg, "wgb")):
        P.dma("pool", dst[:], r3(src), [], [k])
    P.dma("pool", triMb[:], triM, [], ["triMb"])
    for dst, src, k in ((wg2s, wg2, "wg2s"), (gbs, gb, "gbs"), (ngs, ngb, "ngs"), (triAs, triA, "triAs"),
                        (triUAs, triUA, "triUAs")):
        P.dma("sp", dst[:], src, [], [k])
    P.dve(lambda e: e.memset(ones1[:], 1.0), [], ["ones1"])
    P.dve(lambda e: e.memset(S[:], 0.0), [], ["S"])
    P.dve(lambda e: e.memset(Sb[:], 0.0), [], ["Sb"])

    bankc = [0]

    def nb():
        b = bankc[0] % 6
        bankc[0] += 1
        return b

    xv = xT.rearrange("(c p) t -> p c t", p=128)
    ogv = og.rearrange("(g c p) e -> g p c e", c=8, p=64)
    def group(G):
        sl = G % 2
        P.dma("pool", xg[:, sl], xv[:, :, G * 512:(G + 1) * 512], [], [("xg", sl)])
        X = ("xg", sl)
        bq, bk, bg = nb(), nb(), nb()
        for kc in range(8):
            P.pe(lambda e, kc=kc: e.matmul(ps[:, bq, :], lhsT=wqb[:, kc, :], rhs=xg[:, sl, kc, :],
                                           start=(kc == 0), stop=(kc == 7)), ["wqb", X], [("ps", bq)])
        for kc in range(8):
            P.pe(lambda e, kc=kc: e.matmul(ps[:, bk, :], lhsT=wkb[:, kc, :], rhs=xg[:, sl, kc, :],
                                           start=(kc == 0), stop=(kc == 7)), ["wkb", X], [("ps", bk)])
        for kc in range(8):
            P.pe(lambda e, kc=kc: e.matmul(ps[0:16, bg, :], lhsT=wgb[:, kc, :], rhs=xg[:, sl, kc, :],
                                           start=(kc == 0), stop=(kc == 7)), ["wgb", X], [("ps", bg)])
        P.act(lambda e: e.copy(out=glr[:], in_=ps[0:16, bg, :]), [("ps", bg)], ["glr"])
        for hf in range(2):
            b = nb()
            for c4 in range(4):
                c = hf * 4 + c4
                P.pe(lambda e, c=c, c4=c4, b=b: e.matmul(ps[0:64, b, c4 * 128:(c4 + 1) * 128],
                                                         lhsT=glr[:, c * 64:(c + 1) * 64], rhs=wg2s[:],
                                                         start=True, stop=False), ["glr", "wg2s"], [("ps", b)])
                P.pe(lambda e, c4=c4, b=b: e.matmul(ps[0:64, b, c4 * 128:(c4 + 1) * 128],
                                                    lhsT=ones1[:], rhs=gbs[:], start=False, stop=True),
                     ["ones1", "gbs"], [("ps", b)])
            P.act(lambda e, hf=hf, b=b: e.activation(out=e1[:, hf * 4:(hf + 1) * 4, :],
                                                     in_=ps[0:64, b, :].rearrange("p (c d) -> p c d", c=4),
                                                     func=AF.Exp, scale=-1.0), [("ps", b)], [("e1", hf)])
            P.act(lambda e, hf=hf: e.activation(out=l1[:, hf * 4:(hf + 1) * 4, :], in_=e1[:, hf * 4:(hf + 1) * 4, :],
                                                func=AF.Ln, bias=1.0, scale=1.0), [("e1", hf)], [("l1", hf)])
        bb = nb()
        for c in range(8):
            P.pe(lambda e, c=c: e.matmul(ps[:, bb, c * 64:(c + 1) * 64], lhsT=l1[:, c, :], rhs=triAs[:],
                                         start=True, stop=True), [("l1", c // 4), "triAs"], [("ps", bb)])
        P.act(lambda e: e.activation(out=ebc[:], in_=ps[:, bb, :], func=AF.Exp), [("ps", bb)], ["ebc"])
        P.act(lambda e: e.activation(out=enb[:], in_=ps[:, bb, :], func=AF.Exp, scale=-1.0), [("ps", bb)], ["enb"])
        P.act(lambda e: e.activation(out=eb[:], in_=ps[:, bb, :].rearrange("p (c i) -> p c i", c=8)[:, :, 63],
                                     func=AF.Exp), [("ps", bb)], ["eb"])
        P.dve(lambda e: e.scalar_tensor_tensor(out=qt[:], in0=ps[:, bq, :], scalar=128.0 ** -0.5, op0=ALU.mult,
                                               in1=ebc[:], op1=ALU.mult), [("ps", bq), "ebc"], ["qt"])
        P.dve(lambda e: e.tensor_tensor(out=kt[:], in0=ps[:, bk, :], in1=enb[:], op=ALU.mult),
              [("ps", bk), "enb"], ["kt"])
        for hf in range(2):
            b = nb()
            P.pe(lambda e, hf=hf, b=b: e.matmul(ps[0:64, b, :], lhsT=triUAs[:],
                                                rhs=l1[:, hf * 4:(hf + 1) * 4, :], start=True, stop=True),
                 [("l1", hf), "triUAs"], [("ps", b)])
            P.act(lambda e, hf=hf, b=b: e.activation(out=ed2[:, hf * 4:(hf + 1) * 4, :],
                                                     in_=ps[0:64, b, :].rearrange("p (c d) -> p c d", c=4),
                                                     func=AF.Exp), [("ps", b)], [("ed2", hf)])
            b2 = nb()
            for c4 in range(4):
                c = hf * 4 + c4
                for kc in range(8):
                    P.pe(lambda e, c=c, c4=c4, kc=kc, b2=b2: e.matmul(
                        ps[0:64, b2, c4 * 128:(c4 + 1) * 128], lhsT=xg[:, sl, kc, c * 64:(c + 1) * 64],
                        rhs=wkb[:, kc, :], start=(kc == 0), stop=(kc == 7)), ["wkb", X], [("ps", b2)])
            P.dve(lambda e, hf=hf, b2=b2: e.tensor_tensor(out=kh[:, hf * 4:(hf + 1) * 4, :],
                                                          in0=ps[0:64, b2, :].rearrange("p (c d) -> p c d", c=4),
                                                          in1=ed2[:, hf * 4:(hf + 1) * 4, :], op=ALU.mult),
                  [("ps", b2), ("ed2", hf)], [("kh", hf)])
        for c2 in range(4):
            b = nb()
            for cc in range(2):
                c = c2 * 2 + cc
                for kc in range(8):
                    P.pe(lambda e, c=c, cc=cc, kc=kc, b=b: e.matmul(
                        ps[0:64, b, cc * 256:(cc + 1) * 256], lhsT=xg[:, sl, kc, c * 64:(c + 1) * 64],
                        rhs=wvb[:, kc, :], start=(kc == 0), stop=(kc == 7)), ["wvb", X], [("ps", b)])
            P.act(lambda e, c2=c2, b=b: e.copy(out=vb[:, c2 * 2:(c2 + 1) * 2, :],
                                               in_=ps[0:64, b, :].rearrange("p (c d) -> p c d", c=2)),
                  [("ps", b)], [("vb", c2)])
            b = nb()
            for cc in range(2):
                c = c2 * 2 + cc
                for kc in range(8):
                    P.pe(lambda e, c=c, cc=cc, kc=kc, b=b: e.matmul(
                        ps[0:64, b, cc * 256:(cc + 1) * 256], lhsT=xg[:, sl, kc, c * 64:(c + 1) * 64],
                        rhs=wrb[:, kc, :], start=(kc == 0), stop=(kc == 7)), ["wrb", X], [("ps", b)])
            P.act(lambda e, c2=c2, b=b: e.activation(out=sig[:, c2 * 2:(c2 + 1) * 2, :],
                                                     in_=ps[0:64, b, :].rearrange("p (c d) -> p c d", c=2),
                                                     func=AF.Sigmoid), [("ps", b)], [("sig", c2)])
            P.dve(lambda e, c2=c2, b=b: e.tensor_tensor(out=sr[:, c2 * 2:(c2 + 1) * 2, :],
                                                        in0=ps[0:64, b, :].rearrange("p (c d) -> p c d", c=2),
                                                        in1=sig[:, c2 * 2:(c2 + 1) * 2, :], op=ALU.mult),
                  [("ps", b), ("sig", c2)], [("sr", c2)])
        ba = nb()
        for c in range(8):
            P.pe(lambda e, c=c: e.matmul(ps[0:64, ba, c * 64:(c + 1) * 64], lhsT=kt[:, c * 64:(c + 1) * 64],
                                         rhs=qt[:, c * 64:(c + 1) * 64], start=True, stop=True),
                 ["kt", "qt"], [("ps", ba)])
        P.dve(lambda e: e.tensor_tensor(out=Am[:], in0=ps[0:64, ba, :].rearrange("p (c i) -> p c i", c=8),
                                        in1=triMb[:].unsqueeze(1).broadcast_to([64, 8, 64]), op=ALU.mult),
              [("ps", ba), "triMb"], ["Am"])
        for c in range(8):
            P.pe(lambda e, c=c: e.matmul(ps[0:64, 6, (c % 2) * 256:(c % 2 + 1) * 256], lhsT=Am[:, c, :], rhs=vb[:, c, :],
                                         start=True, stop=False), ["Am", ("vb", c // 2)], [("ps", 6)])
            P.pe(lambda e, c=c: e.matmul(ps[0:64, 6, (c % 2) * 256:(c % 2 + 1) * 256], lhsT=qt[:, c * 64:(c + 1) * 64],
                                         rhs=Sb[:], start=False, stop=True), ["qt", "Sb"], [("ps", 6)])
            P.act(lambda e, c=c: e.copy(out=obuf[:, c, :], in_=ps[0:64, 6, (c % 2) * 256:(c % 2 + 1) * 256]),
                  [("ps", 6)], ["obuf"])
            P.pe(lambda e, c=c: e.matmul(ps[:, 7, 0:256], lhsT=kh[:, c, :], rhs=vb[:, c, :], start=True, stop=True),
                 [("kh", c // 4), ("vb", c // 2)], [("ps", 7)])
            P.dve(lambda e, c=c: e.scalar_tensor_tensor(out=S[:], in0=S[:], scalar=eb[:, c:c + 1], op0=ALU.mult,
                                                        in1=ps[:, 7, 0:256], op1=ALU.add),
                  ["S", "eb", ("ps", 7)], ["S"])
            P.act(lambda e: e.copy(out=Sb[:], in_=S[:]), ["S"], ["Sb"])
        P.dve(lambda e: e.tensor_tensor(out=otmp[:], in0=obuf[:], in1=obuf[:], op=ALU.mult), ["obuf"], ["otmp"])
        P.dve(lambda e: e.tensor_reduce(out=ss[:], in_=otmp[:], op=ALU.add, axis=AX.X), ["otmp"], ["ss"])
        P.dve(lambda e: e.tensor_scalar(out=ss[:], in0=ss[:], scalar1=1.0 / 256, scalar2=RMS_EPS, op0=ALU.mult,
                                        op1=ALU.add), ["ss"], ["ss"])
        P.act(lambda e: e.activation(out=ss[:], in_=ss[:], func=AF.Sqrt), ["ss"], ["ss"])
        P.dve(lambda e: e.reciprocal(out=ss[:], in_=ss[:]), ["ss"], ["ss"])
        P.dve(lambda e: e.tensor_tensor(out=otmp[:], in0=obuf[:], in1=ss[:].unsqueeze(2).broadcast_to([64, 8, 256]),
                                        op=ALU.mult), ["obuf", "ss"], ["otmp"])
        P.dve(lambda e: e.tensor_tensor(out=otmp[:], in0=otmp[:], in1=ngs[:].unsqueeze(1).broadcast_to([64, 8, 256]),
                                        op=ALU.mult), ["otmp", "ngs"], ["otmp"])
        P.dve(lambda e: e.tensor_tensor(out=otmp[:], in0=otmp[:], in1=sr[:], op=ALU.mult),
              ["otmp"] + [("sr", i) for i in range(4)], ["otmp"])
        P.dma("sp", ogv[G], otmp[:], ["otmp"], [("og", G)], semkey="store")
    for G in range(n_groups):
        group(G)
    if debug:
        for nm, t, shp in (("d_l1", l1, [64, 1024]), ("d_ebc", ebc, [128, 512]), ("d_ed2", ed2, [64, 1024]),
                           ("d_obuf", obuf, [64, 2048]), ("d_ss", ss, [64, 8]), ("d_sr", sr, [64, 2048]),
                           ("d_S", S, [128, 256]), ("d_eb", eb, [128, 8]), ("d_glr", glr, [16, 512])):
            d = nc.dram_tensor(nm, shp, F32, kind="ExternalOutput").ap()
            src = t[:]
            if len(t.shape) == 3:
                src = t[:].rearrange("p a b -> p (a b)")
            P.dma("sp", d, src, ["otmp", "S", "Sb", "obuf", "ss"], [nm], semkey="store")
    P.finalize()
    return nc, len(P.ops)


def gla_inputs(x2b, h, w_in, w_g2, g_bias, norm_g):
    KD, VD = 512, 1024
    oq, ok_, ov_, og_, or_ = 0, KD, 2 * KD, 2 * KD + VD, 2 * KD + VD + 16
    j = np.arange(64)
    tri = (j[:, None] <= j[None, :]).astype(np.float32)
    triU = (j[:, None] > j[None, :]).astype(np.float32)
    return {
        "xT": np.ascontiguousarray(x2b.T),
        "wq": np.ascontiguousarray(w_in[:, oq + h * 128:oq + (h + 1) * 128]),
        "wk": np.ascontiguousarray(w_in[:, ok_ + h * 128:ok_ + (h + 1) * 128]),
        "wv": np.ascontiguousarray(w_in[:, ov_ + h * 256:ov_ + (h + 1) * 256]),
        "wg": np.ascontiguousarray(w_in[:, og_:og_ + 16]),
        "wr": np.ascontiguousarray(w_in[:, or_ + h * 256:or_ + (h + 1) * 256]),
        "wg2": np.ascontiguousarray(w_g2[:, h * 128:(h + 1) * 128]),
        "gb": np.ascontiguousarray(g_bias[None, h * 128:(h + 1) * 128]),
        "ngb": np.ascontiguousarray(np.broadcast_to(norm_g[None, h * 256:(h + 1) * 256], (64, 256))),
        "triA": (-tri / 16.0).astype(np.float32), "triUA": (-triU / 16.0).astype(np.float32), "triM": tri,
    }


_CACHE = {}


def _prog(name, fn):
    if name not in _CACHE:
        _CACHE[name] = fn()[0]
    return _CACHE[name]


def kernel(x, a_w_in, a_kv_norm, a_w_uk, a_w_uv, a_w_out,
           b_w_in, b_w_g2, b_g_bias, b_norm, b_w_out,
           m_w_router, m_b_router, m_w1, m_b1, m_w2, m_b2,
           ln1_g, ln1_b, ln2_g, ln2_b):
    f = lambda a: np.ascontiguousarray(np.asarray(a), dtype=np.float32)
    x = f(x)
    cores = list(range(8))
    B, L, D = x.shape
    def own(arr_b, j):
        return np.concatenate([arr_b[(4 * s + j) * 128:(4 * s + j + 1) * 128] for s in range(16)], axis=0)

    def unown(parts):
        out = np.empty((L, parts[0].shape[1]), np.float32)
        for j in range(4):
            for s in range(16):
                out[(4 * s + j) * 128:(4 * s + j + 1) * 128] = parts[j][s * 128:(s + 1) * 128]
        return out

    nc_a = _prog("dsa", build_dsa)
    ins = [dsa_inputs(x[c // 4], c % 4, f(a_w_in[0]), f(a_kv_norm[0]), f(a_w_uk[0]), f(a_w_uv[0])) for c in cores]
    res = run_bass_kernel_spmd(nc_a, ins, core_ids=cores)
    ovT = [res.results[c]["ovT"] for c in cores]
    nc_b = _prog("post", build_post)
    ins = []
    for c in cores:
        xo = own(x[c // 4], c % 4)
        ins.append(post_inputs(xo.T, ovT[c], f(a_w_out[0]), f(ln1_g[0]), f(ln1_b[0]), f(ln2_g[0]), f(ln2_b[0]),
                               f(m_w_router[0]), f(m_b_router[0]), f(m_w1[0]), f(m_b1[0]), f(m_w2[0]), f(m_b2[0])))
    res = run_bass_kernel_spmd(nc_b, ins, core_ids=cores)
    x2 = np.stack([unown([res.results[b * 4 + j]["outT"].T for j in range(4)]) for b in range(B)])
    nc_c = _prog("gla", build_gla)
    ins = [gla_inputs(x2[c // 4], c % 4, f(b_w_in[0]), f(b_w_g2[0]), f(b_g_bias[0]), f(b_norm[0])) for c in cores]
    res = run_bass_kernel_spmd(nc_c, ins, core_ids=cores)
    og = np.stack([np.concatenate([res.results[b * 4 + h]["og"] for h in range(4)], axis=1) for b in range(B)])
    ins = []
    for c in cores:
        xo = own(x2[c // 4], c % 4)
        mo = own(og[c // 4], c % 4)
        ins.append(post_inputs(xo.T, mo.T, f(b_w_out[0]), f(ln1_g[1]), f(ln1_b[1]), f(ln2_g[1]), f(ln2_b[1]),
                               f(m_w_router[1]), f(m_b_router[1]), f(m_w1[1]), f(m_b1[1]), f(m_w2[1]), f(m_b2[1])))
    res = run_bass_kernel_spmd(nc_b, ins, core_ids=cores)
    out = np.stack([unown([res.results[b * 4 + j]["outT"].T for j in range(4)]) for b in range(B)])
    return out.astype(np.float32)
```

```python
import contextlib
import numpy as np
import concourse.bass as bass
import concourse.mybir as mybir
from concourse.bass_utils import run_bass_kernel_spmd

F32 = mybir.dt.float32
BF16 = mybir.dt.bfloat16
ALU = mybir.AluOpType
AF = mybir.ActivationFunctionType
AX = mybir.AxisListType


class Op:
    __slots__ = ("eng", "fn", "reads", "writes", "dma", "stream", "seq",
                 "waits", "signal", "sigval", "clock")


class Prog:
    BIG_BF16 = 110512

    def __init__(self, nc):
        self.nc = nc
        self.ops = []
        self.stack = contextlib.ExitStack()
        self.big = self.stack.enter_context(nc.sbuf_tensor("BIG", [128, self.BIG_BF16], BF16))
        self.psum = self.stack.enter_context(nc.psum_tensor("PS", [128, 8, 512], F32))
        self.off = 0
        self.allkeys = set()
        self.fence_key = None
        self.nfence = 0

    def sb(self, shape, dtype, name=None):
        shape = list(shape)
        n = 1
        for d in shape[1:]:
            n *= d
        n16 = n * (2 if dtype == F32 else 1)
        n16 = (n16 + 15) // 16 * 16
        assert self.off + n16 <= self.BIG_BF16 - 16, (name, self.off, n16)
        v = self.big[0:shape[0], self.off:self.off + n16]
        self.off += n16
        if dtype == F32:
            v = v.bitcast(F32)
            v = v[:, 0:n]
        else:
            v = v[:, 0:n]
        if len(shape) > 2:
            names = ["a", "b", "c", "d"][:len(shape) - 1]
            pat = "p (" + " ".join(names) + ") -> p " + " ".join(names)
            v = v.rearrange(pat, **{k: s for k, s in zip(names, shape[1:])})
        return v

    def reset_alloc(self):
        self.off = 0

    def fence(self):
        self.nfence += 1
        fk = ("fence", self.nfence)
        dummy = self.big[0:1, self.BIG_BF16 - 16:self.BIG_BF16 - 14].bitcast(F32)
        keys = list(self.allkeys)
        self.fence_key = None
        self.add("dve", lambda e: e.memset(dummy, 0.0), [], keys + [fk])
        self.fence_key = fk

    def add(self, eng, fn, reads=(), writes=(), dma=False, semkey=None):
        o = Op()
        o.eng = eng
        o.fn = fn
        reads = tuple(reads)
        if self.fence_key is not None:
            reads = reads + (self.fence_key,)
        o.reads = reads
        o.writes = tuple(writes)
        self.allkeys.update(o.reads)
        self.allkeys.update(o.writes)
        o.dma = dma
        if dma:
            if semkey is None:
                semkey = o.writes[0]
            o.stream = ("dma", semkey)
        else:
            o.stream = eng
        o.waits = []
        o.signal = False
        self.ops.append(o)
        return o

    def pe(self, fn, reads, writes):
        return self.add("pe", fn, reads, writes)

    def act(self, fn, reads, writes):
        return self.add("act", fn, reads, writes)

    def dve(self, fn, reads, writes):
        return self.add("dve", fn, reads, writes)

    def pool(self, fn, reads, writes):
        return self.add("pool", fn, reads, writes)

    def dma(self, q, out, in_, reads, writes, semkey=None, **kw):
        return self.add(q, lambda e: e.dma_start(out=out, in_=in_, **kw),
                        reads, writes, dma=True, semkey=semkey)

    def finalize(self, final_wait_eng="sp"):
        nc = self.nc
        ops = self.ops
        seqc = {}
        last_writer = {}
        readers = {}
        known = {}
        for o in ops:
            seqc[o.stream] = seqc.get(o.stream, 0) + 1
            o.seq = seqc[o.stream]
            deps = []
            for k in o.reads:
                p = last_writer.get(k)
                if p is not None:
                    deps.append((p, "raw"))
            for k in o.writes:
                p = last_writer.get(k)
                if p is not None:
                    deps.append((p, "waw"))
                for p in readers.get(k, {}).values():
                    deps.append((p, "war"))
            kn = known.setdefault(o.eng, {})
            for p, kind in deps:
                if p is o:
                    continue
                if (not p.dma) and (not o.dma) and p.eng == o.eng and kind != "raw":
                    continue
                if (not p.dma) and (not o.dma) and p.eng == o.eng == "pe":
                    continue
                if kn.get(p.stream, 0) >= p.seq:
                    continue
                o.waits.append(p)
                p.signal = True
                for s, v in p.clock.items():
                    if kn.get(s, 0) < v:
                        kn[s] = v
                kn[p.stream] = p.seq
            o.clock = dict(kn)
            for k in o.writes:
                last_writer[k] = o
                readers[k] = {}
            for k in o.reads:
                readers.setdefault(k, {})[o.stream] = o
            if o.dma:
                o.signal = True
        LIMIT = 32000
        sigc = {}
        semids = {}
        for o in ops:
            if o.signal:
                inc = 16 if o.dma else 1
                c = sigc.get(o.stream, 0) + inc
                sigc[o.stream] = c
                gen = (c - 1) // LIMIT
                o.sigval = ((o.stream, gen), c - gen * LIMIT)
                semids[(o.stream, gen)] = c - gen * LIMIT
        sems = {}
        for sid in semids:
            sems[sid] = self.stack.enter_context(nc.semaphore(f"s{len(sems)}"))
        self.n_sems = len(sems)
        finals = [(sems[sid], v) for sid, v in semids.items() if isinstance(sid[0], tuple)]
        by_eng = {}
        for o in ops:
            by_eng.setdefault(o.eng, []).append(o)

        def emit(name, e):
            for o in by_eng.get(name, []):
                for p in o.waits:
                    e.wait_ge(sems[p.sigval[0]], p.sigval[1])
                ins = o.fn(e)
                if o.signal:
                    ins.then_inc(sems[o.sigval[0]], 16 if o.dma else 1)
            if name == final_wait_eng:
                for s, v in finals:
                    e.wait_ge(s, v)

        with nc.Block() as block:
            @block.tensor
            def _(e):
                emit("pe", e)

            @block.scalar
            def _(e):
                emit("act", e)

            @block.vector
            def _(e):
                emit("dve", e)

            @block.gpsimd
            def _(e):
                emit("pool", e)

            @block.sync
            def _(e):
                emit("sp", e)
        self.stack.close()

DN_ALPHA = 4.0 ** 0.25
LN_EPS = 1e-5
NE = 32
NT = 2048


def emit_post(P, T, with_outproj, out_key, x_keys=(), m_keys=(), h_keys=(), zfill=None):
    TT = NT // 512
    xT = T.get("xT"); mT = T.get("mT"); wout = T.get("wout"); hT = T.get("hT")
    lnp = T["lnp"]; wr = T["wr"]; brt = T["brt"]; w1 = T["w1"]; b1T = T["b1T"]; w2 = T["w2"]; b2 = T["b2"]
    ident = T["ident"]; sel = T["sel"]; outT = T["outT"]
    z = P.sb([128, 8, NT], F32, "z")
    zb = P.sb([128, 8, NT], BF16, "zb")
    w1s = P.sb([128, 2, 8, 1024], BF16, "w1s")
    w2s = P.sb([128, 2, 4, 1024], BF16, "w2s")
    aT = P.sb([128, 2, 4, 512], BF16, "aT")
    tmp = P.sb([128, 8, 512], F32, "tmp")
    lnt = P.sb([128, 6, 512], F32, "lnt")
    gT = P.sb([32, NT], BF16, "gT")
    b1s = P.sb([128, NE * 16], F32, "b1s")
    b2b = P.sb([32, 1024], BF16, "b2b")
    lns = P.sb([128, 32], F32, "lns")
    wrs = P.sb([128, 8, 32], F32, "wrs")
    brs = P.sb([128, 32], F32, "brs")
    ids = P.sb([128, 128], F32, "ids")
    sels = P.sb([32, NE * 128], BF16, "sels")
    ones = P.sb([128, 128], F32, "ones")
    rt = P.sb([128, 8, 32], F32, "rt")
    ps = P.psum

    if zfill is None:
        P.dma("sp", z[:], xT.rearrange("(c p) t -> p c t", p=128), list(x_keys), ["z_all"])
    if zfill is not None:
        zfill(P, z, zb)
    elif with_outproj:
        P.dma("pool", zb[:], mT.rearrange("(c p) t -> p c t", p=128), list(m_keys), ["zb_all"])
        P.dma("pool", w1s[:, 0], wout.rearrange("(c p) n -> p c n", p=128), [], [("w1s", 0)])
    else:
        hv = hT.rearrange("(c p) t -> p c t", p=128)
        for t in range(TT):
            ts = slice(t * 512, (t + 1) * 512)
            P.dma("sp", tmp[:], hv[:, :, ts], list(h_keys), [("tmp", c) for c in range(8)])
            for c in range(8):
                P.dve(lambda e, c=c, ts=ts: e.scalar_tensor_tensor(
                    out=z[:, c, ts], in0=z[:, c, ts], scalar=DN_ALPHA, op0=ALU.mult,
                    in1=tmp[:, c, :], op1=ALU.add), ["z_all", ("z", c, t), ("tmp", c)], [("z", c, t)])
    P.dma("sp", lns[:], lnp, [], ["lns"])
    P.dma("sp", wrs[:], wr.rearrange("(c p) n -> p c n", p=128), [], ["wrs"])
    P.dma("sp", brs[:], brt, [], ["brs"])
    P.dma("sp", b1s[:], b1T, [], ["b1s"])
    P.dma("sp", ids[:], ident, [], ["ids"])
    P.dma("pool", b2b[:], b2, [], ["b2b"])
    P.dma("pool", sels[:], sel, [], ["sels"])
    P.dve(lambda e: e.memset(ones[:], 1.0), [], ["ones"])
    b1v = b1s[:].rearrange("p (e c) -> p e c", c=16)
    P.dve(lambda e: e.tensor_scalar(out=b1v[:, :, 8:16], in0=b1v[:, :, 8:16], scalar1=1.0,
                                    scalar2=None, op0=ALU.add), ["b1s"], ["b1s"])

    def zk(c, t):
        return ("z", c, t)

    first_z = [True]

    def zreads(c, t):
        return [zk(c, t), "z_all"]

    if with_outproj:
        for t in range(TT):
            ts = slice(t * 512, (t + 1) * 512)
            for j in range(8):
                bank = (t * 8 + j) % 4
                for kc in range(8):
                    P.pe(lambda e, bank=bank, kc=kc, j=j, ts=ts: e.matmul(
                        ps[:, bank, :], lhsT=w1s[:, 0, kc, j * 128:(j + 1) * 128],
                        rhs=zb[:, kc, ts], start=(kc == 0), stop=(kc == 7)),
                        [("w1s", 0), "zb_all"], [("ps", bank)])
                P.dve(lambda e, bank=bank, j=j, ts=ts: e.scalar_tensor_tensor(
                    out=z[:, j, ts], in0=z[:, j, ts], scalar=DN_ALPHA, op0=ALU.mult,
                    in1=ps[:, bank, :], op1=ALU.add),
                    zreads(j, t) + [("ps", bank)], [zk(j, t)])

    def layer_norm(goff, boff, make_bf16):
        for t in range(TT):
            ts = slice(t * 512, (t + 1) * 512)
            for c in range(8):
                P.act(lambda e, c=c, ts=ts: e.activation(out=tmp[:, c, :], in_=z[:, c, ts], func=AF.Square),
                      zreads(c, t), [("tmp", c)])
            for c in range(8):
                P.pe(lambda e, c=c, ts=ts: e.matmul(ps[:, 6, :], lhsT=ones[:], rhs=z[:, c, ts],
                                                    start=(c == 0), stop=(c == 7)),
                     zreads(c, t) + ["ones"], [("ps", 6)])
            for c in range(8):
                P.pe(lambda e, c=c: e.matmul(ps[:, 7, :], lhsT=ones[:], rhs=tmp[:, c, :],
                                             start=(c == 0), stop=(c == 7)),
                     [("tmp", c), "ones"], [("ps", 7)])
            mean, m2, var, rstd, mr = (lnt[:, i, :] for i in range(5))
            P.dve(lambda e: e.tensor_scalar(out=mean, in0=ps[:, 6, :], scalar1=1.0 / 1024, scalar2=None,
                                            op0=ALU.mult), [("ps", 6)], [("lnt", 0)])
            P.dve(lambda e: e.tensor_tensor(out=m2, in0=mean, in1=mean, op=ALU.mult),
                  [("lnt", 0)], [("lnt", 1)])
            P.dve(lambda e: e.scalar_tensor_tensor(out=var, in0=ps[:, 7, :], scalar=1.0 / 1024, op0=ALU.mult,
                                                   in1=m2, op1=ALU.subtract),
                  [("ps", 7), ("lnt", 1)], [("lnt", 2)])
            P.dve(lambda e: e.tensor_scalar(out=var, in0=var, scalar1=LN_EPS, scalar2=None, op0=ALU.add),
                  [("lnt", 2)], [("lnt", 2)])
            P.act(lambda e: e.activation(out=rstd, in_=var, func=AF.Sqrt),
                  [("lnt", 2)], [("lnt", 3)])
            P.dve(lambda e: e.reciprocal(out=rstd, in_=rstd), [("lnt", 3)], [("lnt", 3)])
            P.dve(lambda e: e.tensor_tensor(out=mr, in0=mean, in1=rstd, op=ALU.mult),
                  [("lnt", 0), ("lnt", 3)], [("lnt", 4)])
            for c in range(8):
                P.dve(lambda e, c=c, ts=ts: e.tensor_tensor(out=tmp[:, c, :], in0=z[:, c, ts], in1=rstd, op=ALU.mult),
                      zreads(c, t) + [("lnt", 3)], [("tmp", c)])
                P.dve(lambda e, c=c: e.tensor_tensor(out=tmp[:, c, :], in0=tmp[:, c, :], in1=mr, op=ALU.subtract),
                      [("tmp", c), ("lnt", 4)], [("tmp", c)])
                P.act(lambda e, c=c, ts=ts: e.activation(out=z[:, c, ts], in_=tmp[:, c, :], func=AF.Identity,
                                                         scale=lns[:, goff + c:goff + c + 1],
                                                         bias=lns[:, boff + c:boff + c + 1]),
                      [("tmp", c), "lns", "z_all"], [zk(c, t)])
                if make_bf16:
                    P.pool(lambda e, c=c, ts=ts: e.tensor_copy(out=zb[:, c, ts], in_=z[:, c, ts]),
                           [zk(c, t), "zb_all"], [("zb", t)])

    layer_norm(0, 8, True)

    for tt in range(NT // 128):
        tsl = slice(tt * 128, (tt + 1) * 128)
        t = tt // 4
        for c in range(8):
            P.pe(lambda e, c=c, tsl=tsl: e.matmul(ps[:, 7, 0:32], lhsT=z[:, c, tsl], rhs=wrs[:, c, :],
                                                  start=(c == 0), stop=(c == 7)),
                 [zk(c, t), "wrs"], [("ps", 7)])
        lg, m8, negm, ex, em, ssum, gt = (rt[:, i, :] for i in range(7))
        P.dve(lambda e: e.tensor_tensor(out=lg, in0=ps[:, 7, 0:32], in1=brs[:], op=ALU.add),
              [("ps", 7), "brs"], ["rt0"])
        P.dve(lambda e: e.max(out=m8[:, 0:8], in_=lg), ["rt0"], ["rt1"])
        P.dve(lambda e: e.tensor_scalar(out=negm[:, 0:1], in0=m8[:, 0:1], scalar1=-1.0, scalar2=None, op0=ALU.mult),
              ["rt1"], ["rt2"])
        P.act(lambda e: e.activation(out=ex, in_=lg, func=AF.Exp, bias=negm[:, 0:1], scale=1.0),
              ["rt0", "rt2"], ["rt3"])
        P.dve(lambda e: e.scalar_tensor_tensor(out=em, in0=lg, scalar=m8[:, 3:4], op0=ALU.is_ge,
                                               in1=ex, op1=ALU.mult, accum_out=ssum[:, 0:1]),
              ["rt0", "rt1", "rt3"], ["rt4", "rt5"])
        P.dve(lambda e: e.reciprocal(out=ssum[:, 1:2], in_=ssum[:, 0:1]), ["rt5"], ["rt5b"])
        P.dve(lambda e: e.tensor_scalar(out=gt, in0=em, scalar1=ssum[:, 1:2], scalar2=None, op0=ALU.mult),
              ["rt4", "rt5b"], ["rt6"])
        P.pe(lambda e: e.transpose(ps[0:32, 6, 0:128], gt, ids[:]), ["rt6", "ids"], [("ps", 6)])
        P.act(lambda e, tsl=tsl: e.copy(out=gT[:, tsl], in_=ps[0:32, 6, 0:128]), [("ps", 6)], ["gT"])

    for t in range(TT):
        ts = slice(t * 512, (t + 1) * 512)
        for j in range(8):
            bank = 4 + (j % 2)
            P.pe(lambda e, bank=bank, j=j, ts=ts: e.matmul(ps[:, bank, :], lhsT=b2b[:, j * 128:(j + 1) * 128],
                                                          rhs=gT[:, ts], start=True, stop=True),
                 ["b2b", "gT"], [("ps", bank)])
            P.dve(lambda e, bank=bank, j=j, ts=ts: e.scalar_tensor_tensor(
                out=z[:, j, ts], in0=z[:, j, ts], scalar=DN_ALPHA, op0=ALU.mult,
                in1=ps[:, bank, :], op1=ALU.add),
                [zk(j, t), ("ps", bank)], [zk(j, t)])

    w1v = w1.rearrange("e (kc p) n -> e p kc n", p=128)
    w2v = w2.rearrange("e (fc p) d -> e p fc d", p=128)
    units = [(ex_, h) for ex_ in range(NE) for h in range(2)]

    def load_unit(u):
        ex_, h = units[u]
        s = u % 2
        P.dma("pool", w1s[:, s, :, 0:512], w1v[ex_, :, :, h * 512:(h + 1) * 512], [], [("w1s", s)])
        P.dma("pool", w1s[:, s, :, 512:1024], w1v[ex_, :, :, 1024 + h * 512:1024 + (h + 1) * 512], [], [("w1s", s)])
        P.dma("pool", w2s[:, s], w2v[ex_, :, h * 4:(h + 1) * 4, :], [], [("w2s", s)])

    cnt = [0]

    def emit_gu(u, t):
        ex_, h = units[u]
        s = u % 2
        it = cnt[0]
        cnt[0] += 1
        ab = it % 2
        ts = slice(t * 512, (t + 1) * 512)
        P.pe(lambda e: e.matmul(ps[:, 6, :], lhsT=sels[:, ex_ * 128:(ex_ + 1) * 128], rhs=gT[:, ts],
                                start=True, stop=True), ["sels", "gT"], [("ps", 6)])
        for fl in range(4):
            pb = 2 * ((it * 4 + fl) % 2)
            tb = 4 * ((it * 4 + fl) % 2)
            for kc in range(8):
                P.pe(lambda e, kc=kc, fl=fl, pb=pb: e.matmul(
                    ps[:, pb, :], lhsT=w1s[:, s, kc, fl * 128:(fl + 1) * 128], rhs=zb[:, kc, ts],
                    start=(kc == 0), stop=(kc == 7)), [("w1s", s), ("zb", t)], [("ps", pb)])
            for kc in range(8):
                P.pe(lambda e, kc=kc, fl=fl, pb=pb: e.matmul(
                    ps[:, pb + 1, :], lhsT=w1s[:, s, kc, 512 + fl * 128:512 + (fl + 1) * 128], rhs=zb[:, kc, ts],
                    start=(kc == 0), stop=(kc == 7)), [("w1s", s), ("zb", t)], [("ps", pb + 1)])
            fch = h * 4 + fl
            bg = b1s[:, ex_ * 16 + fch:ex_ * 16 + fch + 1]
            bu = b1s[:, ex_ * 16 + 8 + fch:ex_ * 16 + 8 + fch + 1]
            g, sg, glu, u1 = (tmp[:, tb + i, :] for i in range(4))
            P.dve(lambda e, g=g, pb=pb, bg=bg: e.tensor_scalar(out=g, in0=ps[:, pb, :], scalar1=bg, scalar2=7.0,
                                                               op0=ALU.add, op1=ALU.min),
                  [("ps", pb), "b1s"], [("tmp", tb)])
            P.act(lambda e, g=g, sg=sg: e.activation(out=sg, in_=g, func=AF.Sigmoid, scale=1.702),
                  [("tmp", tb)], [("tmp", tb + 1)])
            P.dve(lambda e, u1=u1, pb=pb, bu=bu: e.tensor_scalar(out=u1, in0=ps[:, pb + 1, :], scalar1=bu, scalar2=-6.0,
                                                                 op0=ALU.add, op1=ALU.max),
                  [("ps", pb + 1), "b1s"], [("tmp", tb + 3)])
            P.dve(lambda e, g=g, sg=sg, glu=glu: e.tensor_tensor(out=glu, in0=g, in1=sg, op=ALU.mult),
                  [("tmp", tb), ("tmp", tb + 1)], [("tmp", tb + 2)])
            P.dve(lambda e, u1=u1, glu=glu: e.scalar_tensor_tensor(out=u1, in0=u1, scalar=8.0, op0=ALU.min,
                                                                  in1=glu, op1=ALU.mult),
                  [("tmp", tb + 3), ("tmp", tb + 2)], [("tmp", tb + 3)])
            P.dve(lambda e, u1=u1, fl=fl: e.tensor_tensor(out=aT[:, ab, fl, :], in0=u1, in1=ps[:, 6, :], op=ALU.mult),
                  [("tmp", tb + 3), ("ps", 6)], [("aT", ab)])
        return (u, t, ab)

    def emit_y(info):
        u, t, ab = info
        s = u % 2
        ts = slice(t * 512, (t + 1) * 512)
        for j in range(8):
            bank = 4 + (j % 2)
            for fl in range(4):
                P.pe(lambda e, fl=fl, j=j, bank=bank: e.matmul(
                    ps[:, bank, :], lhsT=w2s[:, s, fl, j * 128:(j + 1) * 128], rhs=aT[:, ab, fl, :],
                    start=(fl == 0), stop=(fl == 3)), [("w2s", s), ("aT", ab)], [("ps", bank)])
            P.dve(lambda e, j=j, bank=bank: e.tensor_tensor(out=z[:, j, ts], in0=z[:, j, ts], in1=ps[:, bank, :],
                                                            op=ALU.add),
                  [zk(j, t), ("ps", bank)], [zk(j, t)])

    load_unit(0)
    prev = None
    for u in range(len(units)):
        for t in range(TT):
            info = emit_gu(u, t)
            if prev is not None:
                emit_y(prev)
            prev = info
            if t == 0 and u + 1 < len(units):
                load_unit(u + 1)
    emit_y(prev)

    layer_norm(16, 24, False)
    ov = outT.rearrange("(c p) t -> p c t", p=128)
    for t in range(TT):
        ts = slice(t * 512, (t + 1) * 512)
        P.dma("sp", ov[:, :, ts], z[:, :, ts], [zk(c, t) for c in range(8)], [(out_key, t)], semkey=("store", out_key))

RMS_EPS = 1e-6
NEG = -1.0e30
NBIS = 24


def emit_dsa(P, T, n_blocks=64, SEQ=8192):
    NKT = SEQ // 512
    xbT = T["xbT"]; wq = T["wq"]; wc = T["wc"]; wqi = T["wqi"]; wki = T["wki"]; wwi = T["wwi"]
    kvp = T["kvp"]; kvb = T["kvb"]; wuk = T["wuk"]; wuv = T["wuv"]; cmask = T["cmask"]; ident = T["ident"]
    ovT = T["ovT"]
    kiT = P.sb([64, SEQ], BF16, "kiT")
    cT = P.sb([128, 2, SEQ], BF16, "cT")
    C = P.sb([128, SEQ // 128, 256], BF16, "C")
    sc = P.sb([128, SEQ], F32, "sc")
    junk = P.sb([128, 1024], BF16, "junk")
    maskT = P.sb([128, SEQ // 128, 128], BF16, "maskT")
    wqb = P.sb([128, 8, 1024], BF16, "wqb")
    wqib = P.sb([128, 8, 512], BF16, "wqib")
    wcb = P.sb([128, 8, 256], BF16, "wcb")
    wkib = P.sb([128, 8, 64], BF16, "wkib")
    wwib = P.sb([128, 8, 8], BF16, "wwib")
    wukb = P.sb([64, 16, 256], BF16, "wukb")
    wuvb = P.sb([128, 16, 2, 64], BF16, "wuvb")
    kvps = P.sb([128, 2], F32, "kvps"); kvbs = P.sb([128, 256], F32, "kvbs")
    cms = P.sb([128, 4, 512], BF16, "cms")
    idb = P.sb([128, 128], BF16, "idb")
    onesf = P.sb([128, 128], F32, "onesf"); onesb = P.sb([128, 128], BF16, "onesb")
    half = P.sb([128, 1], F32, "half")
    R = P.sb([128, 14336], BF16, "R")
    xkb = R[:, 0:8192].rearrange("p (s c t) -> p s c t", s=2, c=8)
    cpre = R[:, 8192:10240].bitcast(F32).rearrange("p (c t) -> p c t", c=2)
    sq = R[:, 10240:12288].bitcast(F32).rearrange("p (c t) -> p c t", c=2)
    rinvk = R[:, 12288:13312].bitcast(F32)
    ktmp = R[:, 13312:13824].bitcast(F32)
    ksm = P.sb([128, 4], F32, "ksm")
    qT = R[0:64, 0:2048].rearrange("p (h q) -> p h q", h=16)
    qiT = R[0:64, 2048:3072].rearrange("p (h q) -> p h q", h=8)
    qlT = R[:, 3072:7168].rearrange("p (c h q) -> p c h q", c=2, h=16)
    rbuf = R[:, 7168:8704].rearrange("p (r t) -> p r t", r=3)
    pe_ = R[:, 8704:9728].rearrange("p (r t) -> p r t", r=2)
    pm = R[:, 9728:10752].rearrange("p (r t) -> p r t", r=2)
    oT = R[:, 10752:11776].rearrange("p (h c q) -> p h c q", h=4, c=2)
    rinv = R[:, 11776:12800].bitcast(F32)
    xqb = P.sb([128, 8, 128], BF16, "xqb")
    wis = P.sb([128, 8], F32, "wis")
    ovs = P.sb([64, 16, 128], F32, "ovs")
    fdum = P.sb([128, 1], F32, "fdum")
    bs = P.sb([128, 8], F32, "bs")
    cnt4 = P.sb([128, 8], F32, "cnt4")
    ps = P.psum
    psb = ps[:].bitcast(BF16)

    r3 = lambda a: a.rearrange("(c p) n -> p c n", p=128)
    P.dma("pool", wcb[:], r3(wc), [], ["wcb"])
    P.dma("pool", wkib[:], r3(wki), [], ["wkib"])
    P.dma("pool", wqb[:], r3(wq), [], ["wqb"])
    P.dma("pool", wqib[:], r3(wqi), [], ["wqib"])
    P.dma("pool", wwib[:], r3(wwi), [], ["wwib"])
    P.dma("pool", wukb[:], wuk.rearrange("h d c -> d h c"), [], ["wukb"])
    P.dma("pool", wuvb[:], wuv.rearrange("h (cc p) v -> p h cc v", p=128), [], ["wuvb"])
    P.dma("pool", idb[:], ident, [], ["idb"])
    P.dma("sp", kvps[:], kvp, [], ["kvps"])
    P.dma("sp", kvbs[:], kvb, [], ["kvbs"])
    P.dma("pool", cms[:], cmask.rearrange("p (j k) -> p j k", j=4), [], ["cms"])
    P.dve(lambda e: e.memset(onesf[:], 1.0), [], ["onesf"])
    P.dve(lambda e: e.memset(onesb[:], 1.0), [], ["onesb"])
    P.dve(lambda e: e.memset(half[:], 0.5), [], ["half"])
    zerosb = P.sb([128, 128], BF16, "zerosb")
    P.dve(lambda e: e.memset(zerosb[:], 0.0), [], ["zerosb"])

    xbv = xbT.rearrange("(c p) t -> p c t", p=128)
    for kt in range(NKT):
        sl = kt % 2
        ks = slice(kt * 512, (kt + 1) * 512)
        P.dma("pool", xkb[:, sl], xbv[:, :, ks], [], [("xkb", sl)])
        for kc in range(8):
            P.pe(lambda e, kc=kc, sl=sl: e.matmul(ps[0:64, 0, :], lhsT=wkib[:, kc, :], rhs=xkb[:, sl, kc, :],
                                                  start=(kc == 0), stop=(kc == 7)),
                 ["wkib", ("xkb", sl)], [("ps", 0)])
        P.act(lambda e, ks=ks: e.copy(out=kiT[:, ks], in_=ps[0:64, 0, :]), [("ps", 0)], ["kiT"])
        for cc in range(2):
            for kc in range(8):
                P.pe(lambda e, kc=kc, sl=sl, cc=cc: e.matmul(ps[:, 1 + cc, :], lhsT=wcb[:, kc, cc * 128:(cc + 1) * 128],
                                                            rhs=xkb[:, sl, kc, :], start=(kc == 0), stop=(kc == 7)),
                     ["wcb", ("xkb", sl)], [("ps", 1 + cc)])
            P.act(lambda e, cc=cc: e.copy(out=cpre[:, cc, :], in_=ps[:, 1 + cc, :]), [("ps", 1 + cc)], [("cpre", cc)])
            P.act(lambda e, cc=cc: e.activation(out=sq[:, cc, :], in_=ps[:, 1 + cc, :], func=AF.Square),
                  [("ps", 1 + cc)], [("sq", cc)])
        for cc in range(2):
            P.pe(lambda e, cc=cc: e.matmul(ps[:, 3, :], lhsT=onesf[:], rhs=sq[:, cc, :], start=(cc == 0), stop=(cc == 1)),
                 ["onesf", ("sq", cc)], [("ps", 3)])
        P.dve(lambda e: e.tensor_scalar(out=rinvk, in0=ps[:, 3, :], scalar1=1.0 / 256, scalar2=RMS_EPS,
                                        op0=ALU.mult, op1=ALU.add), [("ps", 3)], ["rinvk"])
        P.act(lambda e: e.activation(out=rinvk, in_=rinvk, func=AF.Sqrt), ["rinvk"], ["rinvk"])
        P.dve(lambda e: e.reciprocal(out=rinvk, in_=rinvk), ["rinvk"], ["rinvk"])
        for cc in range(2):
            P.dve(lambda e, cc=cc, ks=ks: e.scalar_tensor_tensor(out=cT[:, cc, ks], in0=cpre[:, cc, :],
                                                                 scalar=kvps[:, cc:cc + 1], op0=ALU.mult,
                                                                 in1=rinvk, op1=ALU.mult),
                  [("cpre", cc), "kvps", "rinvk"], ["cT"])
        for k4 in range(4):
            kb = kt * 4 + k4
            bank = 4 + (kb % 2)
            for kc in range(8):
                P.pe(lambda e, kc=kc, sl=sl, k4=k4, bank=bank: e.matmul(
                    ps[:, bank, 0:256], lhsT=xkb[:, sl, kc, k4 * 128:(k4 + 1) * 128], rhs=wcb[:, kc, :],
                    start=(kc == 0), stop=(kc == 7)), ["wcb", ("xkb", sl)], [("ps", bank)])
            P.act(lambda e, bank=bank: e.activation(out=ktmp, in_=ps[:, bank, 0:256], func=AF.Square,
                                                    accum_out=ksm[:, 0:1]), [("ps", bank)], ["ktmp", "ksm0"])
            P.dve(lambda e: e.tensor_scalar(out=ksm[:, 1:2], in0=ksm[:, 0:1], scalar1=1.0 / 256, scalar2=RMS_EPS,
                                            op0=ALU.mult, op1=ALU.add), ["ksm0"], ["ksm1"])
            P.act(lambda e: e.activation(out=ksm[:, 2:3], in_=ksm[:, 1:2], func=AF.Sqrt), ["ksm1"], ["ksm2"])
            P.dve(lambda e: e.reciprocal(out=ksm[:, 3:4], in_=ksm[:, 2:3]), ["ksm2"], ["ksm3"])
            P.dve(lambda e, kb=kb, bank=bank: e.scalar_tensor_tensor(out=C[:, kb, :], in0=ps[:, bank, 0:256],
                                                                    scalar=ksm[:, 3:4], op0=ALU.mult,
                                                                    in1=kvbs[:], op1=ALU.mult),
                  [("ps", bank), "ksm3", "kvbs"], ["C"])

    kkeys = [("xkb", 0), ("xkb", 1), ("cpre", 0), ("cpre", 1), ("sq", 0), ("sq", 1), "rinvk", "ktmp"]
    qkeys = ["qT", "qiT", "qlT", ("rbuf", 0), ("rbuf", 1), ("rbuf", 2), ("pe", 0), ("pe", 1),
             ("pm", 0), ("pm", 1), "oT", "rinv"]
    P.dve(lambda e: e.memset(fdum[:], 0.0), [], kkeys + qkeys + ["fdum"])
    xqv = xbv
    ovv = ovT.rearrange("(h p) t -> p h t", p=64)
    lo, hi, mid, cnt, ge, d1 = (bs[:, i:i + 1] for i in range(6))
    for g in range(n_blocks):
        s = g // 4
        j = g % 4
        qs = slice(g * 128, (g + 1) * 128)
        nk = 512 * (s + 1)
        nkb = nk // 128
        P.dma("pool", xqb[:], xqv[:, :, qs], [], ["xqb"])
        for h in range(16):
            bank = h % 2
            for kc in range(8):
                P.pe(lambda e, h=h, kc=kc, bank=bank: e.matmul(ps[0:64, bank, 0:128], lhsT=wqb[:, kc, h * 64:(h + 1) * 64],
                                                                rhs=xqb[:, kc, :], start=(kc == 0), stop=(kc == 7)),
                     ["wqb", "xqb"], [("ps", bank)])
            P.act(lambda e, h=h, bank=bank: e.copy(out=qT[:, h, :], in_=ps[0:64, bank, 0:128]), [("ps", bank)], ["qT"])
        for h in range(8):
            bank = h % 2
            for kc in range(8):
                P.pe(lambda e, h=h, kc=kc, bank=bank: e.matmul(ps[0:64, bank, 0:128], lhsT=wqib[:, kc, h * 64:(h + 1) * 64],
                                                                rhs=xqb[:, kc, :], start=(kc == 0), stop=(kc == 7)),
                     ["wqib", "xqb"], [("ps", bank)])
            P.act(lambda e, h=h, bank=bank: e.copy(out=qiT[:, h, :], in_=ps[0:64, bank, 0:128]), [("ps", bank)], ["qiT"])
        for kc in range(8):
            P.pe(lambda e, kc=kc: e.matmul(ps[:, 2, 0:8], lhsT=xqb[:, kc, :], rhs=wwib[:, kc, :],
                                           start=(kc == 0), stop=(kc == 7)), ["wwib", "xqb"], [("ps", 2)])
        P.act(lambda e: e.copy(out=wis[:], in_=ps[:, 2, 0:8]), [("ps", 2)], ["wis"])
        for cc in range(2):
            for hg in range(4):
                bank = 4 + ((cc * 4 + hg) % 2)
                for hl in range(4):
                    h = hg * 4 + hl
                    P.pe(lambda e, h=h, hl=hl, cc=cc, bank=bank: e.matmul(
                        ps[:, bank, hl * 128:(hl + 1) * 128], lhsT=wukb[:, h, cc * 128:(cc + 1) * 128], rhs=qT[:, h, :],
                        start=True, stop=True), ["wukb", "qT"], [("ps", bank)])
                P.act(lambda e, cc=cc, hg=hg, bank=bank: e.activation(
                    out=qlT[:, cc, hg * 4:(hg + 1) * 4, :], in_=ps[:, bank, :].rearrange("p (h q) -> p h q", h=4),
                    func=AF.Copy, scale=0.125), [("ps", bank)], ["qlT"])
        it = 0
        for kt in range(s + 1):
            ks = slice(kt * 512, (kt + 1) * 512)
            for h in range(8):
                bank = it % 2
                rb = it % 3
                it += 1
                P.pe(lambda e, h=h, ks=ks, bank=bank: e.matmul(ps[:, bank, :], lhsT=qiT[:, h, :], rhs=kiT[:, ks],
                                                               start=True, stop=True), ["qiT", "kiT"], [("ps", bank)])
                P.act(lambda e, bank=bank, rb=rb: e.activation(out=rbuf[:, rb, :], in_=ps[:, bank, :], func=AF.Relu),
                      [("ps", bank)], [("rbuf", rb)])
                if h == 0:
                    P.dve(lambda e, rb=rb, ks=ks: e.tensor_scalar(out=sc[:, ks], in0=rbuf[:, rb, :], scalar1=wis[:, 0:1],
                                                                  scalar2=None, op0=ALU.mult),
                          [("rbuf", rb), "wis"], [("sc", kt)])
                else:
                    P.dve(lambda e, rb=rb, ks=ks, h=h: e.scalar_tensor_tensor(
                        out=sc[:, ks], in0=rbuf[:, rb, :], scalar=wis[:, h:h + 1], op0=ALU.mult,
                        in1=sc[:, ks], op1=ALU.add), [("rbuf", rb), "wis", ("sc", kt)], [("sc", kt)])
        sck = [("sc", kt) for kt in range(s + 1)]
        P.dve(lambda e, nk=nk: e.tensor_reduce(out=lo, in_=sc[:, 0:nk], op=ALU.min, axis=AX.X), sck, ["lo"])
        P.dve(lambda e: e.tensor_scalar(out=lo, in0=lo, scalar1=-1.0, scalar2=None, op0=ALU.add), ["lo"], ["lo"])
        P.dve(lambda e, nk=nk, j=j: e.tensor_tensor(out=sc[:, nk - 512:nk], in0=sc[:, nk - 512:nk], in1=cms[:, j, :], op=ALU.add),
              [("sc", s), "cms"], [("sc", s)])
        P.dve(lambda e, nk=nk: e.tensor_reduce(out=hi, in_=sc[:, 0:nk], op=ALU.max, axis=AX.X), sck, ["hi"])
        nch = (nk + 1023) // 1024
        for itb in range(NBIS):
            P.dve(lambda e: e.scalar_tensor_tensor(out=mid, in0=lo, scalar=hi, op0=ALU.add, in1=half[:], op1=ALU.mult),
                  ["lo", "hi", "half"], ["mid"])
            for ch in range(nch):
                c0 = ch * 1024
                c1 = min(nk, c0 + 1024)
                P.dve(lambda e, c0=c0, c1=c1, ch=ch: e.tensor_scalar(out=junk[:, 0:c1 - c0], in0=sc[:, c0:c1], scalar1=mid,
                                                                     scalar2=None, op0=ALU.is_ge, op1=ALU.add,
                                                                     accum_out=cnt4[:, ch:ch + 1]),
                      sck + ["mid"], ["junk", ("cnt4", ch)])
            if nch == 1:
                P.dve(lambda e: e.tensor_copy(out=cnt, in_=cnt4[:, 0:1]), [("cnt4", 0)], ["cnt"])
            else:
                P.dve(lambda e, nch=nch: e.tensor_reduce(out=cnt, in_=cnt4[:, 0:nch], op=ALU.add, axis=AX.X),
                      [("cnt4", i) for i in range(nch)], ["cnt"])
            P.dve(lambda e: e.tensor_scalar(out=ge, in0=cnt, scalar1=256.0, scalar2=None, op0=ALU.is_ge),
                  ["cnt"], ["ge"])
            P.dve(lambda e: e.tensor_tensor(out=d1, in0=mid, in1=lo, op=ALU.subtract), ["mid", "lo"], ["d1"])
            P.dve(lambda e: e.scalar_tensor_tensor(out=lo, in0=d1, scalar=ge, op0=ALU.mult, in1=lo, op1=ALU.add),
                  ["d1", "ge", "lo"], ["lo"])
            P.dve(lambda e: e.tensor_tensor(out=d1, in0=hi, in1=mid, op=ALU.subtract), ["mid", "hi"], ["d1"])
            P.dve(lambda e: e.scalar_tensor_tensor(out=hi, in0=d1, scalar=ge, op0=ALU.mult, in1=mid, op1=ALU.add),
                  ["d1", "ge", "mid"], ["hi"])
        for ch in range(nch):
            c0 = ch * 1024
            c1 = min(nk, c0 + 1024)
            P.dve(lambda e, c0=c0, c1=c1: e.tensor_scalar(out=junk[:, 0:c1 - c0], in0=sc[:, c0:c1], scalar1=lo,
                                                          scalar2=None, op0=ALU.is_ge), sck + ["lo"], ["junk"])
            for g4 in range((c1 - c0) // 512):
                bank = 2 + (g4 % 2)
                for k4 in range(4):
                    col = g4 * 512 + k4 * 128
                    P.pe(lambda e, col=col, k4=k4, bank=bank: e.transpose(
                        psb[:, bank, k4 * 128:(k4 + 1) * 128], junk[:, col:col + 128], idb[:]),
                        ["junk", "idb"], [("ps", bank)])
                kb0 = (c0 + g4 * 512) // 128
                P.act(lambda e, kb0=kb0, bank=bank: e.copy(
                    out=maskT[:, kb0:kb0 + 4, :], in_=psb[:, bank, 0:512].rearrange("p (k q) -> p k q", k=4)),
                    [("ps", bank)], ["maskT"])
        step = 0
        for hg in range(4):
            for kb in range(nkb):
                sb_ = step % 2
                step += 1
                for cc in range(2):
                    P.pe(lambda e, cc=cc, kb=kb, sb_=sb_, hg=hg: e.matmul(
                        ps[:, sb_, :], lhsT=cT[:, cc, kb * 128:(kb + 1) * 128],
                        rhs=qlT[:, cc, hg * 4:(hg + 1) * 4, :], start=(cc == 0), stop=(cc == 1)),
                        ["cT", "qlT"], [("ps", sb_)])
                P.act(lambda e, sb_=sb_: e.activation(out=pe_[:, sb_, :], in_=ps[:, sb_, :], func=AF.Exp),
                      [("ps", sb_)], [("pe", sb_)])
                P.dve(lambda e, sb_=sb_, kb=kb: e.tensor_tensor(
                    out=pm[:, sb_, :].rearrange("p (h q) -> p h q", h=4),
                    in0=pe_[:, sb_, :].rearrange("p (h q) -> p h q", h=4),
                    in1=maskT[:, kb, :].unsqueeze(1).broadcast_to([128, 4, 128]), op=ALU.mult),
                    [("pe", sb_), "maskT"], [("pm", sb_)])
                if kb == 0:
                    for b2 in range(2):
                        P.pe(lambda e, b2=b2, sb_=sb_: e.matmul(ps[:, 4 + b2, :], lhsT=zerosb[:], rhs=pm[:, sb_, :],
                                                              start=True, stop=False),
                             ["zerosb", ("pm", sb_)], [("ps", 4 + b2)])
                for hl in range(4):
                    for cc in range(2):
                        P.pe(lambda e, hl=hl, cc=cc, kb=kb, sb_=sb_, nkb_=nkb: e.matmul(
                            ps[:, 4 + hl // 2, ((hl % 2) * 2 + cc) * 128:((hl % 2) * 2 + cc + 1) * 128],
                            lhsT=C[:, kb, cc * 128:(cc + 1) * 128], rhs=pm[:, sb_, hl * 128:(hl + 1) * 128],
                            start=False, stop=(kb == nkb_ - 1)), ["C", ("pm", sb_)], [("ps", 4 + hl // 2)])
                P.pe(lambda e, kb=kb, sb_=sb_, nkb_=nkb: e.matmul(ps[:, 6, :], lhsT=onesb[:], rhs=pm[:, sb_, :],
                                                        start=(kb == 0), stop=(kb == nkb_ - 1)),
                     ["onesb", ("pm", sb_)], [("ps", 6)])
            P.dve(lambda e: e.reciprocal(out=rinv, in_=ps[:, 6, :]), [("ps", 6)], ["rinv"])
            for hl in range(4):
                for cc in range(2):
                    P.dve(lambda e, hl=hl, cc=cc: e.tensor_tensor(
                        out=oT[:, hl, cc, :],
                        in0=ps[:, 4 + hl // 2, ((hl % 2) * 2 + cc) * 128:((hl % 2) * 2 + cc + 1) * 128],
                        in1=rinv[:, hl * 128:(hl + 1) * 128], op=ALU.mult),
                        [("ps", 4 + hl // 2), "rinv"], ["oT"])
            for hl in range(4):
                h = hg * 4 + hl
                for cc in range(2):
                    P.pe(lambda e, h=h, hl=hl, cc=cc: e.matmul(ps[0:64, 7, hl * 128:(hl + 1) * 128],
                                                               lhsT=wuvb[:, h, cc, :], rhs=oT[:, hl, cc, :],
                                                               start=(cc == 0), stop=(cc == 1)),
                         ["wuvb", "oT"], [("ps", 7)])
            P.act(lambda e, hg=hg: e.copy(out=ovs[:, hg * 4:(hg + 1) * 4, :],
                                          in_=ps[0:64, 7, :].rearrange("p (h q) -> p h q", h=4)),
                  [("ps", 7)], ["ovs"])
        P.dma("sp", ovv[:, :, qs], ovs[:], ["ovs"], [("ov", g)], semkey="store_ov")


def emit_gla(P, T, h, n_groups=16, SEQ=8192):
    x2g = T["x2full"]; hpart = T["hpart"][h]; ident = T["ident"]; wo1 = T["wo1"][h * 256:(h + 1) * 256, :]
    wq = T["gwq"][:, h * 128:(h + 1) * 128]; wk = T["gwk"][:, h * 128:(h + 1) * 128]
    wv = T["gwv"][:, h * 256:(h + 1) * 256]; wg = T["gwg"]; wr = T["gwr"][:, h * 256:(h + 1) * 256]
    wg2 = T["wg2"][:, h * 128:(h + 1) * 128]; gb = T["gb"][:, h * 128:(h + 1) * 128]
    ngb = T["ngb"][:, h * 256:(h + 1) * 256]; triA = T["triA"]; triUA = T["triUA"]; triM = T["triM"]
    xg = P.sb([128, 2, 8, 512], BF16, "xg")
    wqb = P.sb([128, 8, 128], BF16, "wqb"); wkb = P.sb([128, 8, 128], BF16, "wkb")
    wvb = P.sb([128, 8, 256], BF16, "wvb"); wrb = P.sb([128, 8, 256], BF16, "wrb")
    wgb = P.sb([128, 8, 16], BF16, "wgb")
    wg2s = P.sb([16, 128], F32, "wg2s"); gbs = P.sb([1, 128], F32, "gbs"); ngs = P.sb([64, 256], F32, "ngs")
    triAs = P.sb([64, 64], F32, "triAs"); triUAs = P.sb([64, 64], F32, "triUAs"); triMb = P.sb([64, 64], BF16, "triMb")
    ones1 = P.sb([1, 64], F32, "ones1")
    glr = P.sb([16, 512], F32, "glr")
    e1 = P.sb([64, 8, 128], F32, "e1"); l1 = P.sb([64, 8, 128], F32, "l1")
    ebc = P.sb([128, 512], F32, "ebc"); enb = P.sb([128, 512], F32, "enb")
    eb = P.sb([128, 8], F32, "eb")
    qt = P.sb([128, 512], BF16, "qt"); kt = P.sb([128, 512], BF16, "kt")
    ed2 = P.sb([64, 8, 128], F32, "ed2"); kh = P.sb([64, 8, 128], BF16, "kh")
    vb = P.sb([64, 8, 256], BF16, "vb")
    sig = P.sb([64, 8, 256], F32, "sig"); sr = P.sb([64, 8, 256], F32, "sr")
    Am = P.sb([64, 8, 64], BF16, "Am")
    obuf = P.sb([64, 8, 256], F32, "obuf"); otmp = P.sb([64, 8, 256], F32, "otmp")
    ss = P.sb([64, 8], F32, "ss")
    S = P.sb([128, 256], F32, "S"); Sb = P.sb([128, 256], BF16, "Sb")
    ps = P.psum
    idf = P.sb([128, 128], F32, "idf")
    wo1b = P.sb([128, 2, 1024], BF16, "wo1b")
    ogT = P.sb([128, 2, 512], BF16, "ogT")
    hp = P.sb([128, 8, 512], F32, "hp")

    r3 = lambda a: a.rearrange("(c p) n -> p c n", p=128)
    for dst, src, k in ((wqb, wq, "wqb"), (wkb, wk, "wkb"), (wvb, wv, "wvb"), (wrb, wr, "wrb"), (wgb, wg, "wgb")):
        P.dma("pool", dst[:], r3(src), [], [k])
    P.dma("pool", triMb[:], triM, [], ["triMb"])
    P.dma("pool", wo1b[:], wo1.rearrange("(ec p) d -> p ec d", p=128), [], ["wo1b"])
    P.dma("sp", idf[:], ident, [], ["idf"])
    for dst, src, k in ((wg2s, wg2, "wg2s"), (gbs, gb, "gbs"), (ngs, ngb, "ngs"), (triAs, triA, "triAs"),
                        (triUAs, triUA, "triUAs")):
        P.dma("sp", dst[:], src, [], [k])
    P.dve(lambda e: e.memset(ones1[:], 1.0), [], ["ones1"])
    P.dve(lambda e: e.memset(S[:], 0.0), [], ["S"])
    P.dve(lambda e: e.memset(Sb[:], 0.0), [], ["Sb"])

    bankc = [0]

    def nb():
        b = bankc[0] % 6
        bankc[0] += 1
        return b

    xv = x2g.rearrange("(c p) t -> p c t", p=128)
    hpv = hpart.rearrange("(c p) t -> p c t", p=128)
    def group(G):
        sl = G % 2
        P.dma("pool", xg[:, sl], xv[:, :, G * 512:(G + 1) * 512], [(("x2f", G // 4), G % 4)], [("xg", sl)])
        X = ("xg", sl)
        bq, bk, bg = nb(), nb(), nb()
        for kc in range(8):
            P.pe(lambda e, kc=kc: e.matmul(ps[:, bq, :], lhsT=wqb[:, kc, :], rhs=xg[:, sl, kc, :],
                                           start=(kc == 0), stop=(kc == 7)), ["wqb", X], [("ps", bq)])
        for kc in range(8):
            P.pe(lambda e, kc=kc: e.matmul(ps[:, bk, :], lhsT=wkb[:, kc, :], rhs=xg[:, sl, kc, :],
                                           start=(kc == 0), stop=(kc == 7)), ["wkb", X], [("ps", bk)])
        for kc in range(8):
            P.pe(lambda e, kc=kc: e.matmul(ps[0:16, bg, :], lhsT=wgb[:, kc, :], rhs=xg[:, sl, kc, :],
                                           start=(kc == 0), stop=(kc == 7)), ["wgb", X], [("ps", bg)])
        P.act(lambda e: e.copy(out=glr[:], in_=ps[0:16, bg, :]), [("ps", bg)], ["glr"])
        for hf in range(2):
            b = nb()
            for c4 in range(4):
                c = hf * 4 + c4
                P.pe(lambda e, c=c, c4=c4, b=b: e.matmul(ps[0:64, b, c4 * 128:(c4 + 1) * 128],
                                                         lhsT=glr[:, c * 64:(c + 1) * 64], rhs=wg2s[:],
                                                         start=True, stop=False), ["glr", "wg2s"], [("ps", b)])
                P.pe(lambda e, c4=c4, b=b: e.matmul(ps[0:64, b, c4 * 128:(c4 + 1) * 128],
                                                    lhsT=ones1[:], rhs=gbs[:], start=False, stop=True),
                     ["ones1", "gbs"], [("ps", b)])
            P.act(lambda e, hf=hf, b=b: e.activation(out=e1[:, hf * 4:(hf + 1) * 4, :],
                                                     in_=ps[0:64, b, :].rearrange("p (c d) -> p c d", c=4),
                                                     func=AF.Exp, scale=-1.0), [("ps", b)], [("e1", hf)])
            P.act(lambda e, hf=hf: e.activation(out=l1[:, hf * 4:(hf + 1) * 4, :], in_=e1[:, hf * 4:(hf + 1) * 4, :],
                                                func=AF.Ln, bias=1.0, scale=1.0), [("e1", hf)], [("l1", hf)])
        bb = nb()
        for c in range(8):
            P.pe(lambda e, c=c: e.matmul(ps[:, bb, c * 64:(c + 1) * 64], lhsT=l1[:, c, :], rhs=triAs[:],
                                         start=True, stop=True), [("l1", c // 4), "triAs"], [("ps", bb)])
        P.act(lambda e: e.activation(out=ebc[:], in_=ps[:, bb, :], func=AF.Exp), [("ps", bb)], ["ebc"])
        P.act(lambda e: e.activation(out=enb[:], in_=ps[:, bb, :], func=AF.Exp, scale=-1.0), [("ps", bb)], ["enb"])
        P.act(lambda e: e.activation(out=eb[:], in_=ps[:, bb, :].rearrange("p (c i) -> p c i", c=8)[:, :, 63],
                                     func=AF.Exp), [("ps", bb)], ["eb"])
        P.dve(lambda e: e.scalar_tensor_tensor(out=qt[:], in0=ps[:, bq, :], scalar=128.0 ** -0.5, op0=ALU.mult,
                                               in1=ebc[:], op1=ALU.mult), [("ps", bq), "ebc"], ["qt"])
        P.dve(lambda e: e.tensor_tensor(out=kt[:], in0=ps[:, bk, :], in1=enb[:], op=ALU.mult),
              [("ps", bk), "enb"], ["kt"])
        for hf in range(2):
            b = nb()
            P.pe(lambda e, hf=hf, b=b: e.matmul(ps[0:64, b, :], lhsT=triUAs[:],
                                                rhs=l1[:, hf * 4:(hf + 1) * 4, :], start=True, stop=True),
                 [("l1", hf), "triUAs"], [("ps", b)])
            P.act(lambda e, hf=hf, b=b: e.activation(out=ed2[:, hf * 4:(hf + 1) * 4, :],
                                                     in_=ps[0:64, b, :].rearrange("p (c d) -> p c d", c=4),
                                                     func=AF.Exp), [("ps", b)], [("ed2", hf)])
            b2 = nb()
            for c4 in range(4):
                c = hf * 4 + c4
                for kc in range(8):
                    P.pe(lambda e, c=c, c4=c4, kc=kc, b2=b2: e.matmul(
                        ps[0:64, b2, c4 * 128:(c4 + 1) * 128], lhsT=xg[:, sl, kc, c * 64:(c + 1) * 64],
                        rhs=wkb[:, kc, :], start=(kc == 0), stop=(kc == 7)), ["wkb", X], [("ps", b2)])
            P.dve(lambda e, hf=hf, b2=b2: e.tensor_tensor(out=kh[:, hf * 4:(hf + 1) * 4, :],
                                                          in0=ps[0:64, b2, :].rearrange("p (c d) -> p c d", c=4),
                                                          in1=ed2[:, hf * 4:(hf + 1) * 4, :], op=ALU.mult),
                  [("ps", b2), ("ed2", hf)], [("kh", hf)])
        for c2 in range(4):
            b = nb()
            for cc in range(2):
                c = c2 * 2 + cc
                for kc in range(8):
                    P.pe(lambda e, c=c, cc=cc, kc=kc, b=b: e.matmul(
                        ps[0:64, b, cc * 256:(cc + 1) * 256], lhsT=xg[:, sl, kc, c * 64:(c + 1) * 64],
                        rhs=wvb[:, kc, :], start=(kc == 0), stop=(kc == 7)), ["wvb", X], [("ps", b)])
            P.act(lambda e, c2=c2, b=b: e.copy(out=vb[:, c2 * 2:(c2 + 1) * 2, :],
                                               in_=ps[0:64, b, :].rearrange("p (c d) -> p c d", c=2)),
                  [("ps", b)], [("vb", c2)])
            b = nb()
            for cc in range(2):
                c = c2 * 2 + cc
                for kc in range(8):
                    P.pe(lambda e, c=c, cc=cc, kc=kc, b=b: e.matmul(
                        ps[0:64, b, cc * 256:(cc + 1) * 256], lhsT=xg[:, sl, kc, c * 64:(c + 1) * 64],
                        rhs=wrb[:, kc, :], start=(kc == 0), stop=(kc == 7)), ["wrb", X], [("ps", b)])
            P.act(lambda e, c2=c2, b=b: e.activation(out=sig[:, c2 * 2:(c2 + 1) * 2, :],
                                                     in_=ps[0:64, b, :].rearrange("p (c d) -> p c d", c=2),
                                                     func=AF.Sigmoid), [("ps", b)], [("sig", c2)])
            P.dve(lambda e, c2=c2, b=b: e.tensor_tensor(out=sr[:, c2 * 2:(c2 + 1) * 2, :],
                                                        in0=ps[0:64, b, :].rearrange("p (c d) -> p c d", c=2),
                                                        in1=sig[:, c2 * 2:(c2 + 1) * 2, :], op=ALU.mult),
                  [("ps", b), ("sig", c2)], [("sr", c2)])
        ba = nb()
        for c in range(8):
            P.pe(lambda e, c=c: e.matmul(ps[0:64, ba, c * 64:(c + 1) * 64], lhsT=kt[:, c * 64:(c + 1) * 64],
                                         rhs=qt[:, c * 64:(c + 1) * 64], start=True, stop=True),
                 ["kt", "qt"], [("ps", ba)])
        P.dve(lambda e: e.tensor_tensor(out=Am[:], in0=ps[0:64, ba, :].rearrange("p (c i) -> p c i", c=8),
                                        in1=triMb[:].unsqueeze(1).broadcast_to([64, 8, 64]), op=ALU.mult),
              [("ps", ba), "triMb"], ["Am"])
        for c in range(8):
            P.pe(lambda e, c=c: e.matmul(ps[0:64, 6, (c % 2) * 256:(c % 2 + 1) * 256], lhsT=Am[:, c, :], rhs=vb[:, c, :],
                                         start=True, stop=False), ["Am", ("vb", c // 2)], [("ps", 6)])
            P.pe(lambda e, c=c: e.matmul(ps[0:64, 6, (c % 2) * 256:(c % 2 + 1) * 256], lhsT=qt[:, c * 64:(c + 1) * 64],
                                         rhs=Sb[:], start=False, stop=True), ["qt", "Sb"], [("ps", 6)])
            P.act(lambda e, c=c: e.copy(out=obuf[:, c, :], in_=ps[0:64, 6, (c % 2) * 256:(c % 2 + 1) * 256]),
                  [("ps", 6)], ["obuf"])
            P.pe(lambda e, c=c: e.matmul(ps[:, 7, 0:256], lhsT=kh[:, c, :], rhs=vb[:, c, :], start=True, stop=True),
                 [("kh", c // 4), ("vb", c // 2)], [("ps", 7)])
            P.dve(lambda e, c=c: e.scalar_tensor_tensor(out=S[:], in0=S[:], scalar=eb[:, c:c + 1], op0=ALU.mult,
                                                        in1=ps[:, 7, 0:256], op1=ALU.add),
                  ["S", "eb", ("ps", 7)], ["S"])
            P.act(lambda e: e.copy(out=Sb[:], in_=S[:]), ["S"], ["Sb"])
        P.dve(lambda e: e.tensor_tensor(out=otmp[:], in0=obuf[:], in1=obuf[:], op=ALU.mult), ["obuf"], ["otmp"])
        P.dve(lambda e: e.tensor_reduce(out=ss[:], in_=otmp[:], op=ALU.add, axis=AX.X), ["otmp"], ["ss"])
        P.dve(lambda e: e.tensor_scalar(out=ss[:], in0=ss[:], scalar1=1.0 / 256, scalar2=RMS_EPS, op0=ALU.mult,
                                        op1=ALU.add), ["ss"], ["ss"])
        P.act(lambda e: e.activation(out=ss[:], in_=ss[:], func=AF.Sqrt), ["ss"], ["ss"])
        P.dve(lambda e: e.reciprocal(out=ss[:], in_=ss[:]), ["ss"], ["ss"])
        P.dve(lambda e: e.tensor_tensor(out=otmp[:], in0=obuf[:], in1=ss[:].unsqueeze(2).broadcast_to([64, 8, 256]),
                                        op=ALU.mult), ["obuf", "ss"], ["otmp"])
        P.dve(lambda e: e.tensor_tensor(out=otmp[:], in0=otmp[:], in1=ngs[:].unsqueeze(1).broadcast_to([64, 8, 256]),
                                        op=ALU.mult), ["otmp", "ngs"], ["otmp"])
        P.dve(lambda e: e.tensor_tensor(out=otmp[:], in0=otmp[:], in1=sr[:], op=ALU.mult),
              ["otmp"] + [("sr", i) for i in range(4)], ["otmp"])
        for ec in range(2):
            b = nb()
            for c in range(8):
                P.pe(lambda e, c=c, ec=ec, b=b: e.transpose(ps[:, b, c * 64:(c + 1) * 64],
                                                            otmp[:, c, ec * 128:(ec + 1) * 128], idf[0:64, 0:64]),
                     ["otmp", "idf"], [("ps", b)])
            P.act(lambda e, ec=ec, b=b: e.copy(out=ogT[:, ec, :], in_=ps[:, b, :]), [("ps", b)], [("ogT", ec)])
        for dc in range(8):
            b = nb()
            for ec in range(2):
                P.pe(lambda e, dc=dc, ec=ec, b=b: e.matmul(ps[:, b, :], lhsT=wo1b[:, ec, dc * 128:(dc + 1) * 128],
                                                          rhs=ogT[:, ec, :], start=(ec == 0), stop=(ec == 1)),
                     ["wo1b", ("ogT", ec)], [("ps", b)])
            if dc % 2 == 0:
                P.act(lambda e, dc=dc, b=b: e.copy(out=hp[:, dc, :], in_=ps[:, b, :]), [("ps", b)], [("hp", dc)])
            else:
                P.dve(lambda e, dc=dc, b=b: e.tensor_copy(out=hp[:, dc, :], in_=ps[:, b, :]), [("ps", b)], [("hp", dc)])
        P.dma("sp", hpv[:, :, G * 512:(G + 1) * 512], hp[:],
              [("hp", dc) for dc in range(8)], [("hpart", h, G)], semkey="store_hp")
    for G in range(n_groups):
        group(G)


def build_fused():
    nc = bass.Bass("TRN2", target_bir_lowering=False, dynamic_dma_scratch_size=8192)
    T = {}

    def din(n, s):
        T[n] = nc.dram_tensor(n, list(s), F32, kind="ExternalInput").ap()

    for n, s in (("xbT", [1024, 8192]), ("wq", [1024, 1024]), ("wc", [1024, 256]),
                 ("wqi", [1024, 512]), ("wki", [1024, 64]), ("wwi", [1024, 8]), ("kvp", [128, 2]),
                 ("kvb", [128, 256]), ("wuk", [16, 64, 256]), ("wuv", [16, 256, 64]), ("cmask", [128, 2048]),
                 ("ident", [128, 128]), ("sel", [32, NE * 128]), ("msel", [128, 4]),
                 ("wout0", [1024, 1024]), ("wo1", [1024, 1024]),
                 ("gwq", [1024, 512]), ("gwk", [1024, 512]), ("gwv", [1024, 1024]), ("gwg", [1024, 16]),
                 ("gwr", [1024, 1024]), ("wg2", [16, 512]), ("gb", [1, 512]), ("ngb", [64, 1024]),
                 ("triA", [64, 64]), ("triUA", [64, 64]), ("triM", [64, 64])):
        din(n, s)
    for L in range(2):
        for n, s in (("lnp", [128, 32]), ("wr", [1024, 32]), ("brt", [128, 32]), ("w1", [NE, 1024, 2048]),
                     ("b1T", [128, NE * 16]), ("w2", [NE, 1024, 1024]), ("b2", [NE, 1024])):
            din(f"{n}{L}", s)
    outT = nc.dram_tensor("outT", [1024, 2048], F32, kind="ExternalOutput").ap()
    ovT = nc.dram_tensor("ovT_i", [1024, 8192], F32).ap()
    x2full = nc.dram_tensor("x2full", [1024, 8192], F32).ap()
    hpart = [nc.dram_tensor(f"hpart{h}", [1024, 8192], F32).ap() for h in range(4)]

    P = Prog(nc)
    emit_dsa(P, dict(T, ovT=ovT))
    for p in range(4):
        P.fence()
        P.reset_alloc()
        cs = slice(p * 2048, (p + 1) * 2048)
        T0 = dict(xT=T["xbT"][:, cs], mT=ovT[:, cs], wout=T["wout0"], lnp=T["lnp0"], wr=T["wr0"], brt=T["brt0"],
                  w1=T["w10"], b1T=T["b1T0"], w2=T["w20"], b2=T["b20"], ident=T["ident"], sel=T["sel"],
                  outT=x2full[:, cs])
        emit_post(P, T0, True, ("x2f", p), m_keys=[("ov", g) for g in range(16 * p, 16 * p + 16)])
    for h in range(4):
        P.fence()
        P.reset_alloc()
        emit_gla(P, dict(T, x2full=x2full, hpart=hpart), h)
    P.fence()
    P.reset_alloc()
    x2v = x2full.rearrange("(c p) t -> p c t", p=128)
    hv = [hp_.rearrange("(c p) t -> p c t", p=128) for hp_ in hpart]
    msel = T["msel"]

    def zfill(P, z, zb):
        msl = P.sb([128, 8], F32, "msl")
        zbf = zb.rearrange("p c t -> p (c t)")
        selx = zbf[:, 0:4096].bitcast(F32).rearrange("p (b c q) -> p b c q", b=2, c=8)
        selh = zbf[:, 4096:12288].bitcast(F32).rearrange("p (h c q) -> p h c q", h=4, c=8)
        P.dma("sp", msl[:, 0:4], msel, [], ["msl0"])
        P.dve(lambda e: e.tensor_scalar(out=msl[:, 4:8], in0=msl[:, 0:4], scalar1=DN_ALPHA, scalar2=None,
                                        op0=ALU.mult), ["msl0"], ["msl"])
        it = 0
        for s in range(16):
            zs = z[:, :, s * 128:(s + 1) * 128]
            for r in range(4):
                g = 4 * s + r
                b = it % 2
                it += 1
                cols = slice(g * 128, (g + 1) * 128)
                P.dma("sp", selx[:, b], x2v[:, :, cols], [(("x2f", g // 16), (g % 16) // 4)], [("selx", b)])
                for h in range(4):
                    P.dma("sp", selh[:, h], hv[h][:, :, cols], [("hpart", h, g // 4)], [("selh", h)])
                if r == 0:
                    P.dve(lambda e, zs=zs, b=b, r=r: e.tensor_scalar(out=zs, in0=selx[:, b], scalar1=msl[:, 4 + r:5 + r],
                                                                     scalar2=None, op0=ALU.mult),
                          [("selx", b), "msl"], ["z_all"])
                else:
                    P.dve(lambda e, zs=zs, b=b, r=r: e.scalar_tensor_tensor(out=zs, in0=selx[:, b],
                                                                            scalar=msl[:, 4 + r:5 + r], op0=ALU.mult,
                                                                            in1=zs, op1=ALU.add),
                          [("selx", b), "msl", "z_all"], ["z_all"])
                for h in range(4):
                    P.dve(lambda e, zs=zs, b=b, r=r, h=h: e.scalar_tensor_tensor(out=zs, in0=selh[:, h],
                                                                                 scalar=msl[:, r:r + 1], op0=ALU.mult,
                                                                                 in1=zs, op1=ALU.add),
                          [("selh", h), "msl", "msl0", "z_all"], ["z_all"])

    T1 = dict(lnp=T["lnp1"], wr=T["wr1"], brt=T["brt1"], w1=T["w11"], b1T=T["b1T1"],
              w2=T["w21"], b2=T["b21"], ident=T["ident"], sel=T["sel"], outT=outT)
    emit_post(P, T1, False, "out", zfill=zfill)
    P.finalize()
    return nc, len(P.ops)


_CACHE = {}


def kernel(x, a_w_in, a_kv_norm, a_w_uk, a_w_uv, a_w_out,
           b_w_in, b_w_g2, b_g_bias, b_norm, b_w_out,
           m_w_router, m_b_router, m_w1, m_b1, m_w2, m_b2,
           ln1_g, ln1_b, ln2_g, ln2_b):
    f = lambda a: np.ascontiguousarray(np.asarray(a), dtype=np.float32)
    x = f(x)
    B, L, D = x.shape
    cores = list(range(8))
    if "nc" not in _CACHE:
        _CACHE["nc"] = build_fused()[0]
    nc = _CACHE["nc"]
    w_in = f(a_w_in[0]); kvn = f(a_kv_norm[0])
    o1, o2, o3, o4 = 1024, 1280, 1792, 1856
    shared = {
        "wq": np.ascontiguousarray(w_in[:, :o1]), "wc": np.ascontiguousarray(w_in[:, o1:o2]),
        "wqi": np.ascontiguousarray(w_in[:, o2:o3]), "wki": np.ascontiguousarray(w_in[:, o3:o4]),
        "wwi": np.ascontiguousarray(w_in[:, o4:]),
        "kvp": np.ascontiguousarray(kvn.reshape(2, 128).T),
        "kvb": np.ascontiguousarray(np.broadcast_to(kvn[None, :], (128, 256))),
        "wuk": f(a_w_uk[0]), "wuv": f(a_w_uv[0]),
        "ident": np.eye(128, dtype=np.float32),
        "wout0": f(a_w_out[0]), "wo1": f(b_w_out[0]),
    }
    cm = np.zeros((128, 4, 512), np.float32)
    for j in range(4):
        cm[:, j, :][np.arange(512)[None, :] > 128 * j + np.arange(128)[:, None]] = NEG
    shared["cmask"] = cm.reshape(128, 2048)
    sel = np.zeros((32, NE, 128), np.float32)
    for e in range(NE):
        sel[e, e, :] = 1.0
    shared["sel"] = sel.reshape(32, NE * 128)
    pc = lambda v: np.ascontiguousarray(f(v).reshape(8, 128).T)
    for Lr in range(2):
        shared[f"lnp{Lr}"] = np.concatenate([pc(ln1_g[Lr]), pc(ln1_b[Lr]), pc(ln2_g[Lr]), pc(ln2_b[Lr])], axis=1)
        shared[f"wr{Lr}"] = f(m_w_router[Lr])
        shared[f"brt{Lr}"] = np.ascontiguousarray(np.broadcast_to(f(m_b_router[Lr])[None, :], (128, 32)))
        shared[f"w1{Lr}"] = f(m_w1[Lr])
        shared[f"b1T{Lr}"] = np.ascontiguousarray(f(m_b1[Lr]).reshape(NE, 16, 128).transpose(2, 0, 1).reshape(128, NE * 16))
        shared[f"w2{Lr}"] = f(m_w2[Lr])
        shared[f"b2{Lr}"] = f(m_b2[Lr])
    jj = np.arange(64)
    tri = (jj[:, None] <= jj[None, :]).astype(np.float32)
    triU = (jj[:, None] > jj[None, :]).astype(np.float32)
    shared["triA"] = (-tri / 16.0).astype(np.float32)
    shared["triUA"] = (-triU / 16.0).astype(np.float32)
    shared["triM"] = tri
    bw = f(b_w_in[0])
    shared["gwq"] = np.ascontiguousarray(bw[:, 0:512]); shared["gwk"] = np.ascontiguousarray(bw[:, 512:1024])
    shared["gwv"] = np.ascontiguousarray(bw[:, 1024:2048]); shared["gwg"] = np.ascontiguousarray(bw[:, 2048:2064])
    shared["gwr"] = np.ascontiguousarray(bw[:, 2064:3088])
    shared["wg2"] = f(b_w_g2[0]); shared["gb"] = f(b_g_bias[0])[None, :]
    shared["ngb"] = np.ascontiguousarray(np.broadcast_to(f(b_norm[0])[None, :], (64, 1024)))
    xbT = [np.ascontiguousarray(x[b].T) for b in range(B)]
    in_maps = []
    for c in cores:
        b, j = c // 4, c % 4
        ms = np.zeros((128, 4), np.float32)
        ms[:, j] = 1.0
        m = dict(shared)
        m.update({"xbT": xbT[b], "msel": ms})
        in_maps.append(m)
    res = run_bass_kernel_spmd(nc, in_maps, core_ids=cores)
    out = np.empty((B, L, D), np.float32)
    for c in cores:
        b, j = c // 4, c % 4
        o = res.results[c]["outT"].T
        for s in range(16):
            out[b, (4 * s + j) * 128:(4 * s + j + 1) * 128] = o[s * 128:(s + 1) * 128]
    return out
```

```python
import contextlib
import numpy as np
import concourse.bass as bass
import concourse.mybir as mybir
from concourse.bass_utils import run_bass_kernel_spmd

F32 = mybir.dt.float32
BF16 = mybir.dt.bfloat16
ALU = mybir.AluOpType
AF = mybir.ActivationFunctionType
AX = mybir.AxisListType


class Op:
    __slots__ = ("eng", "fn", "reads", "writes", "dma", "stream", "seq",
                 "waits", "signal", "sigval", "clock")


class Prog:
    BIG_BF16 = 110512

    def __init__(self, nc):
        self.nc = nc
        self.ops = []
        self.stack = contextlib.ExitStack()
        self.big = self.stack.enter_context(nc.sbuf_tensor("BIG", [128, self.BIG_BF16], BF16))
        self.psum = self.stack.enter_context(nc.psum_tensor("PS", [128, 8, 512], F32))
        self.off = 0
        self.allkeys = set()
        self.fence_key = None
        self.nfence = 0

    def sb(self, shape, dtype, name=None):
        shape = list(shape)
        n = 1
        for d in shape[1:]:
            n *= d
        n16 = n * (2 if dtype == F32 else 1)
        n16 = (n16 + 15) // 16 * 16
        assert self.off + n16 <= self.BIG_BF16 - 16, (name, self.off, n16)
        v = self.big[0:shape[0], self.off:self.off + n16]
        self.off += n16
        if dtype == F32:
            v = v.bitcast(F32)
            v = v[:, 0:n]
        else:
            v = v[:, 0:n]
        if len(shape) > 2:
            names = ["a", "b", "c", "d"][:len(shape) - 1]
            pat = "p (" + " ".join(names) + ") -> p " + " ".join(names)
            v = v.rearrange(pat, **{k: s for k, s in zip(names, shape[1:])})
        return v

    def reset_alloc(self):
        self.off = 0

    def fence(self):
        self.nfence += 1
        fk = ("fence", self.nfence)
        dummy = self.big[0:1, self.BIG_BF16 - 16:self.BIG_BF16 - 14].bitcast(F32)
        keys = list(self.allkeys)
        self.fence_key = None
        self.add("dve", lambda e: e.memset(dummy, 0.0), [], keys + [fk])
        self.fence_key = fk

    def add(self, eng, fn, reads=(), writes=(), dma=False, semkey=None):
        o = Op()
        o.eng = eng
        o.fn = fn
        reads = tuple(reads)
        if self.fence_key is not None:
            reads = reads + (self.fence_key,)
        o.reads = reads
        o.writes = tuple(writes)
        self.allkeys.update(o.reads)
        self.allkeys.update(o.writes)
        o.dma = dma
        if dma:
            if semkey is None:
                semkey = o.writes[0]
            o.stream = ("dma", semkey)
        else:
            o.stream = eng
        o.waits = []
        o.signal = False
        self.ops.append(o)
        return o

    def pe(self, fn, reads, writes):
        return self.add("pe", fn, reads, writes)

    def act(self, fn, reads, writes):
        return self.add("act", fn, reads, writes)

    def dve(self, fn, reads, writes):
        return self.add("dve", fn, reads, writes)

    def pool(self, fn, reads, writes):
        return self.add("pool", fn, reads, writes)

    def dma(self, q, out, in_, reads, writes, semkey=None, **kw):
        return self.add(q, lambda e: e.dma_start(out=out, in_=in_, **kw),
                        reads, writes, dma=True, semkey=semkey)

    def finalize(self, final_wait_eng="sp"):
        nc = self.nc
        ops = self.ops
        seqc = {}
        last_writer = {}
        readers = {}
        known = {}
        for o in ops:
            seqc[o.stream] = seqc.get(o.stream, 0) + 1
            o.seq = seqc[o.stream]
            deps = []
            for k in o.reads:
                p = last_writer.get(k)
                if p is not None:
                    deps.append((p, "raw"))
            for k in o.writes:
                p = last_writer.get(k)
                if p is not None:
                    deps.append((p, "waw"))
                for p in readers.get(k, {}).values():
                    deps.append((p, "war"))
            kn = known.setdefault(o.eng, {})
            for p, kind in deps:
                if p is o:
                    continue
                if (not p.dma) and (not o.dma) and p.eng == o.eng and kind != "raw":
                    continue
                if (not p.dma) and (not o.dma) and p.eng == o.eng == "pe":
                    continue
                if kn.get(p.stream, 0) >= p.seq:
                    continue
                o.waits.append(p)
                p.signal = True
                for s, v in p.clock.items():
                    if kn.get(s, 0) < v:
                        kn[s] = v
                kn[p.stream] = p.seq
            o.clock = dict(kn)
            for k in o.writes:
                last_writer[k] = o
                readers[k] = {}
            for k in o.reads:
                readers.setdefault(k, {})[o.stream] = o
            if o.dma:
                o.signal = True
        LIMIT = 32000
        sigc = {}
        semids = {}
        for o in ops:
            if o.signal:
                inc = 16 if o.dma else 1
                c = sigc.get(o.stream, 0) + inc
                sigc[o.stream] = c
                gen = (c - 1) // LIMIT
                o.sigval = ((o.stream, gen), c - gen * LIMIT)
                semids[(o.stream, gen)] = c - gen * LIMIT
        sems = {}
        for sid in semids:
            sems[sid] = self.stack.enter_context(nc.semaphore(f"s{len(sems)}"))
        self.n_sems = len(sems)
        finals = [(sems[sid], v) for sid, v in semids.items() if isinstance(sid[0], tuple)]
        by_eng = {}
        for o in ops:
            by_eng.setdefault(o.eng, []).append(o)

        def emit(name, e):
            for o in by_eng.get(name, []):
                for p in o.waits:
                    e.wait_ge(sems[p.sigval[0]], p.sigval[1])
                ins = o.fn(e)
                if o.signal:
                    ins.then_inc(sems[o.sigval[0]], 16 if o.dma else 1)
            if name == final_wait_eng:
                for s, v in finals:
                    e.wait_ge(s, v)

        with nc.Block() as block:
            @block.tensor
            def _(e):
                emit("pe", e)

            @block.scalar
            def _(e):
                emit("act", e)

            @block.vector
            def _(e):
                emit("dve", e)

            @block.gpsimd
            def _(e):
                emit("pool", e)

            @block.sync
            def _(e):
                emit("sp", e)
        self.stack.close()

DN_ALPHA = 4.0 ** 0.25
LN_EPS = 1e-5
NE = 32
NT = 2048


def emit_post(P, T, with_outproj, out_key, x_keys=(), m_keys=(), h_keys=(), zfill=None):
    TT = NT // 512
    xT = T.get("xT"); mT = T.get("mT"); wout = T.get("wout"); hT = T.get("hT")
    lnp = T["lnp"]; wr = T["wr"]; brt = T["brt"]; w1 = T["w1"]; b1T = T["b1T"]; w2 = T["w2"]; b2 = T["b2"]
    ident = T["ident"]; sel = T["sel"]; outT = T["outT"]
    z = P.sb([128, 8, NT], F32, "z")
    zb = P.sb([128, 8, NT], BF16, "zb")
    w1s = P.sb([128, 2, 8, 1024], BF16, "w1s")
    w2s = P.sb([128, 2, 4, 1024], BF16, "w2s")
    aT = P.sb([128, 2, 4, 512], BF16, "aT")
    tmp = P.sb([128, 8, 512], F32, "tmp")
    lnt = P.sb([128, 6, 512], F32, "lnt")
    gT = P.sb([32, NT], BF16, "gT")
    b1s = P.sb([128, NE * 16], F32, "b1s")
    b2b = P.sb([32, 1024], BF16, "b2b")
    lns = P.sb([128, 32], F32, "lns")
    wrs = P.sb([128, 8, 32], F32, "wrs")
    brs = P.sb([128, 32], F32, "brs")
    ids = P.sb([128, 128], F32, "ids")
    sels = P.sb([32, NE * 128], BF16, "sels")
    ones = P.sb([128, 128], F32, "ones")
    rt = P.sb([128, 8, 32], F32, "rt")
    ps = P.psum

    if zfill is None:
        P.dma("sp", z[:], xT.rearrange("(c p) t -> p c t", p=128), list(x_keys), ["z_all"])
    if zfill is not None:
        zfill(P, z, zb)
    elif with_outproj:
        P.dma("pool", zb[:], mT.rearrange("(c p) t -> p c t", p=128), list(m_keys), ["zb_all"])
        P.dma("pool", w1s[:, 0], wout.rearrange("(c p) n -> p c n", p=128), [], [("w1s", 0)])
    else:
        hv = hT.rearrange("(c p) t -> p c t", p=128)
        for t in range(TT):
            ts = slice(t * 512, (t + 1) * 512)
            P.dma("sp", tmp[:], hv[:, :, ts], list(h_keys), [("tmp", c) for c in range(8)])
            for c in range(8):
                P.dve(lambda e, c=c, ts=ts: e.scalar_tensor_tensor(
                    out=z[:, c, ts], in0=z[:, c, ts], scalar=DN_ALPHA, op0=ALU.mult,
                    in1=tmp[:, c, :], op1=ALU.add), ["z_all", ("z", c, t), ("tmp", c)], [("z", c, t)])
    P.dma("sp", lns[:], lnp, [], ["lns"])
    P.dma("sp", wrs[:], wr.rearrange("(c p) n -> p c n", p=128), [], ["wrs"])
    P.dma("sp", brs[:], brt, [], ["brs"])
    P.dma("sp", b1s[:], b1T, [], ["b1s"])
    P.dma("sp", ids[:], ident, [], ["ids"])
    P.dma("pool", b2b[:], b2, [], ["b2b"])
    P.dma("pool", sels[:], sel, [], ["sels"])
    P.dve(lambda e: e.memset(ones[:], 1.0), [], ["ones"])
    b1v = b1s[:].rearrange("p (e c) -> p e c", c=16)
    P.dve(lambda e: e.tensor_scalar(out=b1v[:, :, 8:16], in0=b1v[:, :, 8:16], scalar1=1.0,
                                    scalar2=None, op0=ALU.add), ["b1s"], ["b1s"])

    def zk(c, t):
        return ("z", c, t)

    first_z = [True]

    def zreads(c, t):
        return [zk(c, t), "z_all"]

    if with_outproj:
        for t in range(TT):
            ts = slice(t * 512, (t + 1) * 512)
            for j in range(8):
                bank = (t * 8 + j) % 4
                for kc in range(8):
                    P.pe(lambda e, bank=bank, kc=kc, j=j, ts=ts: e.matmul(
                        ps[:, bank, :], lhsT=w1s[:, 0, kc, j * 128:(j + 1) * 128],
                        rhs=zb[:, kc, ts], start=(kc == 0), stop=(kc == 7)),
                        [("w1s", 0), "zb_all"], [("ps", bank)])
                P.dve(lambda e, bank=bank, j=j, ts=ts: e.scalar_tensor_tensor(
                    out=z[:, j, ts], in0=z[:, j, ts], scalar=DN_ALPHA, op0=ALU.mult,
                    in1=ps[:, bank, :], op1=ALU.add),
                    zreads(j, t) + [("ps", bank)], [zk(j, t)])

    def layer_norm(goff, boff, make_bf16):
        for t in range(TT):
            ts = slice(t * 512, (t + 1) * 512)
            for c in range(8):
                P.act(lambda e, c=c, ts=ts: e.activation(out=tmp[:, c, :], in_=z[:, c, ts], func=AF.Square),
                      zreads(c, t), [("tmp", c)])
            for c in range(8):
                P.pe(lambda e, c=c, ts=ts: e.matmul(ps[:, 6, :], lhsT=ones[:], rhs=z[:, c, ts],
                                                    start=(c == 0), stop=(c == 7)),
                     zreads(c, t) + ["ones"], [("ps", 6)])
            for c in range(8):
                P.pe(lambda e, c=c: e.matmul(ps[:, 7, :], lhsT=ones[:], rhs=tmp[:, c, :],
                                             start=(c == 0), stop=(c == 7)),
                     [("tmp", c), "ones"], [("ps", 7)])
            mean, m2, var, rstd, mr = (lnt[:, i, :] for i in range(5))
            P.dve(lambda e: e.tensor_scalar(out=mean, in0=ps[:, 6, :], scalar1=1.0 / 1024, scalar2=None,
                                            op0=ALU.mult), [("ps", 6)], [("lnt", 0)])
            P.dve(lambda e: e.tensor_tensor(out=m2, in0=mean, in1=mean, op=ALU.mult),
                  [("lnt", 0)], [("lnt", 1)])
            P.dve(lambda e: e.scalar_tensor_tensor(out=var, in0=ps[:, 7, :], scalar=1.0 / 1024, op0=ALU.mult,
                                                   in1=m2, op1=ALU.subtract),
                  [("ps", 7), ("lnt", 1)], [("lnt", 2)])
            P.dve(lambda e: e.tensor_scalar(out=var, in0=var, scalar1=LN_EPS, scalar2=None, op0=ALU.add),
                  [("lnt", 2)], [("lnt", 2)])
            P.act(lambda e: e.activation(out=rstd, in_=var, func=AF.Sqrt),
                  [("lnt", 2)], [("lnt", 3)])
            P.dve(lambda e: e.reciprocal(out=rstd, in_=rstd), [("lnt", 3)], [("lnt", 3)])
            P.dve(lambda e: e.tensor_tensor(out=mr, in0=mean, in1=rstd, op=ALU.mult),
                  [("lnt", 0), ("lnt", 3)], [("lnt", 4)])
            for c in range(8):
                P.dve(lambda e, c=c, ts=ts: e.tensor_tensor(out=tmp[:, c, :], in0=z[:, c, ts], in1=rstd, op=ALU.mult),
                      zreads(c, t) + [("lnt", 3)], [("tmp", c)])
                P.dve(lambda e, c=c: e.tensor_tensor(out=tmp[:, c, :], in0=tmp[:, c, :], in1=mr, op=ALU.subtract),
                      [("tmp", c), ("lnt", 4)], [("tmp", c)])
                P.act(lambda e, c=c, ts=ts: e.activation(out=z[:, c, ts], in_=tmp[:, c, :], func=AF.Identity,
                                                         scale=lns[:, goff + c:goff + c + 1],
                                                         bias=lns[:, boff + c:boff + c + 1]),
                      [("tmp", c), "lns", "z_all"], [zk(c, t)])
                if make_bf16:
                    P.pool(lambda e, c=c, ts=ts: e.tensor_copy(out=zb[:, c, ts], in_=z[:, c, ts]),
                           [zk(c, t), "zb_all"], [("zb", t)])

    layer_norm(0, 8, True)

    for tt in range(NT // 128):
        tsl = slice(tt * 128, (tt + 1) * 128)
        t = tt // 4
        for c in range(8):
            P.pe(lambda e, c=c, tsl=tsl: e.matmul(ps[:, 7, 0:32], lhsT=z[:, c, tsl], rhs=wrs[:, c, :],
                                                  start=(c == 0), stop=(c == 7)),
                 [zk(c, t), "wrs"], [("ps", 7)])
        lg, m8, negm, ex, em, ssum, gt = (rt[:, i, :] for i in range(7))
        P.dve(lambda e: e.tensor_tensor(out=lg, in0=ps[:, 7, 0:32], in1=brs[:], op=ALU.add),
              [("ps", 7), "brs"], ["rt0"])
        P.dve(lambda e: e.max(out=m8[:, 0:8], in_=lg), ["rt0"], ["rt1"])
        P.dve(lambda e: e.tensor_scalar(out=negm[:, 0:1], in0=m8[:, 0:1], scalar1=-1.0, scalar2=None, op0=ALU.mult),
              ["rt1"], ["rt2"])
        P.act(lambda e: e.activation(out=ex, in_=lg, func=AF.Exp, bias=negm[:, 0:1], scale=1.0),
              ["rt0", "rt2"], ["rt3"])
        P.dve(lambda e: e.scalar_tensor_tensor(out=em, in0=lg, scalar=m8[:, 3:4], op0=ALU.is_ge,
                                               in1=ex, op1=ALU.mult, accum_out=ssum[:, 0:1]),
              ["rt0", "rt1", "rt3"], ["rt4", "rt5"])
        P.dve(lambda e: e.reciprocal(out=ssum[:, 1:2], in_=ssum[:, 0:1]), ["rt5"], ["rt5b"])
        P.dve(lambda e: e.tensor_scalar(out=gt, in0=em, scalar1=ssum[:, 1:2], scalar2=None, op0=ALU.mult),
              ["rt4", "rt5b"], ["rt6"])
        P.pe(lambda e: e.transpose(ps[0:32, 6, 0:128], gt, ids[:]), ["rt6", "ids"], [("ps", 6)])
        P.act(lambda e, tsl=tsl: e.copy(out=gT[:, tsl], in_=ps[0:32, 6, 0:128]), [("ps", 6)], ["gT"])

    for t in range(TT):
        ts = slice(t * 512, (t + 1) * 512)
        for j in range(8):
            bank = 4 + (j % 2)
            P.pe(lambda e, bank=bank, j=j, ts=ts: e.matmul(ps[:, bank, :], lhsT=b2b[:, j * 128:(j + 1) * 128],
                                                          rhs=gT[:, ts], start=True, stop=True),
                 ["b2b", "gT"], [("ps", bank)])
            P.dve(lambda e, bank=bank, j=j, ts=ts: e.scalar_tensor_tensor(
                out=z[:, j, ts], in0=z[:, j, ts], scalar=DN_ALPHA, op0=ALU.mult,
                in1=ps[:, bank, :], op1=ALU.add),
                [zk(j, t), ("ps", bank)], [zk(j, t)])

    w1v = w1.rearrange("e (kc p) n -> e p kc n", p=128)
    w2v = w2.rearrange("e (fc p) d -> e p fc d", p=128)
    units = [(ex_, h) for ex_ in range(NE) for h in range(2)]

    def load_unit(u):
        ex_, h = units[u]
        s = u % 2
        P.dma("pool", w1s[:, s, :, 0:512], w1v[ex_, :, :, h * 512:(h + 1) * 512], [], [("w1s", s)])
        P.dma("pool", w1s[:, s, :, 512:1024], w1v[ex_, :, :, 1024 + h * 512:1024 + (h + 1) * 512], [], [("w1s", s)])
        P.dma("pool", w2s[:, s], w2v[ex_, :, h * 4:(h + 1) * 4, :], [], [("w2s", s)])

    cnt = [0]

    def emit_gu(u, t):
        ex_, h = units[u]
        s = u % 2
        it = cnt[0]
        cnt[0] += 1
        ab = it % 2
        ts = slice(t * 512, (t + 1) * 512)
        P.pe(lambda e: e.matmul(ps[:, 6, :], lhsT=sels[:, ex_ * 128:(ex_ + 1) * 128], rhs=gT[:, ts],
                                start=True, stop=True), ["sels", "gT"], [("ps", 6)])
        for fl in range(4):
            pb = 2 * ((it * 4 + fl) % 2)
            tb = 4 * ((it * 4 + fl) % 2)
            for kc in range(8):
                P.pe(lambda e, kc=kc, fl=fl, pb=pb: e.matmul(
                    ps[:, pb, :], lhsT=w1s[:, s, kc, fl * 128:(fl + 1) * 128], rhs=zb[:, kc, ts],
                    start=(kc == 0), stop=(kc == 7)), [("w1s", s), ("zb", t)], [("ps", pb)])
            for kc in range(8):
                P.pe(lambda e, kc=kc, fl=fl, pb=pb: e.matmul(
                    ps[:, pb + 1, :], lhsT=w1s[:, s, kc, 512 + fl * 128:512 + (fl + 1) * 128], rhs=zb[:, kc, ts],
                    start=(kc == 0), stop=(kc == 7)), [("w1s", s), ("zb", t)], [("ps", pb + 1)])
            fch = h * 4 + fl
            bg = b1s[:, ex_ * 16 + fch:ex_ * 16 + fch + 1]
            bu = b1s[:, ex_ * 16 + 8 + fch:ex_ * 16 + 8 + fch + 1]
            g, sg, glu, u1 = (tmp[:, tb + i, :] for i in range(4))
            P.dve(lambda e, g=g, pb=pb, bg=bg: e.tensor_scalar(out=g, in0=ps[:, pb, :], scalar1=bg, scalar2=7.0,
                                                               op0=ALU.add, op1=ALU.min),
                  [("ps", pb), "b1s"], [("tmp", tb)])
            P.act(lambda e, g=g, sg=sg: e.activation(out=sg, in_=g, func=AF.Sigmoid, scale=1.702),
                  [("tmp", tb)], [("tmp", tb + 1)])
            P.dve(lambda e, u1=u1, pb=pb, bu=bu: e.tensor_scalar(out=u1, in0=ps[:, pb + 1, :], scalar1=bu, scalar2=-6.0,
                                                                 op0=ALU.add, op1=ALU.max),
                  [("ps", pb + 1), "b1s"], [("tmp", tb + 3)])
            P.dve(lambda e, g=g, sg=sg, glu=glu: e.tensor_tensor(out=glu, in0=g, in1=sg, op=ALU.mult),
                  [("tmp", tb), ("tmp", tb + 1)], [("tmp", tb + 2)])
            P.dve(lambda e, u1=u1, glu=glu: e.scalar_tensor_tensor(out=u1, in0=u1, scalar=8.0, op0=ALU.min,
                                                                  in1=glu, op1=ALU.mult),
                  [("tmp", tb + 3), ("tmp", tb + 2)], [("tmp", tb + 3)])
            P.dve(lambda e, u1=u1, fl=fl: e.tensor_tensor(out=aT[:, ab, fl, :], in0=u1, in1=ps[:, 6, :], op=ALU.mult),
                  [("tmp", tb + 3), ("ps", 6)], [("aT", ab)])
        return (u, t, ab)

    def emit_y(info):
        u, t, ab = info
        s = u % 2
        ts = slice(t * 512, (t + 1) * 512)
        for j in range(8):
            bank = 4 + (j % 2)
            for fl in range(4):
                P.pe(lambda e, fl=fl, j=j, bank=bank: e.matmul(
                    ps[:, bank, :], lhsT=w2s[:, s, fl, j * 128:(j + 1) * 128], rhs=aT[:, ab, fl, :],
                    start=(fl == 0), stop=(fl == 3)), [("w2s", s), ("aT", ab)], [("ps", bank)])
            P.dve(lambda e, j=j, bank=bank: e.tensor_tensor(out=z[:, j, ts], in0=z[:, j, ts], in1=ps[:, bank, :],
                                                            op=ALU.add),
                  [zk(j, t), ("ps", bank)], [zk(j, t)])

    load_unit(0)
    prev = None
    for u in range(len(units)):
        for t in range(TT):
            info = emit_gu(u, t)
            if prev is not None:
                emit_y(prev)
            prev = info
            if t == 0 and u + 1 < len(units):
                load_unit(u + 1)
    emit_y(prev)

    layer_norm(16, 24, False)
    ov = outT.rearrange("(c p) t -> p c t", p=128)
    for t in range(TT):
        ts = slice(t * 512, (t + 1) * 512)
        P.dma("sp", ov[:, :, ts], z[:, :, ts], [zk(c, t) for c in range(8)], [(out_key, t)], semkey=("store", out_key))

RMS_EPS = 1e-6
NEG = -1.0e30
NBIS = 16


def emit_dsa(P, T, n_blocks=64, SEQ=8192):
    NKT = SEQ // 512
    xbT = T["xbT"]; wq = T["wq"]; wc = T["wc"]; wqi = T["wqi"]; wki = T["wki"]; wwi = T["wwi"]
    kvp = T["kvp"]; kvb = T["kvb"]; wuk = T["wuk"]; wuv = T["wuv"]; cmask = T["cmask"]; ident = T["ident"]
    ovT = T["ovT"]
    kiT = P.sb([64, SEQ], BF16, "kiT")
    cT = P.sb([128, 2, SEQ], BF16, "cT")
    C = P.sb([128, SEQ // 128, 256], BF16, "C")
    sc = P.sb([128, SEQ], F32, "sc")
    junk = P.sb([128, 1024], BF16, "junk")
    maskT = P.sb([128, SEQ // 128, 128], BF16, "maskT")
    wqb = P.sb([128, 8, 1024], BF16, "wqb")
    wqib = P.sb([128, 8, 512], BF16, "wqib")
    wcb = P.sb([128, 8, 256], BF16, "wcb")
    wkib = P.sb([128, 8, 64], BF16, "wkib")
    wwib = P.sb([128, 8, 8], BF16, "wwib")
    wukb = P.sb([64, 16, 256], BF16, "wukb")
    wuvb = P.sb([128, 16, 2, 64], BF16, "wuvb")
    kvps = P.sb([128, 2], F32, "kvps"); kvbs = P.sb([128, 256], F32, "kvbs")
    cms = P.sb([128, 4, 512], BF16, "cms")
    idb = P.sb([128, 128], BF16, "idb")
    onesf = P.sb([128, 128], F32, "onesf"); onesb = P.sb([128, 128], BF16, "onesb")
    half = P.sb([128, 1], F32, "half")
    R = P.sb([128, 14336], BF16, "R")
    xkb = R[:, 0:8192].rearrange("p (s c t) -> p s c t", s=2, c=8)
    cpre = R[:, 8192:10240].bitcast(F32).rearrange("p (c t) -> p c t", c=2)
    sq = R[:, 10240:12288].bitcast(F32).rearrange("p (c t) -> p c t", c=2)
    rinvk = R[:, 12288:13312].bitcast(F32)
    ktmp = R[:, 13312:13824].bitcast(F32)
    ksm = P.sb([128, 4], F32, "ksm")
    qT = R[0:64, 0:2048].rearrange("p (h q) -> p h q", h=16)
    qiT = R[0:64, 2048:3072].rearrange("p (h q) -> p h q", h=8)
    qlT = R[:, 3072:7168].rearrange("p (c h q) -> p c h q", c=2, h=16)
    rbuf = R[:, 7168:8704].rearrange("p (r t) -> p r t", r=3)
    pe_ = R[:, 8704:9728].rearrange("p (r t) -> p r t", r=2)
    pm = R[:, 9728:10752].rearrange("p (r t) -> p r t", r=2)
    oT = R[:, 10752:11776].rearrange("p (h c q) -> p h c q", h=4, c=2)
    rinv = R[:, 11776:12800].bitcast(F32)
    xqb = P.sb([128, 8, 128], BF16, "xqb")
    wis = P.sb([128, 8], F32, "wis")
    ovs = P.sb([64, 16, 128], F32, "ovs")
    fdum = P.sb([128, 1], F32, "fdum")
    bs = P.sb([128, 8], F32, "bs")
    cnt4 = P.sb([128, 8], F32, "cnt4")
    ps = P.psum
    psb = ps[:].bitcast(BF16)

    r3 = lambda a: a.rearrange("(c p) n -> p c n", p=128)
    P.dma("pool", wcb[:], r3(wc), [], ["wcb"])
    P.dma("pool", wkib[:], r3(wki), [], ["wkib"])
    P.dma("pool", wqb[:], r3(wq), [], ["wqb"])
    P.dma("pool", wqib[:], r3(wqi), [], ["wqib"])
    P.dma("pool", wwib[:], r3(wwi), [], ["wwib"])
    P.dma("pool", wukb[:], wuk.rearrange("h d c -> d h c"), [], ["wukb"])
    P.dma("pool", wuvb[:], wuv.rearrange("h (cc p) v -> p h cc v", p=128), [], ["wuvb"])
    P.dma("pool", idb[:], ident, [], ["idb"])
    P.dma("sp", kvps[:], kvp, [], ["kvps"])
    P.dma("sp", kvbs[:], kvb, [], ["kvbs"])
    P.dma("pool", cms[:], cmask.rearrange("p (j k) -> p j k", j=4), [], ["cms"])
    P.dve(lambda e: e.memset(onesf[:], 1.0), [], ["onesf"])
    P.dve(lambda e: e.memset(onesb[:], 1.0), [], ["onesb"])
    P.dve(lambda e: e.memset(half[:], 0.5), [], ["half"])
    zerosb = P.sb([128, 128], BF16, "zerosb")
    P.dve(lambda e: e.memset(zerosb[:], 0.0), [], ["zerosb"])

    xbv = xbT.rearrange("(c p) t -> p c t", p=128)
    for kt in range(NKT):
        sl = kt % 2
        ks = slice(kt * 512, (kt + 1) * 512)
        P.dma("pool", xkb[:, sl], xbv[:, :, ks], [], [("xkb", sl)])
        for kc in range(8):
            P.pe(lambda e, kc=kc, sl=sl: e.matmul(ps[0:64, 0, :], lhsT=wkib[:, kc, :], rhs=xkb[:, sl, kc, :],
                                                  start=(kc == 0), stop=(kc == 7)),
                 ["wkib", ("xkb", sl)], [("ps", 0)])
        P.act(lambda e, ks=ks: e.copy(out=kiT[:, ks], in_=ps[0:64, 0, :]), [("ps", 0)], ["kiT"])
        for cc in range(2):
            for kc in range(8):
                P.pe(lambda e, kc=kc, sl=sl, cc=cc: e.matmul(ps[:, 1 + cc, :], lhsT=wcb[:, kc, cc * 128:(cc + 1) * 128],
                                                            rhs=xkb[:, sl, kc, :], start=(kc == 0), stop=(kc == 7)),
                     ["wcb", ("xkb", sl)], [("ps", 1 + cc)])
            P.act(lambda e, cc=cc: e.copy(out=cpre[:, cc, :], in_=ps[:, 1 + cc, :]), [("ps", 1 + cc)], [("cpre", cc)])
            P.act(lambda e, cc=cc: e.activation(out=sq[:, cc, :], in_=ps[:, 1 + cc, :], func=AF.Square),
                  [("ps", 1 + cc)], [("sq", cc)])
        for cc in range(2):
            P.pe(lambda e, cc=cc: e.matmul(ps[:, 3, :], lhsT=onesf[:], rhs=sq[:, cc, :], start=(cc == 0), stop=(cc == 1)),
                 ["onesf", ("sq", cc)], [("ps", 3)])
        P.dve(lambda e: e.tensor_scalar(out=rinvk, in0=ps[:, 3, :], scalar1=1.0 / 256, scalar2=RMS_EPS,
                                        op0=ALU.mult, op1=ALU.add), [("ps", 3)], ["rinvk"])
        P.act(lambda e: e.activation(out=rinvk, in_=rinvk, func=AF.Sqrt), ["rinvk"], ["rinvk"])
        P.dve(lambda e: e.reciprocal(out=rinvk, in_=rinvk), ["rinvk"], ["rinvk"])
        for cc in range(2):
            P.dve(lambda e, cc=cc, ks=ks: e.scalar_tensor_tensor(out=cT[:, cc, ks], in0=cpre[:, cc, :],
                                                                 scalar=kvps[:, cc:cc + 1], op0=ALU.mult,
                                                                 in1=rinvk, op1=ALU.mult),
                  [("cpre", cc), "kvps", "rinvk"], ["cT"])
        for k4 in range(4):
            kb = kt * 4 + k4
            bank = 4 + (kb % 2)
            for kc in range(8):
                P.pe(lambda e, kc=kc, sl=sl, k4=k4, bank=bank: e.matmul(
                    ps[:, bank, 0:256], lhsT=xkb[:, sl, kc, k4 * 128:(k4 + 1) * 128], rhs=wcb[:, kc, :],
                    start=(kc == 0), stop=(kc == 7)), ["wcb", ("xkb", sl)], [("ps", bank)])
            P.act(lambda e, bank=bank: e.activation(out=ktmp, in_=ps[:, bank, 0:256], func=AF.Square,
                                                    accum_out=ksm[:, 0:1]), [("ps", bank)], ["ktmp", "ksm0"])
            P.dve(lambda e: e.tensor_scalar(out=ksm[:, 1:2], in0=ksm[:, 0:1], scalar1=1.0 / 256, scalar2=RMS_EPS,
                                            op0=ALU.mult, op1=ALU.add), ["ksm0"], ["ksm1"])
            P.act(lambda e: e.activation(out=ksm[:, 2:3], in_=ksm[:, 1:2], func=AF.Sqrt), ["ksm1"], ["ksm2"])
            P.dve(lambda e: e.reciprocal(out=ksm[:, 3:4], in_=ksm[:, 2:3]), ["ksm2"], ["ksm3"])
            P.dve(lambda e, kb=kb, bank=bank: e.scalar_tensor_tensor(out=C[:, kb, :], in0=ps[:, bank, 0:256],
                                                                    scalar=ksm[:, 3:4], op0=ALU.mult,
                                                                    in1=kvbs[:], op1=ALU.mult),
                  [("ps", bank), "ksm3", "kvbs"], ["C"])

    kkeys = [("xkb", 0), ("xkb", 1), ("cpre", 0), ("cpre", 1), ("sq", 0), ("sq", 1), "rinvk", "ktmp"]
    qkeys = ["qT", "qiT", "qlT", ("rbuf", 0), ("rbuf", 1), ("rbuf", 2), ("pe", 0), ("pe", 1),
             ("pm", 0), ("pm", 1), "oT", "rinv"]
    P.dve(lambda e: e.memset(fdum[:], 0.0), [], kkeys + qkeys + ["fdum"])
    xqv = xbv
    ovv = ovT.rearrange("(h p) t -> p h t", p=64)
    lo, hi, mid, cnt, ge, d1 = (bs[:, i:i + 1] for i in range(6))

    def A1(g):
        s = g // 4
        j = g % 4
        qs = slice(g * 128, (g + 1) * 128)
        nk = 512 * (s + 1)
        P.dma("pool", xqb[:], xqv[:, :, qs], [], ["xqb"])
        for h in range(16):
            bank = 2 + h % 2
            for kc in range(8):
                P.pe(lambda e, h=h, kc=kc, bank=bank: e.matmul(ps[0:64, bank, 0:128], lhsT=wqb[:, kc, h * 64:(h + 1) * 64],
                                                                rhs=xqb[:, kc, :], start=(kc == 0), stop=(kc == 7)),
                     ["wqb", "xqb"], [("ps", bank)])
            P.act(lambda e, h=h, bank=bank: e.copy(out=qT[:, h, :], in_=ps[0:64, bank, 0:128]), [("ps", bank)], ["qT"])
            if h % 4 == 3:
                yield
        for h in range(8):
            bank = 2 + h % 2
            for kc in range(8):
                P.pe(lambda e, h=h, kc=kc, bank=bank: e.matmul(ps[0:64, bank, 0:128], lhsT=wqib[:, kc, h * 64:(h + 1) * 64],
                                                                rhs=xqb[:, kc, :], start=(kc == 0), stop=(kc == 7)),
                     ["wqib", "xqb"], [("ps", bank)])
            P.act(lambda e, h=h, bank=bank: e.copy(out=qiT[:, h, :], in_=ps[0:64, bank, 0:128]), [("ps", bank)], ["qiT"])
            if h % 4 == 3:
                yield
        for kc in range(8):
            P.pe(lambda e, kc=kc: e.matmul(ps[:, 3, 0:8], lhsT=xqb[:, kc, :], rhs=wwib[:, kc, :],
                                           start=(kc == 0), stop=(kc == 7)), ["wwib", "xqb"], [("ps", 3)])
        P.act(lambda e: e.copy(out=wis[:], in_=ps[:, 3, 0:8]), [("ps", 3)], ["wis"])
        yield
        it = 0
        for kt in range(s + 1):
            ks = slice(kt * 512, (kt + 1) * 512)
            for h in range(8):
                bank = 2 + it % 2
                rb = it % 3
                it += 1
                P.pe(lambda e, h=h, ks=ks, bank=bank: e.matmul(ps[:, bank, :], lhsT=qiT[:, h, :], rhs=kiT[:, ks],
                                                               start=True, stop=True), ["qiT", "kiT"], [("ps", bank)])
                P.act(lambda e, bank=bank, rb=rb: e.activation(out=rbuf[:, rb, :], in_=ps[:, bank, :], func=AF.Relu),
                      [("ps", bank)], [("rbuf", rb)])
                if h == 0:
                    P.dve(lambda e, rb=rb, ks=ks: e.tensor_scalar(out=sc[:, ks], in0=rbuf[:, rb, :], scalar1=wis[:, 0:1],
                                                                  scalar2=None, op0=ALU.mult),
                          [("rbuf", rb), "wis"], [("sc", kt)])
                else:
                    P.dve(lambda e, rb=rb, ks=ks, h=h: e.scalar_tensor_tensor(
                        out=sc[:, ks], in0=rbuf[:, rb, :], scalar=wis[:, h:h + 1], op0=ALU.mult,
                        in1=sc[:, ks], op1=ALU.add), [("rbuf", rb), "wis", ("sc", kt)], [("sc", kt)])
                yield
        sck = [("sc", kt) for kt in range(s + 1)]
        nch = (nk + 1023) // 1024
        for nm, op, dst in (("mn", ALU.min, lo), ("mx", ALU.max, hi)):
            if nm == "mx":
                P.dve(lambda e, nk=nk, j=j: e.tensor_tensor(out=sc[:, nk - 512:nk], in0=sc[:, nk - 512:nk],
                                                            in1=cms[:, j, :], op=ALU.add), [("sc", s), "cms"], [("sc", s)])
            for ch in range(nch):
                c0 = ch * 1024
                c1 = min(nk, c0 + 1024)
                P.dve(lambda e, c0=c0, c1=c1, ch=ch, op=op: e.tensor_reduce(out=cnt4[:, ch:ch + 1], in_=sc[:, c0:c1],
                                                                            op=op, axis=AX.X), sck, [("cnt4", ch)])
                yield
            if nch == 1:
                P.dve(lambda e, dst=dst: e.tensor_copy(out=dst, in_=cnt4[:, 0:1]), [("cnt4", 0)], ["lo" if nm == "mn" else "hi"])
            else:
                P.dve(lambda e, dst=dst, nch=nch, op=op: e.tensor_reduce(out=dst, in_=cnt4[:, 0:nch], op=op, axis=AX.X),
                      [("cnt4", i) for i in range(nch)], ["lo" if nm == "mn" else "hi"])
            if nm == "mn":
                P.dve(lambda e: e.tensor_scalar(out=lo, in0=lo, scalar1=-1.0, scalar2=None, op0=ALU.add), ["lo"], ["lo"])
        for itb in range(NBIS):
            P.dve(lambda e: e.scalar_tensor_tensor(out=mid, in0=lo, scalar=hi, op0=ALU.add, in1=half[:], op1=ALU.mult),
                  ["lo", "hi", "half"], ["mid"])
            for ch in range(nch):
                c0 = ch * 1024
                c1 = min(nk, c0 + 1024)
                P.dve(lambda e, c0=c0, c1=c1, ch=ch: e.tensor_scalar(out=junk[:, 0:c1 - c0], in0=sc[:, c0:c1], scalar1=mid,
                                                                     scalar2=None, op0=ALU.is_ge, op1=ALU.add,
                                                                     accum_out=cnt4[:, ch:ch + 1]),
                      sck + ["mid"], ["junk", ("cnt4", ch)])
                yield
            if nch == 1:
                P.dve(lambda e: e.tensor_copy(out=cnt, in_=cnt4[:, 0:1]), [("cnt4", 0)], ["cnt"])
            else:
                P.dve(lambda e, nch=nch: e.tensor_reduce(out=cnt, in_=cnt4[:, 0:nch], op=ALU.add, axis=AX.X),
                      [("cnt4", i) for i in range(nch)], ["cnt"])
            P.dve(lambda e: e.tensor_scalar(out=ge, in0=cnt, scalar1=256.0, scalar2=None, op0=ALU.is_ge),
                  ["cnt"], ["ge"])
            P.dve(lambda e: e.tensor_tensor(out=d1, in0=mid, in1=lo, op=ALU.subtract), ["mid", "lo"], ["d1"])
            P.dve(lambda e: e.scalar_tensor_tensor(out=lo, in0=d1, scalar=ge, op0=ALU.mult, in1=lo, op1=ALU.add),
                  ["d1", "ge", "lo"], ["lo"])
            P.dve(lambda e: e.tensor_tensor(out=d1, in0=hi, in1=mid, op=ALU.subtract), ["mid", "hi"], ["d1"])
            P.dve(lambda e: e.scalar_tensor_tensor(out=hi, in0=d1, scalar=ge, op0=ALU.mult, in1=mid, op1=ALU.add),
                  ["d1", "ge", "mid"], ["hi"])
            yield

    def A2(g):
        s = g // 4
        nk = 512 * (s + 1)
        sck = [("sc", kt) for kt in range(s + 1)]
        nch = (nk + 1023) // 1024
        for ch in range(nch):
            c0 = ch * 1024
            c1 = min(nk, c0 + 1024)
            P.dve(lambda e, c0=c0, c1=c1: e.tensor_scalar(out=junk[:, 0:c1 - c0], in0=sc[:, c0:c1], scalar1=lo,
                                                          scalar2=None, op0=ALU.is_ge), sck + ["lo"], ["junk"])
            for g4 in range((c1 - c0) // 512):
                bank = 2 + (g4 % 2)
                for k4 in range(4):
                    col = g4 * 512 + k4 * 128
                    P.pe(lambda e, col=col, k4=k4, bank=bank: e.transpose(
                        psb[:, bank, k4 * 128:(k4 + 1) * 128], junk[:, col:col + 128], idb[:]),
                        ["junk", "idb"], [("ps", bank)])
                kb0 = (c0 + g4 * 512) // 128
                P.act(lambda e, kb0=kb0, bank=bank: e.copy(
                    out=maskT[:, kb0:kb0 + 4, :], in_=psb[:, bank, 0:512].rearrange("p (k q) -> p k q", k=4)),
                    [("ps", bank)], ["maskT"])
        for cc in range(2):
            for hg in range(4):
                bank = 4 + ((cc * 4 + hg) % 2)
                for hl in range(4):
                    h = hg * 4 + hl
                    P.pe(lambda e, h=h, hl=hl, cc=cc, bank=bank: e.matmul(
                        ps[:, bank, hl * 128:(hl + 1) * 128], lhsT=wukb[:, h, cc * 128:(cc + 1) * 128], rhs=qT[:, h, :],
                        start=True, stop=True), ["wukb", "qT"], [("ps", bank)])
                P.act(lambda e, cc=cc, hg=hg, bank=bank: e.activation(
                    out=qlT[:, cc, hg * 4:(hg + 1) * 4, :], in_=ps[:, bank, :].rearrange("p (h q) -> p h q", h=4),
                    func=AF.Copy, scale=0.125), [("ps", bank)], ["qlT"])

    stepc = [0]

    def B(g, gen):
        s = g // 4
        qs = slice(g * 128, (g + 1) * 128)
        nk = 512 * (s + 1)
        nkb = nk // 128

        def advance():
            if gen is not None:
                next(gen, None)

        for hg in range(4):
            def emit_S(kb, sb_, hg=hg):
                for cc in range(2):
                    P.pe(lambda e, cc=cc, kb=kb, sb_=sb_, hg=hg: e.matmul(
                        ps[:, sb_, :], lhsT=cT[:, cc, kb * 128:(kb + 1) * 128],
                        rhs=qlT[:, cc, hg * 4:(hg + 1) * 4, :], start=(cc == 0), stop=(cc == 1)),
                        ["cT", "qlT"], [("ps", sb_)])
                P.act(lambda e, sb_=sb_: e.activation(out=pe_[:, sb_, :], in_=ps[:, sb_, :], func=AF.Exp),
                      [("ps", sb_)], [("pe", sb_)])
                P.dve(lambda e, sb_=sb_, kb=kb: e.tensor_tensor(
                    out=pm[:, sb_, :].rearrange("p (h q) -> p h q", h=4),
                    in0=pe_[:, sb_, :].rearrange("p (h q) -> p h q", h=4),
                    in1=maskT[:, kb, :].unsqueeze(1).broadcast_to([128, 4, 128]), op=ALU.mult),
                    [("pe", sb_), "maskT"], [("pm", sb_)])

            def emit_PV(kb, sb_, nkb_=nkb):
                if kb == 0:
                    for b2 in range(2):
                        P.pe(lambda e, b2=b2, sb_=sb_: e.matmul(ps[:, 4 + b2, :], lhsT=zerosb[:], rhs=pm[:, sb_, :],
                                                              start=True, stop=False),
                             ["zerosb", ("pm", sb_)], [("ps", 4 + b2)])
                for hl in range(4):
                    for cc in range(2):
                        P.pe(lambda e, hl=hl, cc=cc, kb=kb, sb_=sb_, nkb_=nkb_: e.matmul(
                            ps[:, 4 + hl // 2, ((hl % 2) * 2 + cc) * 128:((hl % 2) * 2 + cc + 1) * 128],
                            lhsT=C[:, kb, cc * 128:(cc + 1) * 128], rhs=pm[:, sb_, hl * 128:(hl + 1) * 128],
                            start=False, stop=(kb == nkb_ - 1)), ["C", ("pm", sb_)], [("ps", 4 + hl // 2)])
                P.pe(lambda e, kb=kb, sb_=sb_, nkb_=nkb_: e.matmul(ps[:, 6, :], lhsT=onesb[:], rhs=pm[:, sb_, :],
                                                                  start=(kb == 0), stop=(kb == nkb_ - 1)),
                     ["onesb", ("pm", sb_)], [("ps", 6)])

            slots = [(stepc[0] + kb) % 2 for kb in range(nkb)]
            stepc[0] += nkb
            emit_S(0, slots[0])
            for kb in range(nkb):
                if kb + 1 < nkb:
                    emit_S(kb + 1, slots[kb + 1])
                advance()
                emit_PV(kb, slots[kb])
            P.dve(lambda e: e.reciprocal(out=rinv, in_=ps[:, 6, :]), [("ps", 6)], ["rinv"])
            for hl in range(4):
                for cc in range(2):
                    P.dve(lambda e, hl=hl, cc=cc: e.tensor_tensor(
                        out=oT[:, hl, cc, :],
                        in0=ps[:, 4 + hl // 2, ((hl % 2) * 2 + cc) * 128:((hl % 2) * 2 + cc + 1) * 128],
                        in1=rinv[:, hl * 128:(hl + 1) * 128], op=ALU.mult),
                        [("ps", 4 + hl // 2), "rinv"], ["oT"])
            for hl in range(4):
                h = hg * 4 + hl
                for cc in range(2):
                    P.pe(lambda e, h=h, hl=hl, cc=cc: e.matmul(ps[0:64, 7, hl * 128:(hl + 1) * 128],
                                                               lhsT=wuvb[:, h, cc, :], rhs=oT[:, hl, cc, :],
                                                               start=(cc == 0), stop=(cc == 1)),
                         ["wuvb", "oT"], [("ps", 7)])
            P.act(lambda e, hg=hg: e.copy(out=ovs[:, hg * 4:(hg + 1) * 4, :],
                                          in_=ps[0:64, 7, :].rearrange("p (h q) -> p h q", h=4)),
                  [("ps", 7)], ["ovs"])
        P.dma("sp", ovv[:, :, qs], ovs[:], ["ovs"], [("ov", g)], semkey="store_ov")

    for _ in A1(0):
        pass
    A2(0)
    for g in range(n_blocks):
        gen = A1(g + 1) if g + 1 < n_blocks else None
        B(g, gen)
        if gen is not None:
            for _ in gen:
                pass
            A2(g + 1)


def emit_gla(P, T, h, n_groups=16, SEQ=8192):
    x2g = T["x2full"]; hpart = T["hpart"][h]; ident = T["ident"]; wo1 = T["wo1"][h * 256:(h + 1) * 256, :]
    wq = T["gwq"][:, h * 128:(h + 1) * 128]; wk = T["gwk"][:, h * 128:(h + 1) * 128]
    wv = T["gwv"][:, h * 256:(h + 1) * 256]; wg = T["gwg"]; wr = T["gwr"][:, h * 256:(h + 1) * 256]
    wg2 = T["wg2"][:, h * 128:(h + 1) * 128]; gb = T["gb"][:, h * 128:(h + 1) * 128]
    ngb = T["ngb"][:, h * 256:(h + 1) * 256]; triA = T["triA"]; triUA = T["triUA"]; triM = T["triM"]
    xg = P.sb([128, 2, 8, 512], BF16, "xg")
    wqb = P.sb([128, 8, 128], BF16, "wqb"); wkb = P.sb([128, 8, 128], BF16, "wkb")
    wvb = P.sb([128, 8, 256], BF16, "wvb"); wrb = P.sb([128, 8, 256], BF16, "wrb")
    wgb = P.sb([128, 8, 16], BF16, "wgb")
    wg2s = P.sb([16, 128], F32, "wg2s"); gbs = P.sb([1, 128], F32, "gbs"); ngs = P.sb([64, 256], F32, "ngs")
    triAs = P.sb([64, 64], F32, "triAs"); triUAs = P.sb([64, 64], F32, "triUAs"); triMb = P.sb([64, 64], BF16, "triMb")
    ones1 = P.sb([1, 64], F32, "ones1")
    glr = P.sb([16, 512], F32, "glr")
    e1 = P.sb([64, 8, 128], F32, "e1"); l1 = P.sb([64, 8, 128], F32, "l1")
    ebc = P.sb([128, 512], F32, "ebc"); enb = P.sb([128, 512], F32, "enb")
    eb = P.sb([128, 8], F32, "eb")
    qt = P.sb([128, 512], BF16, "qt"); kt = P.sb([128, 512], BF16, "kt")
    ed2 = P.sb([64, 8, 128], F32, "ed2"); kh = P.sb([64, 8, 128], BF16, "kh")
    vb = P.sb([64, 8, 256], BF16, "vb")
    sig = P.sb([64, 8, 256], F32, "sig"); sr = P.sb([64, 8, 256], F32, "sr")
    Am = P.sb([64, 8, 64], BF16, "Am")
    obuf = P.sb([64, 8, 256], F32, "obuf"); otmp = P.sb([64, 8, 256], F32, "otmp")
    ss = P.sb([64, 8], F32, "ss")
    S = P.sb([128, 256], F32, "S"); Sb = P.sb([128, 256], BF16, "Sb")
    ps = P.psum
    idf = P.sb([128, 128], F32, "idf")
    wo1b = P.sb([128, 2, 1024], BF16, "wo1b")
    ogT = P.sb([128, 2, 512], BF16, "ogT")
    hp = P.sb([128, 8, 512], F32, "hp")

    r3 = lambda a: a.rearrange("(c p) n -> p c n", p=128)
    for dst, src, k in ((wqb, wq, "wqb"), (wkb, wk, "wkb"), (wvb, wv, "wvb"), (wrb, wr, "wrb"), (wgb, wg, "wgb")):
        P.dma("pool", dst[:], r3(src), [], [k])
    P.dma("pool", triMb[:], triM, [], ["triMb"])
    P.dma("pool", wo1b[:], wo1.rearrange("(ec p) d -> p ec d", p=128), [], ["wo1b"])
    P.dma("sp", idf[:], ident, [], ["idf"])
    for dst, src, k in ((wg2s, wg2, "wg2s"), (gbs, gb, "gbs"), (ngs, ngb, "ngs"), (triAs, triA, "triAs"),
                        (triUAs, triUA, "triUAs")):
        P.dma("sp", dst[:], src, [], [k])
    P.dve(lambda e: e.memset(ones1[:], 1.0), [], ["ones1"])
    P.dve(lambda e: e.memset(S[:], 0.0), [], ["S"])
    P.dve(lambda e: e.memset(Sb[:], 0.0), [], ["Sb"])

    bankc = [0]

    def nb():
        b = bankc[0] % 6
        bankc[0] += 1
        return b

    xv = x2g.rearrange("(c p) t -> p c t", p=128)
    hpv = hpart.rearrange("(c p) t -> p c t", p=128)
    def group(G):
        sl = G % 2
        P.dma("pool", xg[:, sl], xv[:, :, G * 512:(G + 1) * 512], [(("x2f", G // 4), G % 4)], [("xg", sl)])
        X = ("xg", sl)
        bq, bk, bg = nb(), nb(), nb()
        for kc in range(8):
            P.pe(lambda e, kc=kc: e.matmul(ps[:, bq, :], lhsT=wqb[:, kc, :], rhs=xg[:, sl, kc, :],
                                           start=(kc == 0), stop=(kc == 7)), ["wqb", X], [("ps", bq)])
        for kc in range(8):
            P.pe(lambda e, kc=kc: e.matmul(ps[:, bk, :], lhsT=wkb[:, kc, :], rhs=xg[:, sl, kc, :],
                                           start=(kc == 0), stop=(kc == 7)), ["wkb", X], [("ps", bk)])
        for kc in range(8):
            P.pe(lambda e, kc=kc: e.matmul(ps[0:16, bg, :], lhsT=wgb[:, kc, :], rhs=xg[:, sl, kc, :],
                                           start=(kc == 0), stop=(kc == 7)), ["wgb", X], [("ps", bg)])
        P.act(lambda e: e.copy(out=glr[:], in_=ps[0:16, bg, :]), [("ps", bg)], ["glr"])
        for hf in range(2):
            b = nb()
            for c4 in range(4):
                c = hf * 4 + c4
                P.pe(lambda e, c=c, c4=c4, b=b: e.matmul(ps[0:64, b, c4 * 128:(c4 + 1) * 128],
                                                         lhsT=glr[:, c * 64:(c + 1) * 64], rhs=wg2s[:],
                                                         start=True, stop=False), ["glr", "wg2s"], [("ps", b)])
                P.pe(lambda e, c4=c4, b=b: e.matmul(ps[0:64, b, c4 * 128:(c4 + 1) * 128],
                                                    lhsT=ones1[:], rhs=gbs[:], start=False, stop=True),
                     ["ones1", "gbs"], [("ps", b)])
            P.act(lambda e, hf=hf, b=b: e.activation(out=e1[:, hf * 4:(hf + 1) * 4, :],
                                                     in_=ps[0:64, b, :].rearrange("p (c d) -> p c d", c=4),
                                                     func=AF.Exp, scale=-1.0), [("ps", b)], [("e1", hf)])
            P.act(lambda e, hf=hf: e.activation(out=l1[:, hf * 4:(hf + 1) * 4, :], in_=e1[:, hf * 4:(hf + 1) * 4, :],
                                                func=AF.Ln, bias=1.0, scale=1.0), [("e1", hf)], [("l1", hf)])
        bb = nb()
        for c in range(8):
            P.pe(lambda e, c=c: e.matmul(ps[:, bb, c * 64:(c + 1) * 64], lhsT=l1[:, c, :], rhs=triAs[:],
                                         start=True, stop=True), [("l1", c // 4), "triAs"], [("ps", bb)])
        P.act(lambda e: e.activation(out=ebc[:], in_=ps[:, bb, :], func=AF.Exp), [("ps", bb)], ["ebc"])
        P.act(lambda e: e.activation(out=enb[:], in_=ps[:, bb, :], func=AF.Exp, scale=-1.0), [("ps", bb)], ["enb"])
        P.act(lambda e: e.activation(out=eb[:], in_=ps[:, bb, :].rearrange("p (c i) -> p c i", c=8)[:, :, 63],
                                     func=AF.Exp), [("ps", bb)], ["eb"])
        P.dve(lambda e: e.scalar_tensor_tensor(out=qt[:], in0=ps[:, bq, :], scalar=128.0 ** -0.5, op0=ALU.mult,
                                               in1=ebc[:], op1=ALU.mult), [("ps", bq), "ebc"], ["qt"])
        P.dve(lambda e: e.tensor_tensor(out=kt[:], in0=ps[:, bk, :], in1=enb[:], op=ALU.mult),
              [("ps", bk), "enb"], ["kt"])
        for hf in range(2):
            b = nb()
            P.pe(lambda e, hf=hf, b=b: e.matmul(ps[0:64, b, :], lhsT=triUAs[:],
                                                rhs=l1[:, hf * 4:(hf + 1) * 4, :], start=True, stop=True),
                 [("l1", hf), "triUAs"], [("ps", b)])
            P.act(lambda e, hf=hf, b=b: e.activation(out=ed2[:, hf * 4:(hf + 1) * 4, :],
                                                     in_=ps[0:64, b, :].rearrange("p (c d) -> p c d", c=4),
                                                     func=AF.Exp), [("ps", b)], [("ed2", hf)])
            b2 = nb()
            for c4 in range(4):
                c = hf * 4 + c4
                for kc in range(8):
                    P.pe(lambda e, c=c, c4=c4, kc=kc, b2=b2: e.matmul(
                        ps[0:64, b2, c4 * 128:(c4 + 1) * 128], lhsT=xg[:, sl, kc, c * 64:(c + 1) * 64],
                        rhs=wkb[:, kc, :], start=(kc == 0), stop=(kc == 7)), ["wkb", X], [("ps", b2)])
            P.dve(lambda e, hf=hf, b2=b2: e.tensor_tensor(out=kh[:, hf * 4:(hf + 1) * 4, :],
                                                          in0=ps[0:64, b2, :].rearrange("p (c d) -> p c d", c=4),
                                                          in1=ed2[:, hf * 4:(hf + 1) * 4, :], op=ALU.mult),
                  [("ps", b2), ("ed2", hf)], [("kh", hf)])
        for c2 in range(4):
            b = nb()
            for cc in range(2):
                c = c2 * 2 + cc
                for kc in range(8):
                    P.pe(lambda e, c=c, cc=cc, kc=kc, b=b: e.matmul(
                        ps[0:64, b, cc * 256:(cc + 1) * 256], lhsT=xg[:, sl, kc, c * 64:(c + 1) * 64],
                        rhs=wvb[:, kc, :], start=(kc == 0), stop=(kc == 7)), ["wvb", X], [("ps", b)])
            P.act(lambda e, c2=c2, b=b: e.copy(out=vb[:, c2 * 2:(c2 + 1) * 2, :],
                                               in_=ps[0:64, b, :].rearrange("p (c d) -> p c d", c=2)),
                  [("ps", b)], [("vb", c2)])
            b = nb()
            for cc in range(2):
                c = c2 * 2 + cc
                for kc in range(8):
                    P.pe(lambda e, c=c, cc=cc, kc=kc, b=b: e.matmul(
                        ps[0:64, b, cc * 256:(cc + 1) * 256], lhsT=xg[:, sl, kc, c * 64:(c + 1) * 64],
                        rhs=wrb[:, kc, :], start=(kc == 0), stop=(kc == 7)), ["wrb", X], [("ps", b)])
            P.act(lambda e, c2=c2, b=b: e.activation(out=sig[:, c2 * 2:(c2 + 1) * 2, :],
                                                     in_=ps[0:64, b, :].rearrange("p (c d) -> p c d", c=2),
                                                     func=AF.Sigmoid), [("ps", b)], [("sig", c2)])
            P.dve(lambda e, c2=c2, b=b: e.tensor_tensor(out=sr[:, c2 * 2:(c2 + 1) * 2, :],
                                                        in0=ps[0:64, b, :].rearrange("p (c d) -> p c d", c=2),
                                                        in1=sig[:, c2 * 2:(c2 + 1) * 2, :], op=ALU.mult),
                  [("ps", b), ("sig", c2)], [("sr", c2)])
        ba = nb()
        for c in range(8):
            P.pe(lambda e, c=c: e.matmul(ps[0:64, ba, c * 64:(c + 1) * 64], lhsT=kt[:, c * 64:(c + 1) * 64],
                                         rhs=qt[:, c * 64:(c + 1) * 64], start=True, stop=True),
                 ["kt", "qt"], [("ps", ba)])
        P.dve(lambda e: e.tensor_tensor(out=Am[:], in0=ps[0:64, ba, :].rearrange("p (c i) -> p c i", c=8),
                                        in1=triMb[:].unsqueeze(1).broadcast_to([64, 8, 64]), op=ALU.mult),
              [("ps", ba), "triMb"], ["Am"])
        for c in range(8):
            P.pe(lambda e, c=c: e.matmul(ps[0:64, 6, (c % 2) * 256:(c % 2 + 1) * 256], lhsT=Am[:, c, :], rhs=vb[:, c, :],
                                         start=True, stop=False), ["Am", ("vb", c // 2)], [("ps", 6)])
            P.pe(lambda e, c=c: e.matmul(ps[0:64, 6, (c % 2) * 256:(c % 2 + 1) * 256], lhsT=qt[:, c * 64:(c + 1) * 64],
                                         rhs=Sb[:], start=False, stop=True), ["qt", "Sb"], [("ps", 6)])
            P.act(lambda e, c=c: e.copy(out=obuf[:, c, :], in_=ps[0:64, 6, (c % 2) * 256:(c % 2 + 1) * 256]),
                  [("ps", 6)], ["obuf"])
            P.pe(lambda e, c=c: e.matmul(ps[:, 7, 0:256], lhsT=kh[:, c, :], rhs=vb[:, c, :], start=True, stop=True),
                 [("kh", c // 4), ("vb", c // 2)], [("ps", 7)])
            P.dve(lambda e, c=c: e.scalar_tensor_tensor(out=S[:], in0=S[:], scalar=eb[:, c:c + 1], op0=ALU.mult,
                                                        in1=ps[:, 7, 0:256], op1=ALU.add),
                  ["S", "eb", ("ps", 7)], ["S"])
            P.act(lambda e: e.copy(out=Sb[:], in_=S[:]), ["S"], ["Sb"])
        P.dve(lambda e: e.tensor_tensor(out=otmp[:], in0=obuf[:], in1=obuf[:], op=ALU.mult), ["obuf"], ["otmp"])
        P.dve(lambda e: e.tensor_reduce(out=ss[:], in_=otmp[:], op=ALU.add, axis=AX.X), ["otmp"], ["ss"])
        P.dve(lambda e: e.tensor_scalar(out=ss[:], in0=ss[:], scalar1=1.0 / 256, scalar2=RMS_EPS, op0=ALU.mult,
                                        op1=ALU.add), ["ss"], ["ss"])
        P.act(lambda e: e.activation(out=ss[:], in_=ss[:], func=AF.Sqrt), ["ss"], ["ss"])
        P.dve(lambda e: e.reciprocal(out=ss[:], in_=ss[:]), ["ss"], ["ss"])
        P.dve(lambda e: e.tensor_tensor(out=otmp[:], in0=obuf[:], in1=ss[:].unsqueeze(2).broadcast_to([64, 8, 256]),
                                        op=ALU.mult), ["obuf", "ss"], ["otmp"])
        P.dve(lambda e: e.tensor_tensor(out=otmp[:], in0=otmp[:], in1=ngs[:].unsqueeze(1).broadcast_to([64, 8, 256]),
                                        op=ALU.mult), ["otmp", "ngs"], ["otmp"])
        P.dve(lambda e: e.tensor_tensor(out=otmp[:], in0=otmp[:], in1=sr[:], op=ALU.mult),
              ["otmp"] + [("sr", i) for i in range(4)], ["otmp"])
        for ec in range(2):
            b = nb()
            for c in range(8):
                P.pe(lambda e, c=c, ec=ec, b=b: e.transpose(ps[:, b, c * 64:(c + 1) * 64],
                                                            otmp[:, c, ec * 128:(ec + 1) * 128], idf[0:64, 0:64]),
                     ["otmp", "idf"], [("ps", b)])
            P.act(lambda e, ec=ec, b=b: e.copy(out=ogT[:, ec, :], in_=ps[:, b, :]), [("ps", b)], [("ogT", ec)])
        for dc in range(8):
            b = nb()
            for ec in range(2):
                P.pe(lambda e, dc=dc, ec=ec, b=b: e.matmul(ps[:, b, :], lhsT=wo1b[:, ec, dc * 128:(dc + 1) * 128],
                                                          rhs=ogT[:, ec, :], start=(ec == 0), stop=(ec == 1)),
                     ["wo1b", ("ogT", ec)], [("ps", b)])
            if dc % 2 == 0:
                P.act(lambda e, dc=dc, b=b: e.copy(out=hp[:, dc, :], in_=ps[:, b, :]), [("ps", b)], [("hp", dc)])
            else:
                P.dve(lambda e, dc=dc, b=b: e.tensor_copy(out=hp[:, dc, :], in_=ps[:, b, :]), [("ps", b)], [("hp", dc)])
        P.dma("sp", hpv[:, :, G * 512:(G + 1) * 512], hp[:],
              [("hp", dc) for dc in range(8)], [("hpart", h, G)], semkey="store_hp")
    for G in range(n_groups):
        group(G)


def build_fused():
    nc = bass.Bass("TRN2", target_bir_lowering=False, dynamic_dma_scratch_size=8192)
    T = {}

    def din(n, s):
        T[n] = nc.dram_tensor(n, list(s), F32, kind="ExternalInput").ap()

    for n, s in (("xbT", [1024, 8192]), ("wq", [1024, 1024]), ("wc", [1024, 256]),
                 ("wqi", [1024, 512]), ("wki", [1024, 64]), ("wwi", [1024, 8]), ("kvp", [128, 2]),
                 ("kvb", [128, 256]), ("wuk", [16, 64, 256]), ("wuv", [16, 256, 64]), ("cmask", [128, 2048]),
                 ("ident", [128, 128]), ("sel", [32, NE * 128]), ("msel", [128, 4]),
                 ("wout0", [1024, 1024]), ("wo1", [1024, 1024]),
                 ("gwq", [1024, 512]), ("gwk", [1024, 512]), ("gwv", [1024, 1024]), ("gwg", [1024, 16]),
                 ("gwr", [1024, 1024]), ("wg2", [16, 512]), ("gb", [1, 512]), ("ngb", [64, 1024]),
                 ("triA", [64, 64]), ("triUA", [64, 64]), ("triM", [64, 64])):
        din(n, s)
    for L in range(2):
        for n, s in (("lnp", [128, 32]), ("wr", [1024, 32]), ("brt", [128, 32]), ("w1", [NE, 1024, 2048]),
                     ("b1T", [128, NE * 16]), ("w2", [NE, 1024, 1024]), ("b2", [NE, 1024])):
            din(f"{n}{L}", s)
    outT = nc.dram_tensor("outT", [1024, 2048], F32, kind="ExternalOutput").ap()
    ovT = nc.dram_tensor("ovT_i", [1024, 8192], F32).ap()
    x2full = nc.dram_tensor("x2full", [1024, 8192], F32).ap()
    hpart = [nc.dram_tensor(f"hpart{h}", [1024, 8192], F32).ap() for h in range(4)]

    P = Prog(nc)
    emit_dsa(P, dict(T, ovT=ovT))
    for p in range(4):
        P.fence()
        P.reset_alloc()
        cs = slice(p * 2048, (p + 1) * 2048)
        T0 = dict(xT=T["xbT"][:, cs], mT=ovT[:, cs], wout=T["wout0"], lnp=T["lnp0"], wr=T["wr0"], brt=T["brt0"],
                  w1=T["w10"], b1T=T["b1T0"], w2=T["w20"], b2=T["b20"], ident=T["ident"], sel=T["sel"],
                  outT=x2full[:, cs])
        emit_post(P, T0, True, ("x2f", p), m_keys=[("ov", g) for g in range(16 * p, 16 * p + 16)])
    for h in range(4):
        P.fence()
        P.reset_alloc()
        emit_gla(P, dict(T, x2full=x2full, hpart=hpart), h)
    P.fence()
    P.reset_alloc()
    x2v = x2full.rearrange("(c p) t -> p c t", p=128)
    hv = [hp_.rearrange("(c p) t -> p c t", p=128) for hp_ in hpart]
    msel = T["msel"]

    def zfill(P, z, zb):
        msl = P.sb([128, 8], F32, "msl")
        zbf = zb.rearrange("p c t -> p (c t)")
        selx = zbf[:, 0:4096].bitcast(F32).rearrange("p (b c q) -> p b c q", b=2, c=8)
        selh = zbf[:, 4096:12288].bitcast(F32).rearrange("p (h c q) -> p h c q", h=4, c=8)
        P.dma("sp", msl[:, 0:4], msel, [], ["msl0"])
        P.dve(lambda e: e.tensor_scalar(out=msl[:, 4:8], in0=msl[:, 0:4], scalar1=DN_ALPHA, scalar2=None,
                                        op0=ALU.mult), ["msl0"], ["msl"])
        it = 0
        for s in range(16):
            zs = z[:, :, s * 128:(s + 1) * 128]
            for r in range(4):
                g = 4 * s + r
                b = it % 2
                it += 1
                cols = slice(g * 128, (g + 1) * 128)
                P.dma("sp", selx[:, b], x2v[:, :, cols], [(("x2f", g // 16), (g % 16) // 4)], [("selx", b)])
                for h in range(4):
                    P.dma("sp", selh[:, h], hv[h][:, :, cols], [("hpart", h, g // 4)], [("selh", h)])
                if r == 0:
                    P.dve(lambda e, zs=zs, b=b, r=r: e.tensor_scalar(out=zs, in0=selx[:, b], scalar1=msl[:, 4 + r:5 + r],
                                                                     scalar2=None, op0=ALU.mult),
                          [("selx", b), "msl"], ["z_all"])
                else:
                    P.dve(lambda e, zs=zs, b=b, r=r: e.scalar_tensor_tensor(out=zs, in0=selx[:, b],
                                                                            scalar=msl[:, 4 + r:5 + r], op0=ALU.mult,
                                                                            in1=zs, op1=ALU.add),
                          [("selx", b), "msl", "z_all"], ["z_all"])
                for h in range(4):
                    P.dve(lambda e, zs=zs, b=b, r=r, h=h: e.scalar_tensor_tensor(out=zs, in0=selh[:, h],
                                                                                 scalar=msl[:, r:r + 1], op0=ALU.mult,
                                                                                 in1=zs, op1=ALU.add),
                          [("selh", h), "msl", "msl0", "z_all"], ["z_all"])

    T1 = dict(lnp=T["lnp1"], wr=T["wr1"], brt=T["brt1"], w1=T["w11"], b1T=T["b1T1"],
              w2=T["w21"], b2=T["b21"], ident=T["ident"], sel=T["sel"], outT=outT)
    emit_post(P, T1, False, "out", zfill=zfill)
    P.finalize()
    return nc, len(P.ops)


_CACHE = {}


def kernel(x, a_w_in, a_kv_norm, a_w_uk, a_w_uv, a_w_out,
           b_w_in, b_w_g2, b_g_bias, b_norm, b_w_out,
           m_w_router, m_b_router, m_w1, m_b1, m_w2, m_b2,
           ln1_g, ln1_b, ln2_g, ln2_b):
    f = lambda a: np.ascontiguousarray(np.asarray(a), dtype=np.float32)
    x = f(x)
    B, L, D = x.shape
    cores = list(range(8))
    if "nc" not in _CACHE:
        _CACHE["nc"] = build_fused()[0]
    nc = _CACHE["nc"]
    w_in = f(a_w_in[0]); kvn = f(a_kv_norm[0])
    o1, o2, o3, o4 = 1024, 1280, 1792, 1856
    shared = {
        "wq": np.ascontiguousarray(w_in[:, :o1]), "wc": np.ascontiguousarray(w_in[:, o1:o2]),
        "wqi": np.ascontiguousarray(w_in[:, o2:o3]), "wki": np.ascontiguousarray(w_in[:, o3:o4]),
        "wwi": np.ascontiguousarray(w_in[:, o4:]),
        "kvp": np.ascontiguousarray(kvn.reshape(2, 128).T),
        "kvb": np.ascontiguousarray(np.broadcast_to(kvn[None, :], (128, 256))),
        "wuk": f(a_w_uk[0]), "wuv": f(a_w_uv[0]),
        "ident": np.eye(128, dtype=np.float32),
        "wout0": f(a_w_out[0]), "wo1": f(b_w_out[0]),
    }
    cm = np.zeros((128, 4, 512), np.float32)
    for j in range(4):
        cm[:, j, :][np.arange(512)[None, :] > 128 * j + np.arange(128)[:, None]] = NEG
    shared["cmask"] = cm.reshape(128, 2048)
    sel = np.zeros((32, NE, 128), np.float32)
    for e in range(NE):
        sel[e, e, :] = 1.0
    shared["sel"] = sel.reshape(32, NE * 128)
    pc = lambda v: np.ascontiguousarray(f(v).reshape(8, 128).T)
    for Lr in range(2):
        shared[f"lnp{Lr}"] = np.concatenate([pc(ln1_g[Lr]), pc(ln1_b[Lr]), pc(ln2_g[Lr]), pc(ln2_b[Lr])], axis=1)
        shared[f"wr{Lr}"] = f(m_w_router[Lr])
        shared[f"brt{Lr}"] = np.ascontiguousarray(np.broadcast_to(f(m_b_router[Lr])[None, :], (128, 32)))
        shared[f"w1{Lr}"] = f(m_w1[Lr])
        shared[f"b1T{Lr}"] = np.ascontiguousarray(f(m_b1[Lr]).reshape(NE, 16, 128).transpose(2, 0, 1).reshape(128, NE * 16))
        shared[f"w2{Lr}"] = f(m_w2[Lr])
        shared[f"b2{Lr}"] = f(m_b2[Lr])
    jj = np.arange(64)
    tri = (jj[:, None] <= jj[None, :]).astype(np.float32)
    triU = (jj[:, None] > jj[None, :]).astype(np.float32)
    shared["triA"] = (-tri / 16.0).astype(np.float32)
    shared["triUA"] = (-triU / 16.0).astype(np.float32)
    shared["triM"] = tri
    bw = f(b_w_in[0])
    shared["gwq"] = np.ascontiguousarray(bw[:, 0:512]); shared["gwk"] = np.ascontiguousarray(bw[:, 512:1024])
    shared["gwv"] = np.ascontiguousarray(bw[:, 1024:2048]); shared["gwg"] = np.ascontiguousarray(bw[:, 2048:2064])
    shared["gwr"] = np.ascontiguousarray(bw[:, 2064:3088])
    shared["wg2"] = f(b_w_g2[0]); shared["gb"] = f(b_g_bias[0])[None, :]
    shared["ngb"] = np.ascontiguousarray(np.broadcast_to(f(b_norm[0])[None, :], (64, 1024)))
    xbT = [np.ascontiguousarray(x[b].T) for b in range(B)]
    in_maps = []
    for c in cores:
        b, j = c // 4, c % 4
        ms = np.zeros((128, 4), np.float32)
        ms[:, j] = 1.0
        m = dict(shared)
        m.update({"xbT": xbT[b], "msel": ms})
        in_maps.append(m)
    res = run_bass_kernel_spmd(nc, in_maps, core_ids=cores)
    out = np.empty((B, L, D), np.float32)
    for c in cores:
        b, j = c // 4, c % 4
        o = res.results[c]["outT"].T
        for s in range(16):
            out[b, (4 * s + j) * 128:(4 * s + j + 1) * 128] = o[s * 128:(s + 1) * 128]
    return out
```
